# Optimizing a Trainium2 kernel written in Bass

```python
import math
import jax, jax.numpy as jnp
from jax import lax
import numpy as np

D_MODEL = 1024
BATCH = 16
SEQ = 4096
DEPTH = 1

PLE_DIM = 256
EPS = 1e-6
FOX_HEADS = 8
FOX_HEAD_DIM = 64
FOX_BLOCK = 128
FOX_WIDTH = FOX_HEADS * FOX_HEAD_DIM
GDN_HEADS = 4
GDN_HEAD_DIM = 128
GDN_CONV = 4
GDN_CHUNK = 64
GDN_WIDTH = GDN_HEADS * GDN_HEAD_DIM
N_GROUPS = 4
EXPERTS_PER_GROUP = 8
N_EXPERTS = N_GROUPS * EXPERTS_PER_GROUP
TOP_K_IN_GROUP = 2
EXPERT_FF = 256
IN_SPLITS = (FOX_WIDTH, FOX_WIDTH, FOX_WIDTH, FOX_HEADS, 3 * GDN_WIDTH, GDN_HEADS, GDN_HEADS, GDN_WIDTH, D_MODEL, D_MODEL)
IN_WIDTH = 3 * FOX_WIDTH + FOX_HEADS + 4 * GDN_WIDTH + 2 * GDN_HEADS + 2 * D_MODEL

kernel_name = 'hybrid_fox_gdn_hmoe_block'


def rmsnorm(x, g):
    xf = x.astype(jnp.float32)
    y = xf * lax.rsqrt(jnp.mean(xf * xf, axis=-1, keepdims=True) + EPS)
    return (y * g.astype(jnp.float32)).astype(x.dtype)


def l2norm(x):
    xf = x.astype(jnp.float32)
    return xf * lax.rsqrt(jnp.sum(xf * xf, axis=-1, keepdims=True) + EPS)


def _split_columns(z, sizes):
    parts, start = [], 0
    for n in sizes:
        parts.append(z[..., start:start + n])
        start += n
    return parts


def _heads(t, n):
    return t.reshape(t.shape[:-1] + (n, -1))


def forgetting_attention(q, k, v, f_logit, b_forget):
    log_f = jax.nn.log_sigmoid(f_logit.astype(jnp.float32) + b_forget.astype(jnp.float32))
    cum = jnp.transpose(jnp.cumsum(log_f, axis=1), (0, 2, 1))
    scale = FOX_HEAD_DIM ** -0.5
    seq = q.shape[1]
    outs = []
    for blk in range(seq // FOX_BLOCK):
        lo, hi = blk * FOX_BLOCK, (blk + 1) * FOX_BLOCK
        s = jnp.einsum('bqhd,bkhd->bhqk', q[:, lo:hi], k[:, :hi], preferred_element_type=jnp.float32) * scale
        s = s + cum[:, :, lo:hi, None] - cum[:, :, None, :hi]
        causal = jnp.arange(hi)[None, :] <= (lo + jnp.arange(FOX_BLOCK))[:, None]
        pr = jax.nn.softmax(jnp.where(causal, s, -jnp.inf), axis=-1)
        outs.append(jnp.einsum('bhqk,bkhd->bqhd', pr.astype(v.dtype), v[:, :hi]))
    return jnp.concatenate(outs, axis=1)


def causal_depthwise_conv(x, w):
    c = x.shape[-1]
    return lax.conv_general_dilated(x, w[:, None, :].astype(x.dtype), window_strides=(1,),
                                    padding=[(GDN_CONV - 1, 0)],
                                    dimension_numbers=('NWC', 'WIO', 'NWC'),
                                    feature_group_count=c)


def gated_delta_rule(q, k, v, g, beta):
    bsz, seq, nh, dk = q.shape
    dv = v.shape[-1]
    C = GDN_CHUNK
    n = seq // C

    def chunks(t):
        t = t.reshape((bsz, n, C, nh) + t.shape[3:])
        return jnp.moveaxis(t, (1, 3), (0, 2))

    qc, kc, vc = chunks(q), chunks(k), chunks(v)
    bc = chunks(beta)
    gc = jnp.cumsum(chunks(g), axis=-1)
    idx = jnp.arange(C)
    lower = idx[:, None] >= idx[None, :]
    strict = idx[:, None] > idx[None, :]
    decay = jnp.exp(jnp.where(lower, gc[..., :, None] - gc[..., None, :], -jnp.inf))
    kb = kc * bc[..., None]
    lmat = jnp.where(strict, jnp.einsum('nbhid,nbhjd->nbhij', kb, kc) * decay, 0.0)
    amat = lmat + jnp.eye(C, dtype=jnp.float32)
    rhs = jnp.concatenate([vc * bc[..., None], kb * jnp.exp(gc)[..., None]], axis=-1)
    sol = lax.linalg.triangular_solve(amat, rhs, left_side=True, lower=True, unit_diagonal=True)
    u, w = sol[..., :dv], sol[..., dv:]
    intra = jnp.einsum('nbhid,nbhjd->nbhij', qc, kc) * decay
    q_dec = qc * jnp.exp(gc)[..., None]
    k_dec = kc * jnp.exp(gc[..., -1:] - gc)[..., None]
    g_last = jnp.exp(gc[..., -1])

    def step(state, xs):
        q_i, k_i, u_i, w_i, a_i, g_i = xs
        v_new = u_i - jnp.einsum('bhcd,bhde->bhce', w_i, state)
        o = jnp.einsum('bhcd,bhde->bhce', q_i, state) + jnp.einsum('bhij,bhje->bhie', a_i, v_new)
        state = state * g_i[..., None, None] + jnp.einsum('bhcd,bhce->bhde', k_i, v_new)
        return state, o

    state0 = jnp.zeros((bsz, nh, dk, dv), jnp.float32)
    _, o = lax.scan(step, state0, (q_dec, k_dec, u, w, intra, g_last))
    return jnp.moveaxis(o, (0, 2), (1, 3)).reshape(bsz, seq, nh, dv)


def hybrid_mixer(h, w_in, b_forget, conv_w, a_log, dt_bias, g_onorm, w_o_fox, w_o_delta, w_out):
    f32 = jnp.float32
    z = h @ w_in
    fq, fk, fv, ff, qkv, da, db, dz, gate_fox, gate_delta = _split_columns(z, IN_SPLITS)
    y_fox = forgetting_attention(_heads(fq, FOX_HEADS), _heads(fk, FOX_HEADS), _heads(fv, FOX_HEADS), ff, b_forget)
    y_fox = y_fox.reshape(h.shape[:-1] + (FOX_WIDTH,)) @ w_o_fox
    qkv = jax.nn.silu(causal_depthwise_conv(qkv, conv_w)).astype(f32)
    dq, dk, dv = _split_columns(qkv, (GDN_WIDTH, GDN_WIDTH, GDN_WIDTH))
    q = l2norm(_heads(dq, GDN_HEADS)) * GDN_HEAD_DIM ** -0.5
    k = l2norm(_heads(dk, GDN_HEADS))
    v = _heads(dv, GDN_HEADS)
    beta = jax.nn.sigmoid(db.astype(f32))
    g = -jnp.exp(a_log.astype(f32)) * jax.nn.softplus(da.astype(f32) + dt_bias.astype(f32))
    o = gated_delta_rule(q, k, v, g, beta)
    o = rmsnorm(o, g_onorm) * jax.nn.silu(_heads(dz, GDN_HEADS).astype(f32))
    y_delta = o.reshape(h.shape[:-1] + (GDN_WIDTH,)).astype(h.dtype) @ w_o_delta
    merged = jax.nn.sigmoid(gate_fox) * y_fox + jax.nn.sigmoid(gate_delta) * y_delta
    return merged @ w_out


def hierarchical_moe(h, w_group, b_group, w_router, b_router, w_gate, w_up, w_down):
    f32 = jnp.float32
    bsz, seq, d = h.shape
    t = h.reshape(-1, d)
    gl = (t @ w_group).astype(f32) + b_group.astype(f32)
    pg = jax.nn.softmax(gl, axis=-1)
    g_sel = jnp.argmax(gl, axis=-1)
    p_sel = jnp.take_along_axis(pg, g_sel[:, None], axis=-1)
    el = ((t @ w_router).astype(f32) + b_router.astype(f32)).reshape(-1, N_GROUPS, EXPERTS_PER_GROUP)
    el = jnp.take_along_axis(el, g_sel[:, None, None], axis=1)[:, 0]
    pe = jax.nn.softmax(el, axis=-1)
    top_p, top_i = lax.top_k(pe, TOP_K_IN_GROUP)
    top_p = top_p / jnp.sum(top_p, axis=-1, keepdims=True)
    w_grp = jnp.sum(jax.nn.one_hot(top_i, EXPERTS_PER_GROUP, dtype=f32) * top_p[..., None], axis=1)
    gate = (jax.nn.one_hot(g_sel, N_GROUPS, dtype=f32)[:, :, None] * (p_sel * w_grp)[:, None, :]).reshape(-1, N_EXPERTS)
    gate = gate.astype(t.dtype)
    out = jnp.zeros_like(t)
    for e in range(N_EXPERTS):
        hid = jax.nn.silu(t @ w_gate[e]) * (t @ w_up[e])
        out = out + gate[:, e:e + 1] * (hid @ w_down[e])
    return out.reshape(bsz, seq, d)


def setup_inputs(seed: int = 0) -> dict:
    key = jax.random.key(seed)
    ks = jax.random.split(key, 24)
    f32 = jnp.float32
    L = DEPTH

    def nrm(k, shape, fan_in):
        return jax.random.normal(k, shape, f32) * fan_in ** -0.5

    def gain(k, shape):
        return 1.0 + 0.02 * jax.random.normal(k, shape, f32)

    dt = jnp.exp(jax.random.uniform(ks[7], (L, GDN_HEADS), f32, minval=math.log(1e-3), maxval=math.log(1e-1)))
    return {
        'x': jax.random.normal(ks[0], (BATCH, SEQ, D_MODEL), f32),
        'p': jax.random.normal(ks[1], (L, BATCH, SEQ, PLE_DIM), f32),
        'g_mix': gain(ks[2], (L, D_MODEL)),
        'w_in': nrm(ks[3], (L, D_MODEL, IN_WIDTH), D_MODEL),
        'b_forget': jax.random.uniform(ks[4], (L, FOX_HEADS), f32, minval=1.0, maxval=6.0),
        'conv_w': nrm(ks[5], (L, GDN_CONV, 3 * GDN_WIDTH), GDN_CONV),
        'a_log': jnp.log(jax.random.uniform(ks[6], (L, GDN_HEADS), f32, minval=1.0, maxval=16.0)),
        'dt_bias': dt + jnp.log(-jnp.expm1(-dt)),
        'g_onorm': gain(ks[8], (L, GDN_HEAD_DIM)),
        'w_o_fox': nrm(ks[9], (L, FOX_WIDTH, D_MODEL), FOX_WIDTH),
        'w_o_delta': nrm(ks[10], (L, GDN_WIDTH, D_MODEL), GDN_WIDTH),
        'w_out': nrm(ks[11], (L, D_MODEL, D_MODEL), D_MODEL),
        'g_ffn': gain(ks[12], (L, D_MODEL)),
        'w_group': nrm(ks[13], (L, D_MODEL, N_GROUPS), D_MODEL),
        'b_group': 0.01 * jax.random.normal(ks[14], (L, N_GROUPS), f32),
        'w_router': nrm(ks[15], (L, D_MODEL, N_EXPERTS), D_MODEL),
        'b_router': 0.01 * jax.random.normal(ks[16], (L, N_EXPERTS), f32),
        'w_gate': nrm(ks[17], (L, N_EXPERTS, D_MODEL, EXPERT_FF), D_MODEL),
        'w_up': nrm(ks[18], (L, N_EXPERTS, D_MODEL, EXPERT_FF), D_MODEL),
        'w_down': nrm(ks[19], (L, N_EXPERTS, EXPERT_FF, D_MODEL), EXPERT_FF),
        'g_ple': gain(ks[20], (L, D_MODEL)),
        'w_ple_gate': nrm(ks[21], (L, D_MODEL, D_MODEL), D_MODEL),
        'w_ple_proj': nrm(ks[22], (L, PLE_DIM, D_MODEL), PLE_DIM),
        'g_final': gain(ks[23], (D_MODEL,)),
    }


def reference(x, p, g_mix, w_in, b_forget, conv_w, a_log, dt_bias, g_onorm, w_o_fox, w_o_delta, w_out,
              g_ffn, w_group, b_group, w_router, b_router, w_gate, w_up, w_down,
              g_ple, w_ple_gate, w_ple_proj, g_final):
    for i in range(DEPTH):
        h = rmsnorm(x, g_mix[i])
        x = x + hybrid_mixer(h, w_in[i], b_forget[i], conv_w[i], a_log[i], dt_bias[i], g_onorm[i],
                             w_o_fox[i], w_o_delta[i], w_out[i])
        x = x + hierarchical_moe(rmsnorm(x, g_ffn[i]), w_group[i], b_group[i], w_router[i], b_router[i],
                                 w_gate[i], w_up[i], w_down[i])
        ple_gate = jax.nn.sigmoid(rmsnorm(x, g_ple[i]) @ w_ple_gate[i])
        x = x + ple_gate * (p[i] @ w_ple_proj[i])
    return rmsnorm(x, g_final)
```

```python
import numpy as np
from contextlib import ExitStack
import concourse.bass as bass
import concourse.mybir as mybir
from concourse.bass_utils import run_bass_kernel_spmd

F32 = mybir.dt.float32
BF16 = mybir.dt.bfloat16
AF = mybir.ActivationFunctionType
ALU = mybir.AluOpType

NCORES = 8
D = 1024
EPS = 1e-6
NEG = -60000.0
SAME_ENGINE_SYNC = True
FOX_FILL = False
FOX_K = 128
FOX_M = 128
FOX_FILL_N = 256


class Res:
    __slots__ = ("name", "w", "r", "nowaw")

    def __init__(self, name="", nowaw=False):
        self.name = name
        self.nowaw = nowaw
        self.w = {}
        self.r = {}


class KB:
    ENGS = ("pe", "act", "dve", "pool", "sp")
    NDSEM = 20

    def __init__(self, nc, stack):
        self.nc = nc
        self.stack = stack
        self.sem = {}
        self.count = {}
        self.prog = {e: [] for e in self.ENGS}
        self.waited = {e: {} for e in self.ENGS}
        for e in self.ENGS:
            self.sem[e] = stack.enter_context(nc.semaphore("s_" + e))
            self.count[e] = 0
        self.dpool = {}
        self.dnext = {}
        for q in ("sp", "pool"):
            self.dpool[q] = []
            self.dnext[q] = 0
            for i in range(self.NDSEM):
                k = f"d{q}{i}"
                self.sem[k] = stack.enter_context(nc.semaphore(k))
                self.count[k] = 0
                self.dpool[q].append(k)

    def res(self, name=""):
        return Res(name)

    def _need(self, eng, reads, writes, extra=()):
        need = {}

        def add(k, v):
            if need.get(k, 0) < v:
                need[k] = v
        for k, v in extra:
            add(k, v)
        for r in reads:
            for k, v in r.w.items():
                add(k, v)
        for w in writes:
            if not w.nowaw:
                for k, v in w.w.items():
                    add(k, v)
            for k, v in w.r.items():
                add(k, v)
        out = []
        for k, v in need.items():
            if k == eng and (not SAME_ENGINE_SYNC or eng == "pe" or v > self.count[eng]):
                continue
            if self.waited[eng].get(k, 0) >= v:
                continue
            self.waited[eng][k] = v
            out.append((k, v))
        return out

    def _commit(self, ticket, reads, writes):
        k, v = ticket
        for w in writes:
            if w.w.get(k, 0) < v:
                w.w[k] = v
            w.r = {}
        for r in reads:
            if r.r.get(k, 0) < v:
                r.r[k] = v

    def op(self, eng, fn, reads=(), writes=(), sig=True):
        for k, v in self._need(eng, reads, writes):
            self.prog[eng].append(("wait", k, v))
        if sig:
            self.count[eng] += 1
            ticket = (eng, self.count[eng])
            self.prog[eng].append(("op", fn, eng, 1))
        else:
            ticket = (eng, self.count[eng] + 1)
            self.prog[eng].append(("op", fn, None, 0))
        self._commit(ticket, reads, writes)

    def dma(self, queue, stream, fn, reads=(), writes=()):
        pool = self.dpool[queue]
        k = pool[self.dnext[queue] % len(pool)]
        self.dnext[queue] += 1
        extra = [(k, self.count[k])] if self.count[k] > 0 else []
        for kk, v in self._need(queue, reads, writes, extra):
            self.prog[queue].append(("wait", kk, v))
        self.count[k] += 16
        ticket = (k, self.count[k])
        self.prog[queue].append(("op", fn, k, 16))
        self._commit(ticket, reads, writes)

    def emit(self):
        nc = self.nc
        with nc.Block() as block:
            def run(engname, e):
                for item in self.prog[engname]:
                    if item[0] == "wait":
                        e.wait_ge(self.sem[item[1]], item[2])
                    else:
                        _, fn, semk, inc = item
                        ins = fn(e)
                        if semk is not None:
                            ins.then_inc(self.sem[semk], inc)

            @block.tensor
            def _(e):
                run("pe", e)

            @block.scalar
            def _(e):
                run("act", e)

            @block.vector
            def _(e):
                run("dve", e)

            @block.gpsimd
            def _(e):
                run("pool", e)

            @block.sync
            def _(e):
                run("sp", e)


def host_consts():
    c = {}
    c["identF"] = np.eye(128, dtype=np.float32)
    p = np.arange(128)[:, None]
    f = np.arange(128)[None, :]
    c["mask_u"] = np.where(f >= p, 0.0, NEG).astype(np.float32)
    c["mask_l"] = np.where(f >= p, -NEG, 0.0).astype(np.float32)
    sel = np.zeros((128, 4, 128), np.float32)
    for h in range(4):
        sel[32 + h, h, :] = 1.0
    c["selA"] = sel.reshape(128, 512)
    c["tstart"] = np.tile((np.arange(160, dtype=np.float32) * 128.0)[None, :], (128, 1))
    c["pcol"] = np.arange(128, dtype=np.float32)[:, None]
    c["ustrict"] = (p < f).astype(np.float32)
    return c


def build(NSEQ, S, stages=99, debug=False):
    T = NSEQ * S
    NT = S // 128
    NG = S // 512
    nc = bass.Bass("TRN2", target_bir_lowering=False)

    def din(name, shape, dt=F32):
        return nc.dram_tensor(name, list(shape), dt, kind="ExternalInput").ap()

    def dscr(name, shape, dt=BF16):
        return nc.dram_tensor(name, list(shape), dt, kind="ExternalOutput" if debug else "Internal").ap()

    x_d = din("x", [T, D])
    p_d = din("p", [T, 256])
    g_mix = din("g_mix", [D]); w_in = din("w_in", [D, 5648]); b_forget = din("b_forget", [8])
    conv_w = din("conv_w", [4, 1536]); a_log = din("a_log", [4]); dt_bias = din("dt_bias", [4])
    g_onorm = din("g_onorm", [128]); w_o_fox = din("w_o_fox", [512, D]); w_o_delta = din("w_o_delta", [512, D])
    w_out = din("w_out", [D, D]); g_ffn = din("g_ffn", [D]); w_group = din("w_group", [D, 4])
    b_group = din("b_group", [4]); w_router = din("w_router", [D, 32]); b_router = din("b_router", [32])
    w_gate = din("w_gate", [32, D, 256]); w_up = din("w_up", [32, D, 256]); w_down = din("w_down", [32, 256, D])
    g_ple = din("g_ple", [D]); w_ple_gate = din("w_ple_gate", [D, D]); w_ple_proj = din("w_ple_proj", [256, D])
    g_final = din("g_final", [D])
    identF_d = din("identF", [128, 128]); mask_u_d = din("mask_u", [128, 128]); mask_l_d = din("mask_l", [128, 128])
    selA_d = din("selA", [128, 512])
    out_d = nc.dram_tensor("out", [T, D], F32, kind="ExternalOutput").ap()

    WX = dscr("WX", [32 * 128, 6144])
    NSLT_ = (2 * S) // 128 + 32
    Xg = [dscr(f"Xg{s}", [NSLT_ * 128, D]) for s in range(NSEQ)]
    Yg = [dscr(f"Yg{s}", [NSLT_ * 128, D]) for s in range(NSEQ)]
    X1 = [dscr(f"X1{s}", [S, D], F32) for s in range(NSEQ)]
    tstart_d = din("tstart", [128, 160]); pcol_d = din("pcol", [128, 1]); ustrict_d = din("ustrict", [128, 128])
    QT = [dscr(f"QT{s}", [512, S]) for s in range(NSEQ)]
    KT = [dscr(f"KT{s}", [512, S]) for s in range(NSEQ)]
    GQKV = [dscr(f"GQKV{s}", [1536, S]) for s in range(NSEQ)]
    DZ = [dscr(f"DZ{s}", [512, S]) for s in range(NSEQ)]
    THF = [dscr(f"THF{s}", [D, S]) for s in range(NSEQ)]
    THD = [dscr(f"THD{s}", [D, S]) for s in range(NSEQ)]
    AUGQ = [dscr(f"AUGQ{s}", [8, 6, S]) for s in range(NSEQ)]
    AUGK = [dscr(f"AUGK{s}", [8, 6, S]) for s in range(NSEQ)]
    ATs = [dscr(f"AT{s}", [512, S]) for s in range(NSEQ)]
    OGs = [dscr(f"OG{s}", [512, S]) for s in range(NSEQ)]
    GTs = [dscr(f"GT{s}", [32, S]) for s in range(NSEQ)]

    with ExitStack() as st:
        kb = KB(nc, st)

        ARENA_BYTES = 209920
        arena_t = st.enter_context(nc.sbuf_tensor("arena", [128, ARENA_BYTES // 4], F32))
        arena_off = [0]
        arena_hw = [0]

        def sb(name, shape, dt):
            esz = 2 if dt == BF16 else 4
            n = 1
            for d_ in shape[1:]:
                n *= d_
            nbytes = (n * esz + 31) // 32 * 32
            o = arena_off[0]
            assert o + nbytes <= ARENA_BYTES, (name, o, nbytes)
            arena_off[0] = o + nbytes
            arena_hw[0] = max(arena_hw[0], o + nbytes)
            v = arena_t[:, o // 4:(o + nbytes) // 4]
            if dt != F32:
                v = v.bitcast(dt)
            v = v[:, 0:n]
            if len(shape) == 3:
                v = v.rearrange("p (a b) -> p a b", a=shape[1])
            elif len(shape) == 4:
                v = v.rearrange("p (a b c) -> p a b c", a=shape[1], b=shape[2])
            return v

        def R(name=""):
            return kb.res(name)

        def mm(out, lhsT, rhs, start, stop, reads, writes, sig=None):
            if sig is None:
                sig = stop
            kb.op("pe", lambda e: e.matmul(out, lhsT=lhsT, rhs=rhs, start=start, stop=stop),
                  reads=reads, writes=writes, sig=sig)

        def tr(out, in_, ident, reads, writes):
            kb.op("pe", lambda e: e.transpose(out, in_, ident), reads=reads, writes=writes)

        def act(out, in_, func, reads, writes, bias=None, scale=None, accum=None):
            kw = {}
            if bias is not None:
                kw["bias"] = bias
            if scale is not None:
                kw["scale"] = scale
            if accum is not None:
                kw["accum_out"] = accum
            kb.op("act", lambda e: e.activation(out=out, in_=in_, func=func, **kw), reads=reads, writes=writes)

        def ts(eng, out, in0, s1, s2, op0, op1, reads, writes):
            if s2 is None:
                kb.op(eng, lambda e: e.tensor_scalar(out=out, in0=in0, scalar1=s1, scalar2=None, op0=op0),
                      reads=reads, writes=writes)
            else:
                kb.op(eng, lambda e: e.tensor_scalar(out=out, in0=in0, scalar1=s1, scalar2=s2, op0=op0, op1=op1),
                      reads=reads, writes=writes)

        def tt(eng, out, in0, in1, op, reads, writes):
            kb.op(eng, lambda e: e.tensor_tensor(out=out, in0=in0, in1=in1, op=op), reads=reads, writes=writes)

        def stt(out, in0, scalar, in1, op0, op1, reads, writes, eng="dve"):
            kb.op(eng, lambda e: e.scalar_tensor_tensor(out=out, in0=in0, scalar=scalar, in1=in1, op0=op0, op1=op1),
                  reads=reads, writes=writes)

        def cp(eng, out, in_, reads, writes):
            if eng == "act":
                kb.op("act", lambda e: e.copy(out, in_), reads=reads, writes=writes)
            else:
                kb.op(eng, lambda e: e.tensor_copy(out, in_), reads=reads, writes=writes)

        def ms(eng, ap, val, writes):
            kb.op(eng, lambda e: e.memset(ap, val), writes=writes)

        def ld(out, in_, writes, reads=(), stream="ld", q="sp", nonc=False):
            if nonc:
                kb.dma(q, stream, lambda e: e.dma_start(out=out, in_=in_, allow_slow_non_contiguous=True),
                       reads=reads, writes=writes)
            else:
                kb.dma(q, stream, lambda e: e.dma_start(out=out, in_=in_), reads=reads, writes=writes)

        def ldc(out, in_, writes, reads=(), stream="ldc", nonc=False):
            ld(out, in_, writes, reads, stream=stream, q="pool", nonc=nonc)

        def stq(out, in_, reads, writes=(), stream="st"):
            kb.dma("sp", stream, lambda e: e.dma_start(out=out, in_=in_), reads=reads, writes=writes)

        dbg_names = []

        def dbg(name, ap, reads, dt=F32):
            if not debug:
                return
            shp = list(ap.shape)
            d_ = nc.dram_tensor("dbg_" + name, shp, dt, kind="ExternalOutput").ap()
            dbg_names.append("dbg_" + name)
            idx = tuple(slice(None) for _ in shp)
            stq(d_[idx], ap, reads)

        class Rot:
            def __init__(self, name, shape, dt, n):
                self.t = [sb(f"{name}{i}", shape, dt) for i in range(n)]
                self.r = [R(f"{name}{i}") for i in range(n)]
                self.i = 0
                self.n = n

            def next(self):
                k = self.i % self.n
                self.i += 1
                return self.t[k], self.r[k]

        def barrier():
            keys = list(kb.count.keys())
            for e_ in KB.ENGS:
                for k in keys:
                    v = kb.count[k]
                    if v > 0 and kb.waited[e_].get(k, 0) < v and k != e_:
                        kb.waited[e_][k] = v
                        kb.prog[e_].append(("wait", k, v))

        PS = [st.enter_context(nc.psum_tensor(f"ps{i}", [128, 512], F32)) for i in range(7)]
        PR = [R(f"ps{i}") for i in range(7)]
        PB = st.enter_context(nc.psum_tensor("psb", [128, 1024], BF16))
        PBR = R("psb")

        identF = sb("identF", [128, 128], F32); r_identF = R()
        identB = sb("identB", [128, 128], BF16); r_identB = R()
        ident4F = sb("ident4F", [128, 512], F32); r_ident4F = R()
        masku = sb("masku", [128, 128], BF16); r_masku = R()
        maskl = sb("maskl", [128, 128], BF16); r_maskl = R()
        selA = sb("selA", [128, 512], F32); r_selA = R()
        onesF = sb("onesF", [128, 128], F32); r_onesF = R()
        onesB = sb("onesB", [128, 128], BF16); r_onesB = R()
        nhalf = sb("nhalf", [128, 512], F32); r_nhalf = R()
        onesS = sb("onesS", [128, 1024], BF16); r_onesS = R()
        ld(identF[:], identF_d[:, :], [r_identF])
        ldc(identB[:], identF_d[:, :], [r_identB])
        for h in range(4):
            ld(ident4F[:, h * 128:(h + 1) * 128], identF_d[:, :], [r_ident4F])
        ldc(masku[:], mask_u_d[:, :], [r_masku])
        ldc(maskl[:], mask_l_d[:, :], [r_maskl])
        ld(selA[:], selA_d[:, :], [r_selA])
        ms("dve", onesF[:], 1.0, [r_onesF])
        ms("dve", onesB[:], 1.0, [r_onesB])
        ms("dve", nhalf[:], -0.5, [r_nhalf])
        ms("dve", onesS[:], 1.0, [r_onesS])

        def colvec(name, src, nch):
            t = sb(name, [128, nch], F32); r = R()
            ld(t[:], src.rearrange("(c p) -> p c", p=128), [r], nonc=True)
            return t, r
        gmixT, r_gmixT = colvec("gmixT", g_mix, 8)
        gffnT, r_gffnT = colvec("gffnT", g_ffn, 8)
        gpleT, r_gpleT = colvec("gpleT", g_ple, 8)
        gonT, r_gonT = colvec("gonT", g_onorm, 1)
        gfin = sb("gfin", [128, D], F32); r_gfin = R()
        ld(gfin[:], g_final[None, :].to_broadcast([128, D]), [r_gfin])
        cw = sb("cw", [128, 12, 4], F32); r_cw = R()
        for j in range(4):
            ld(cw[:, :, j], conv_w[j, :].rearrange("(c p) -> p c", p=128), [r_cw], nonc=True)
        wrt_sb = sb("wrt", [128, 8, 36], BF16); r_wrt = R()
        ldc(wrt_sb[:, :, 0:4], w_group.rearrange("(k p) c -> p k c", p=128), [r_wrt], nonc=True)
        ldc(wrt_sb[:, :, 4:36], w_router.rearrange("(k p) c -> p k c", p=128), [r_wrt], nonc=True)
        brt = sb("brt", [128, 36], F32); r_brt = R()
        ld(brt[:, 0:4], b_group[None, :].to_broadcast([128, 4]), [r_brt])
        ld(brt[:, 4:36], b_router[None, :].to_broadcast([128, 32]), [r_brt])
        tots = sb("tots", [128, max(NT, 8)], F32); r_tots = R()
        C_FF, C_QKV, C_DA, C_DB, C_DZ, C_GF, C_GD = 1536, 1544, 3080, 3084, 3088, 3600, 4624

        def wcols(c0, n):
            return w_in[:, c0:c0 + n].rearrange("(k p) c -> p k c", p=128)

        def wres(name, src, kch, ncol):
            t = sb(name, [128, kch, ncol], BF16); r = R()
            ldc(t[:], src.rearrange("(k p) c -> p k c", p=128), [r])
            return t, r
        prmA = sb("prmA", [128, 2], F32); r_prmA = R()
        prmB = sb("prmB", [128, 2], F32); r_prmB = R()
        ms("dve", prmA[:], 0.0, [r_prmA])
        ms("dve", prmB[:], 0.0, [r_prmB])
        ld(prmA[0:8, 0:1], b_forget[:, None], [r_prmA], nonc=True)
        for o in (32, 64, 96):
            ld(prmA[o:o + 4, 0:1], dt_bias[:, None], [r_prmA], nonc=True)
            ld(prmA[o:o + 4, 1:2], a_log[:, None], [r_prmA], nonc=True)
        ld(prmB[64:68, 0:1], dt_bias[:, None], [r_prmB], nonc=True)
        ld(prmB[64:68, 1:2], a_log[:, None], [r_prmB], nonc=True)
        ts("dve", prmA[0:8, 0:1], prmA[0:8, 0:1], -1.0, None, ALU.mult, None, [r_prmA], [r_prmA])
        act(prmA[:, 1:2], prmA[:, 1:2], AF.Exp, [r_prmA], [r_prmA])
        ts("dve", prmA[:, 1:2], prmA[:, 1:2], -1.0, None, ALU.mult, None, [r_prmA], [r_prmA])
        act(prmB[:, 1:2], prmB[:, 1:2], AF.Exp, [r_prmB], [r_prmB])
        ts("dve", prmB[:, 1:2], prmB[:, 1:2], -1.0, None, ALU.mult, None, [r_prmB], [r_prmB])

        r_wexp = Res("wexp", nowaw=True)
        if stages >= 7:
            for e_ in range(32):
                rows = WX[e_ * 128:(e_ + 1) * 128, :]
                gu = rows[:, 0:4096].rearrange("p (k t f) -> p k t f", k=8, t=2)
                ldc(gu[:, :, 0, :], w_gate[e_].rearrange("(k p) f -> p k f", p=128), [r_wexp], stream="wx")
                ldc(gu[:, :, 1, :], w_up[e_].rearrange("(k p) f -> p k f", p=128), [r_wexp], stream="wx")
                ldc(rows[:, 4096:6144].rearrange("p (c f) -> p c f", c=2), w_down[e_].rearrange("(c p) f -> p c f", p=128),
                    [r_wexp], stream="wx")

        def RD():
            return Res("dram", nowaw=True)
        r_QT = [RD() for _ in range(NSEQ)]; r_KT = [RD() for _ in range(NSEQ)]; r_GQKV = [RD() for _ in range(NSEQ)]
        r_DZ = [RD() for _ in range(NSEQ)]; r_THF = [RD() for _ in range(NSEQ)]; r_THD = [RD() for _ in range(NSEQ)]
        r_aug = [RD() for _ in range(NSEQ)]; r_AT = [RD() for _ in range(NSEQ)]; r_OG = [RD() for _ in range(NSEQ)]
        r_GT = [RD() for _ in range(NSEQ)]; r_out = RD()
        r_Xg = [RD() for _ in range(NSEQ)]; r_Yg = [RD() for _ in range(NSEQ)]; r_X1 = [RD() for _ in range(NSEQ)]
        r_Xgz = [RD() for _ in range(NSEQ)]
        tstart = sb("tstart", [128, 160], F32); r_tstart = R()
        pcol = sb("pcol", [128, 1], F32); r_pcol = R()
        ustrict = sb("ustrict", [128, 128], BF16); r_ustrict = R()
        ld(tstart[:], tstart_d[:, :], [r_tstart])
        ld(pcol[:], pcol_d[:, :], [r_pcol])
        ldc(ustrict[:], ustrict_d[:, :], [r_ustrict])

        junk = sb("junk", [128, D], BF16); r_junk = R()
        st1_rot = Rot("st1", [128, 2], F32, 10)
        MARK0 = arena_off[0]

        def norm_stats(src, r_src):
            s1, r_s1 = st1_rot.next()
            ms("dve", s1[:, 0:1], 0.0, [r_s1])
            act(junk[:], src, AF.Square, [r_src, r_s1], [r_junk, r_s1], accum=s1[:, 0:1])
            ts("dve", s1[:, 0:1], s1[:, 0:1], 1.0 / D, EPS, ALU.mult, ALU.add, [r_s1], [r_s1])
            tt("pool", s1[:, 1:2], s1[:, 0:1], nhalf[:, 0:1], ALU.pow, [r_s1, r_nhalf], [r_s1])
            return s1, r_s1

        def norm_transpose(src, r_src, gT, r_gT, dst_fn, r_dst, xn_rot):
            s1, r_s1 = norm_stats(src, r_src)
            xn, r_xn = xn_rot.next()
            ts("dve", xn[:], src, s1[:, 1:2], None, ALU.mult, None, [r_src, r_s1], [r_xn])
            for c in range(8):
                tr(PB[:, c * 128:(c + 1) * 128], xn[:, c * 128:(c + 1) * 128], identB[:], [r_xn, r_identB], [PBR])
            for c in range(8):
                if c % 2 == 0:
                    act(dst_fn(c), PB[:, c * 128:(c + 1) * 128], AF.Copy, [PBR, r_gT], [r_dst], scale=gT[:, c:c + 1])
                else:
                    ts("dve", dst_fn(c), PB[:, c * 128:(c + 1) * 128], gT[:, c:c + 1], None, ALU.mult, None,
                       [PBR, r_gT], [r_dst])

        scale_q = 64 ** -0.5
        scale_gq = 128 ** -0.5

        for s in range(NSEQ):
            tok0 = s * S
            barrier()
            arena_off[0] = MARK0
            ZA = sb("ZA", [128, S], F32); r_ZA = R("ZA")
            ZB = sb("ZB", [128, S], F32); r_ZB = R("ZB")
            MARK1 = arena_off[0]
            Vall = sb("Vall", [128, NT, 8, 65], BF16); r_V = [R(f"V{i}") for i in range(NT)]
            MARK2 = arena_off[0]
            ms("dve", Vall[:], 1.0, r_V)
            hT = sb("hT", [128, 8, S], BF16)
            r_hT = [R(f"hT{i}") for i in range(NT)]
            wv_sb, r_wv = wres("wv", w_in[:, 1024:1536], 8, 512)
            wgA = sb("wgA", [128, 8, 128], BF16); r_wgA = R()
            wgB = sb("wgB", [128, 8, 128], BF16); r_wgB = R()
            ms("dve", wgA[:], 0.0, [r_wgA])
            ms("dve", wgB[:], 0.0, [r_wgB])
            ldc(wgA[:, :, 0:8], wcols(C_FF, 8), [r_wgA], nonc=True)
            for o in (32, 64, 96):
                ldc(wgA[:, :, o:o + 4], wcols(C_DA, 4), [r_wgA], nonc=True)
            ldc(wgB[:, :, 0:4], wcols(C_DB, 4), [r_wgB], nonc=True)
            ldc(wgB[:, :, 32:36], wcols(C_DB, 4), [r_wgB], nonc=True)
            ldc(wgB[:, :, 64:68], wcols(C_DA, 4), [r_wgB], nonc=True)
            xt_rot = Rot("xt", [128, D], F32, 2)
            xn_rot = Rot("xn", [128, D], BF16, 4)
            stg_rot = Rot("stg", [128, 512], BF16, 6)
            f32_rot = Rot("f32w", [128, 520], F32, 5)
            win_rot = Rot("win", [128, 8, 128], BF16, 3)
            p1st = {}

            def p1_a(i):
                xt, r_xt = xt_rot.next()
                ld(xt[:], x_d[tok0 + i * 128: tok0 + (i + 1) * 128, :], [r_xt])
                s1, r_s1 = norm_stats(xt[:], r_xt)
                xn, r_xn = xn_rot.next()
                ts("dve", xn[:], xt[:], s1[:, 1:2], None, ALU.mult, None, [r_xt, r_s1], [r_xn])
                p1st[i] = (xn, r_xn)

            def p1_b(i):
                xn, r_xn = p1st.pop(i)
                for c in range(8):
                    tr(PB[:, c * 128:(c + 1) * 128], xn[:, c * 128:(c + 1) * 128], identB[:], [r_xn, r_identB], [PBR])
                for c in range(8):
                    dstc = hT[:, c, i * 128:(i + 1) * 128]
                    if c % 2 == 0:
                        act(dstc, PB[:, c * 128:(c + 1) * 128], AF.Copy, [PBR, r_gmixT], [r_hT[i]], scale=gmixT[:, c:c + 1])
                    else:
                        ts("dve", dstc, PB[:, c * 128:(c + 1) * 128], gmixT[:, c:c + 1], None, ALU.mult, None,
                           [PBR, r_gmixT], [r_hT[i]])
            for t in range(NT + 2):
                if t < NT:
                    p1_a(t)
                if 0 <= t - 2 < NT:
                    p1_b(t - 2)
            if stages < 1.1:
                continue
            for i in range(NT):
                b = i % 2
                for k in range(8):
                    mm(PS[b][:, :], hT[:, k, i * 128:(i + 1) * 128], wv_sb[:, k, :], k == 0, k == 7,
                       [r_hT[i], r_wv], [PR[b]])
                cp("act" if i % 2 else "dve", Vall[:, i, :, 0:64],
                   PS[b][:, :].rearrange("p (h d) -> p h d", h=8), [PR[b]], [r_V[i]])
            if stages < 1.2:
                continue
            for g in range(NG):
                gt = [r_hT[4 * g + j] for j in range(4)]
                for (wg_, r_wg_, Z, r_Z, b) in ((wgA, r_wgA, ZA, r_ZA, 2), (wgB, r_wgB, ZB, r_ZB, 3)):
                    for k in range(8):
                        mm(PS[b][:, :], wg_[:, k, :], hT[:, k, g * 512:(g + 1) * 512], k == 0, k == 7,
                           gt + [r_wg_], [PR[b]])
                    cp("act", Z[:, g * 512:(g + 1) * 512], PS[b][:, :], [PR[b]], [r_Z])
            if stages < 1.3:
                continue
            chunks = []
            for c in range(4):
                chunks.append((c * 128, "q", c))
            for c in range(4):
                chunks.append((512 + c * 128, "k", c))
            for c in range(12):
                chunks.append((C_QKV + c * 128, "gdn", c))
            for c in range(4):
                chunks.append((C_DZ + c * 128, "dz", c))
            for c in range(8):
                chunks.append((C_GF + c * 128, "gf", c))
            for c in range(8):
                chunks.append((C_GD + c * 128, "gd", c))
            bsel = 0
            p2cnt = [0]
            p2pend = [None]
            if stages < 1.4:
                chunks = chunks[0:8]
            elif stages < 1.5:
                chunks = chunks[0:20]
            for (c0, kind, ci) in chunks:
                wt, r_wt = win_rot.next()
                ldc(wt[:], wcols(c0, 128), [r_wt], stream="ldw")
                halo, r_halo = None, None
                for g in range(NG):
                    gt = [r_hT[4 * g + j] for j in range(4)]
                    b = 4 + (bsel % 2)
                    bsel += 1
                    for k in range(8):
                        mm(PS[b][:, :], wt[:, k, :], hT[:, k, g * 512:(g + 1) * 512], k == 0, k == 7,
                           gt + [r_wt], [PR[b]])
                    cols = slice(g * 512, (g + 1) * 512)
                    if not (kind == "gdn" and ci < 8) and p2pend[0] is not None:
                        p2pend[0]()
                        p2pend[0] = None
                    stg, r_stg = stg_rot.next()
                    if kind == "q":
                        act(stg[:], PS[b][:, :], AF.Copy, [PR[b]], [r_stg], scale=scale_q)
                        stq(QT[s][ci * 128:(ci + 1) * 128, cols], stg[:], [r_stg], [r_QT[s]])
                    elif kind == "k":
                        cp("dve", stg[:], PS[b][:, :], [PR[b]], [r_stg])
                        stq(KT[s][ci * 128:(ci + 1) * 128, cols], stg[:], [r_stg], [r_KT[s]])
                    elif kind == "dz":
                        act(stg[:], PS[b][:, :], AF.Silu, [PR[b]], [r_stg])
                        stq(DZ[s][ci * 128:(ci + 1) * 128, cols], stg[:], [r_stg], [r_DZ[s]])
                    elif kind in ("gf", "gd"):
                        act(stg[:], PS[b][:, :], AF.Tanh, [PR[b]], [r_stg], scale=0.5)
                        dst, r_d = (THF[s], r_THF[s]) if kind == "gf" else (THD[s], r_THD[s])
                        stq(dst[ci * 128:(ci + 1) * 128, cols], stg[:], [r_stg], [r_d])
                    else:
                        zb, r_zb = f32_rot.next()
                        if g == 0:
                            ms("dve", zb[:, 0:3], 0.0, [r_zb])
                        else:
                            cp("dve", zb[:, 0:3], halo[:, 512:515], [r_halo], [r_zb])
                        cp("act", zb[:, 3:515], PS[b][:, :], [PR[b]], [r_zb])
                        halo, r_halo = zb, r_zb
                        cv, r_cv = f32_rot.next()
                        ts("dve", cv[:, 0:512], zb[:, 3:515], cw[:, ci, 3:4], None, ALU.mult, None, [r_zb, r_cw], [r_cv])
                        for j in range(3):
                            stt(cv[:, 0:512], zb[:, j:j + 512], cw[:, ci, j:j + 1], cv[:, 0:512], ALU.mult, ALU.add,
                                [r_zb, r_cw, r_cv], [r_cv])
                        if ci >= 8:
                            act(stg[:], cv[:, 0:512], AF.Silu, [r_cv], [r_stg])
                            stq(GQKV[s][ci * 128:(ci + 1) * 128, cols], stg[:], [r_stg], [r_GQKV[s]])
                        else:
                            act(cv[:, 0:512], cv[:, 0:512], AF.Silu, [r_cv], [r_cv])
                            sq, r_sq = stg_rot.next()
                            act(sq[:], cv[:, 0:512], AF.Square, [r_cv], [r_sq])
                            pss = p2cnt[0] % 2
                            p2cnt[0] += 1
                            for j in range(4):
                                mm(PS[pss][:, j:j + 1], sq[:, j * 128:(j + 1) * 128], onesB[:, 0:1], True, True,
                                   [r_onesB, r_sq], [PR[pss]], sig=(j == 3))

                            def g2(cv=cv, r_cv=r_cv, stg=stg, r_stg=r_stg, pss=pss, ci=ci, cols=cols):
                                rw, r_rw = f32_rot.next()
                                ts("dve", rw[:, 0:4], PS[pss][:, 0:4], EPS, None, ALU.add, None, [PR[pss]], [r_rw])
                                tt("pool", rw[:, 4:8], rw[:, 0:4], nhalf[:, 0:4], ALU.pow, [r_rw, r_nhalf], [r_rw])
                                for j in range(4):
                                    ts("dve", rw[:, 8 + j * 128:8 + (j + 1) * 128], identF[:], rw[:, 4 + j:5 + j], None, ALU.mult, None,
                                       [r_identF, r_rw], [r_rw])
                                for j in range(4):
                                    mm(PS[6][:, j * 128:(j + 1) * 128], onesF[:], rw[:, 8 + j * 128:8 + (j + 1) * 128], True, True,
                                       [r_onesF, r_rw], [PR[6]], sig=(j == 3))
                                stt(stg[:], cv[:, 0:512], scale_gq if ci < 4 else 1.0, PS[6][:, :], ALU.mult, ALU.mult,
                                    [r_cv, PR[6]], [r_stg])
                                stq(GQKV[s][ci * 128:(ci + 1) * 128, cols], stg[:], [r_stg], [r_GQKV[s]])
                            if p2pend[0] is not None:
                                p2pend[0]()
                            p2pend[0] = g2
            if p2pend[0] is not None:
                p2pend[0]()
                p2pend[0] = None
            if stages < 3:
                continue
            barrier()
            arena_off[0] = MARK2
            augb_rot = Rot("augb", [128, 6, 1024], BF16, 1)
            augf_rot = Rot("augf", [128, 2, 1024], F32, 1)
            def softplus_rows(Z, r_Z, prm, r_prm, lo, n, escale):
                rows = slice(lo, lo + n)
                act(Z[rows, :], Z[rows, :], AF.Exp, [r_Z, r_prm], [r_Z], bias=prm[rows, 0:1], scale=escale)
                act(Z[rows, :], Z[rows, :], AF.Ln, [r_Z], [r_Z], bias=1.0)
                ts("dve", Z[rows, :], Z[rows, :], prm[rows, 1:2], None, ALU.mult, None, [r_Z, r_prm], [r_Z])
            softplus_rows(ZA, r_ZA, prmA, r_prmA, 0, 8, -1.0)
            for o in (32, 64, 96):
                softplus_rows(ZA, r_ZA, prmA, r_prmA, o, 4, 1.0)
            softplus_rows(ZB, r_ZB, prmB, r_prmB, 0, 4, -1.0)
            softplus_rows(ZB, r_ZB, prmB, r_prmB, 32, 4, -1.0)
            softplus_rows(ZB, r_ZB, prmB, r_prmB, 64, 4, 1.0)

            def scan(Z, r_Z, rows, c0, n, init):
                kb.op("dve", lambda e: e.tensor_tensor_scan(out=Z[rows, c0:c0 + n], data0=onesS[rows, 0:n],
                                                            data1=Z[rows, c0:c0 + n], initial=init,
                                                            op0=ALU.mult, op1=ALU.add),
                      reads=[r_Z, r_onesS], writes=[r_Z])
            AB = min(1024, S)
            for blk in range(S // AB):
                scan(ZA, r_ZA, slice(0, 8), blk * AB, AB, 0.0 if blk == 0 else ZA[0:8, blk * AB - 1:blk * AB])
            for blk in range(S // AB):
                cs = slice(blk * AB, (blk + 1) * AB)
                ab, r_ab = augb_rot.next()
                af, r_af = augf_rot.next()
                cp("dve", ab[0:8, 0, 0:AB], ZA[0:8, cs], [r_ZA], [r_ab])
                tt("dve", af[0:8, 0, 0:AB], ZA[0:8, cs], ab[0:8, 0, 0:AB], ALU.subtract, [r_ZA, r_ab], [r_af])
                cp("dve", ab[0:8, 1, 0:AB], af[0:8, 0, 0:AB], [r_af], [r_ab])
                tt("dve", af[0:8, 1, 0:AB], af[0:8, 0, 0:AB], ab[0:8, 1, 0:AB], ALU.subtract, [r_af, r_ab], [r_af])
                cp("dve", ab[0:8, 2, 0:AB], af[0:8, 1, 0:AB], [r_af], [r_ab])
                for j in range(3):
                    ts("dve", ab[0:8, 3 + j, 0:AB], ab[0:8, j, 0:AB], -1.0, None, ALU.mult, None, [r_ab], [r_ab])
                for j in range(3):
                    stq(AUGQ[s][:, j, cs], ab[0:8, j, 0:AB], [r_ab], [r_aug[s]])
                    stq(AUGQ[s][:, 3 + j, cs], onesS[0:8, 0:AB], [r_onesS], [r_aug[s]])
                    stq(AUGK[s][:, j, cs], onesS[0:8, 0:AB], [r_onesS], [r_aug[s]])
                    stq(AUGK[s][:, 3 + j, cs], ab[0:8, 3 + j, 0:AB], [r_ab], [r_aug[s]])
            for n in range(NT):
                c0 = n * 128
                for o in (32, 64, 96):
                    scan(ZA, r_ZA, slice(o, o + 4), c0, 128, 0.0)
                scan(ZB, r_ZB, slice(64, 68), c0, 128, 0.0)
                for o in (64, 96):
                    cp("dve", tots[o:o + 4, n:n + 1], ZA[o:o + 4, c0 + 127:c0 + 128], [r_ZA], [r_tots])
                ts("dve", ZA[64:68, c0:c0 + 128], ZA[64:68, c0:c0 + 128], -1.0, tots[64:68, n:n + 1], ALU.mult, ALU.add,
                   [r_ZA, r_tots], [r_ZA])
                ts("dve", ZA[96:100, c0:c0 + 128], ZA[96:100, c0:c0 + 128], 0.0, tots[96:100, n:n + 1], ALU.mult, ALU.add,
                   [r_ZA, r_tots], [r_ZA])
            act(ZA[64:68, :], ZA[64:68, :], AF.Exp, [r_ZA], [r_ZA])
            act(ZA[96:100, :], ZA[96:100, :], AF.Exp, [r_ZA], [r_ZA])
            act(ZB[0:4, :], ZB[0:4, :], AF.Exp, [r_ZB], [r_ZB])
            tt("dve", ZB[32:36, :], ZB[32:36, :], ZA[32:36, :], ALU.add, [r_ZB, r_ZA], [r_ZB])
            act(ZB[32:36, :], ZB[32:36, :], AF.Exp, [r_ZB], [r_ZB])
            ts("dve", ZB[64:68, :], ZB[64:68, :], -1.0, None, ALU.mult, None, [r_ZB], [r_ZB])
            if stages < 4:
                continue
            barrier()
            arena_off[0] = MARK2
            qa_rot = Rot("qa", [128, S], BF16, 2)
            ka_rot = Rot("ka", [128, S], BF16, 2)
            vh_rot = Rot("vh", [128, NT, 128], BF16, 2)
            for i_ in range(2):
                ms("pool", qa_rot.t[i_][64:128, :], 0.0, [qa_rot.r[i_]])
                ms("pool", ka_rot.t[i_][64:128, :], 0.0, [ka_rot.r[i_]])
                ms("pool", vh_rot.t[i_][:], 0.0, [vh_rot.r[i_]])
            pt_rot = Rot("pt", [128, 512], BF16, 3)
            f32_rot = Rot("f32w", [128, 520], F32, 4)
            stg_rot = Rot("stg", [128, 512], BF16, 4)
            for h in range(8):
                QA, r_QA = qa_rot.next()
                KA, r_KA = ka_rot.next()
                ld(QA[0:64, 0:S], QT[s][h * 64:(h + 1) * 64, :], [r_QA], [r_QT[s]])
                ld(QA[64:70, 0:S], AUGQ[s][h], [r_QA], [r_aug[s]])
                ld(KA[0:64, 0:S], KT[s][h * 64:(h + 1) * 64, :], [r_KA], [r_KT[s]])
                ld(KA[64:70, 0:S], AUGK[s][h], [r_KA], [r_aug[s]])
                Vh, r_Vh = vh_rot.next()
                cp("pool", Vh[:, :, 0:65], Vall[:, :, h, :], r_V, [r_Vh])
                for g in range(NG):
                    last = 4 * g + 3
                    po = 2 + 2 * (g % 2)
                    pbc = po + 1

                    def scores(j):
                        m = j - 4 * g
                        c0 = max(m, 0) * 128
                        N = 512 - c0
                        b = j % 2
                        mm(PS[b][:, 0:N], KA[0:FOX_K, j * 128:(j + 1) * 128], QA[0:FOX_K, g * 512 + c0:(g + 1) * 512],
                           True, m < 0, [r_KA, r_QA], [PR[b]], sig=(m < 0))
                        if m >= 0:
                            mm(PS[b][:, 0:128], identB[:], masku[:], False, True, [r_identB, r_masku], [PR[b]])
                    scores(0)
                    for j in range(last + 1):
                        m = j - 4 * g
                        c0 = max(m, 0) * 128
                        N = 512 - c0
                        b = j % 2
                        if j + 1 <= last:
                            scores(j + 1)
                        pt, r_pt = pt_rot.next()
                        act(pt[:, 0:N], PS[b][:, 0:N], AF.Exp, [PR[b]], [r_pt])
                        mm(PS[po][0:FOX_M, c0:512], Vh[:, j, 0:FOX_M], pt[:, 0:N], j == 0, j == last,
                           [r_Vh, r_pt], [PR[po]])
                        if FOX_FILL:
                            kb.op("pe", lambda e: e.matmul(PS[6][:, 0:FOX_FILL_N], lhsT=identB[:], rhs=onesS[:, 0:FOX_FILL_N], start=True, stop=True),
                                  sig=False)
                    rr, r_rr = f32_rot.next()
                    kb.op("dve", lambda e, rr=rr, po=po: e.reciprocal(rr[64:65, 0:512], PS[po][64:65, :]), reads=[PR[po]], writes=[r_rr])
                    mm(PS[pbc][0:64, :], onesF[64:65, 0:64], rr[64:65, 0:512], True, True, [r_onesF, r_rr], [PR[pbc]])
                    ob, r_ob = f32_rot.next()
                    cp("act", ob[0:64, 0:512], PS[po][0:64, :], [PR[po]], [r_ob])
                    stg, r_stg = stg_rot.next()
                    tt("dve", stg[0:64, :], ob[0:64, 0:512], PS[pbc][0:64, :], ALU.mult, [r_ob, PR[pbc]], [r_stg])
                    stq(ATs[s][h * 64:(h + 1) * 64, g * 512:(g + 1) * 512], stg[0:64, :], [r_stg], [r_AT[s]])
            if stages < 5:
                continue
            barrier()
            arena_off[0] = MARK1
            qkv_rot = Rot("qkv", [128, 12, 128], BF16, 2)
            dz_rot = Rot("dzr", [128, 4, 128], BF16, 2)
            tok_rot = Rot("tok", [128, 128], F32, 4)
            e512_rot = Rot("e512", [128, 512], F32, 12)
            wl_rot = Rot("wl", [128, 512], BF16, 18)
            ws_rot = Rot("ws", [128, 512], F32, 8)
            f32_rot = Rot("f32w", [128, 520], F32, 2)
            stg_rot = Rot("stg", [128, 512], BF16, 3)
            Sst = sb("Sst", [128, 512], F32); r_Sst = R()
            Sbf = sb("Sbf", [128, 512], BF16); r_Sbf = R()
            ms("dve", Sst[:], 0.0, [r_Sst])
            ms("dve", Sbf[:], 0.0, [r_Sbf])
            gst_ = {}

            def g_pre(n):
                c0 = n * 128
                ccols = slice(c0, c0 + 128)
                qkvT, r_qkv = qkv_rot.next()
                ld(qkvT[:], GQKV[s][:, ccols].rearrange("(c p) t -> p c t", p=128), [r_qkv], [r_GQKV[s]])
                dzT, r_dz = dz_rot.next()
                ld(dzT[:], DZ[s][:, ccols].rearrange("(c p) t -> p c t", p=128), [r_dz], [r_DZ[s]])
                tokA, r_tokA = tok_rot.next()
                tokB, r_tokB = tok_rot.next()
                tr(PS[3][:, 0:128], ZA[:, ccols], identF[:], [r_ZA, r_identF], [PR[3]])
                cp("act", tokA[:], PS[3][:, 0:128], [PR[3]], [r_tokA])
                tr(PS[4][:, 0:128], ZB[:, ccols], identF[:], [r_ZB, r_identF], [PR[4]])
                cp("dve", tokB[:], PS[4][:, 0:128], [PR[4]], [r_tokB])
                HS = [slice(h * 128, (h + 1) * 128) for h in range(4)]
                for h in range(4):
                    kTh = qkvT[:, 4 + h, :]
                    mm(PS[0][:, HS[h]], kTh, kTh, True, True, [r_qkv], [PR[0]], sig=(h == 3))
                for h in range(4):
                    mm(PS[1][:, HS[h]], qkvT[:, 4 + h, :], qkvT[:, h, :], True, True, [r_qkv], [PR[1]], sig=(h == 3))
                for h in range(4):
                    mm(PS[2][:, HS[h]], selA[:, HS[h]], ZA[:, ccols], True, False, [r_selA, r_ZA], [PR[2]], sig=False)
                    mm(PS[2][:, HS[h]], identB[:], masku[:], False, True, [r_identB, r_masku], [PR[2]], sig=(h == 3))
                for h in range(4):
                    mm(PS[3][:, HS[h]], selA[:, HS[h]], ZA[:, ccols], True, False, [r_selA, r_ZA], [PR[3]], sig=False)
                    mm(PS[3][:, HS[h]], identB[:], maskl[:], False, True, [r_identB, r_maskl], [PR[3]], sig=(h == 3))
                for h in range(4):
                    mm(PS[4][:, HS[h]], selA[:, HS[h]], ZA[:, ccols], True, True, [r_selA, r_ZA], [PR[4]], sig=(h == 3))
                Eu, r_Eu = e512_rot.next()
                El, r_El = e512_rot.next()
                Ep, r_Ep = e512_rot.next()
                for h in range(4):
                    act(Eu[:, HS[h]], PS[2][:, HS[h]], AF.Exp, [PR[2], r_tokB], [r_Eu], bias=tokB[:, 64 + h:65 + h])
                    act(El[:, HS[h]], PS[3][:, HS[h]], AF.Exp, [PR[3], r_tokA], [r_El], bias=tokA[:, 32 + h:33 + h], scale=-1.0)
                act(Ep[:], PS[4][:, :], AF.Exp, [PR[4]], [r_Ep])
                ATt, r_ATt = wl_rot.next()
                tt("dve", ATt[:], PS[1][:, :], Eu[:], ALU.mult, [PR[1], r_Eu], [r_ATt])
                Lt, r_Lt = ws_rot.next()
                for h in range(4):
                    stt(Lt[:, HS[h]], PS[0][:, HS[h]], tokB[:, h:h + 1], El[:, HS[h]], ALU.mult, ALU.mult,
                        [PR[0], r_tokB, r_El], [r_Lt])
                qdec, r_qdec = wl_rot.next()
                tt("dve", qdec[:], qkvT[:, 0:4, :].rearrange("p c t -> p (c t)"), Ep[:], ALU.mult, [r_qkv, r_Ep], [r_qdec])
                for h in range(4):
                    tr(PB[:, HS[h]], qkvT[:, 4 + h, :], identB[:], [r_qkv, r_identB], [PBR])
                    tr(PB[:, 512 + h * 128:512 + (h + 1) * 128], qkvT[:, 8 + h, :], identB[:], [r_qkv, r_identB], [PBR])
                kbg, r_kbg = wl_rot.next()
                kdec, r_kdec = wl_rot.next()
                vb, r_vb = wl_rot.next()
                for h in range(4):
                    ts("dve", kbg[:, HS[h]], PB[:, HS[h]], tokB[:, 32 + h:33 + h], None, ALU.mult, None, [PBR, r_tokB], [r_kbg])
                    act(kdec[:, HS[h]], PB[:, HS[h]], AF.Copy, [PBR, r_tokA], [r_kdec], scale=tokA[:, 64 + h:65 + h])
                    ts("dve", vb[:, HS[h]], PB[:, 512 + h * 128:512 + (h + 1) * 128], tokB[:, h:h + 1], None, ALU.mult, None,
                       [PBR, r_tokB], [r_vb])
                for h in range(4):
                    tr(PS[5][:, HS[h]], Lt[:, HS[h]], identF[:], [r_Lt, r_identF], [PR[5]])
                Mt, r_Mt = ws_rot.next()
                cp("act", Mt[:], PS[5][:, :], [PR[5]], [r_Mt])
                Rt, r_Rt = ws_rot.next()
                tt("dve", Rt[:], ident4F[:], PS[5][:, :], ALU.subtract, [r_ident4F, PR[5]], [r_Rt])
                Pc, r_Pc, Qc, r_Qc = Lt, r_Lt, Mt, r_Mt
                for lvl in range(6):
                    for h in range(4):
                        mm(PS[0][:, HS[h]], Qc[:, HS[h]], Pc[:, HS[h]], True, True, [r_Qc, r_Pc], [PR[0]], sig=(h == 3))
                    if lvl < 5:
                        for h in range(4):
                            mm(PS[1][:, HS[h]], Pc[:, HS[h]], Qc[:, HS[h]], True, True, [r_Qc, r_Pc], [PR[1]], sig=(h == 3))
                    Pn, r_Pn = ws_rot.next()
                    cp("act", Pn[:], PS[0][:, :], [PR[0]], [r_Pn])
                    if lvl < 5:
                        Qn, r_Qn = ws_rot.next()
                        cp("dve", Qn[:], PS[1][:, :], [PR[1]], [r_Qn])
                    for h in range(4):
                        mm(PS[2][:, HS[h]], Pn[:, HS[h]], Rt[:, HS[h]], True, True, [r_Pn, r_Rt], [PR[2]], sig=(h == 3))
                    Rn, r_Rn = ws_rot.next()
                    tt("dve", Rn[:], Rt[:], PS[2][:, :], ALU.add, [r_Rt, PR[2]], [r_Rn])
                    Rt, r_Rt = Rn, r_Rn
                    Pc, r_Pc = Pn, r_Pn
                    if lvl < 5:
                        Qc, r_Qc = Qn, r_Qn
                Rf, r_Rf = Rt, r_Rt
                Rt, r_Rt = wl_rot.next()
                cp("act", Rt[:], Rf[:], [r_Rf], [r_Rt])
                for h in range(4):
                    mm(PS[3][:, HS[h]], kbg[:, HS[h]], Rt[:, HS[h]], True, True, [r_kbg, r_Rt], [PR[3]], sig=(h == 3))
                for h in range(4):
                    mm(PS[4][:, HS[h]], Rt[:, HS[h]], vb[:, HS[h]], True, True, [r_vb, r_Rt], [PR[4]], sig=(h == 3))
                wT, r_wT = wl_rot.next()
                cp("act", wT[:], PS[3][:, :], [PR[3]], [r_wT])
                uu, r_uu = e512_rot.next()
                cp("dve", uu[:], PS[4][:, :], [PR[4]], [r_uu])
                gst_[n] = dict(ccols=ccols, HS=HS, tokA=tokA, r_tokA=r_tokA, dzT=dzT, r_dz=r_dz, ATt=ATt, r_ATt=r_ATt,
                               qdec=qdec, r_qdec=r_qdec, kdec=kdec, r_kdec=r_kdec, wT=wT, r_wT=r_wT, uu=uu, r_uu=r_uu)

            def g_scan(n):
                d_ = gst_.pop(n)
                ccols, HS, tokA, r_tokA, dzT, r_dz = d_['ccols'], d_['HS'], d_['tokA'], d_['r_tokA'], d_['dzT'], d_['r_dz']
                ATt, r_ATt, qdec, r_qdec, kdec, r_kdec = d_['ATt'], d_['r_ATt'], d_['qdec'], d_['r_qdec'], d_['kdec'], d_['r_kdec']
                wT, r_wT, uu, r_uu = d_['wT'], d_['r_wT'], d_['uu'], d_['r_uu']
                for h in range(4):
                    mm(PS[5][:, HS[h]], wT[:, HS[h]], Sbf[:, HS[h]], True, True, [r_wT, r_Sbf], [PR[5]], sig=(h == 3))
                vnew, r_vnew = wl_rot.next()
                tt("dve", vnew[:], uu[:], PS[5][:, :], ALU.subtract, [r_uu, PR[5]], [r_vnew])
                for h in range(4):
                    mm(PS[6][:, HS[h]], Sbf[:, HS[h]], qdec[:, HS[h]], True, False, [r_Sbf, r_qdec], [PR[6]], sig=False)
                    mm(PS[6][:, HS[h]], vnew[:, HS[h]], ATt[:, HS[h]], False, True, [r_vnew, r_ATt], [PR[6]], sig=(h == 3))
                for h in range(4):
                    mm(PS[0][:, HS[h]], kdec[:, HS[h]], vnew[:, HS[h]], True, True, [r_kdec, r_vnew], [PR[0]], sig=(h == 3))
                for h in range(4):
                    stt(Sst[:, HS[h]], Sst[:, HS[h]], tokA[:, 96 + h:97 + h], PS[0][:, HS[h]], ALU.mult, ALU.add,
                        [r_Sst, r_tokA, PR[0]], [r_Sst])
                cp("act", Sbf[:], Sst[:], [r_Sst], [r_Sbf])
                sq, r_sq = wl_rot.next()
                act(sq[:], PS[6][:, :], AF.Square, [PR[6]], [r_sq])
                for h in range(4):
                    mm(PS[1][:, h:h + 1], sq[:, HS[h]], onesB[:, 0:1], True, True, [r_onesB, r_sq], [PR[1]], sig=(h == 3))
                rw, r_rw = f32_rot.next()
                ts("dve", rw[:, 0:4], PS[1][:, 0:4], 1.0 / 128, EPS, ALU.mult, ALU.add, [PR[1]], [r_rw])
                tt("pool", rw[:, 4:8], rw[:, 0:4], nhalf[:, 0:4], ALU.pow, [r_rw, r_nhalf], [r_rw])
                Dg, r_Dg = e512_rot.next()
                for h in range(4):
                    ts("dve", Dg[:, HS[h]], identF[:], rw[:, 4 + h:5 + h], None, ALU.mult, None, [r_identF, r_rw], [r_Dg])
                for h in range(4):
                    mm(PS[2][:, HS[h]], onesF[:], Dg[:, HS[h]], True, True, [r_onesF, r_Dg], [PR[2]], sig=(h == 3))
                rnb, r_rnb = e512_rot.next()
                cp("act", rnb[:], PS[2][:, :], [PR[2]], [r_rnb])
                o1, r_o1 = e512_rot.next()
                stt(o1[:], PS[6][:, :], gonT[:, 0:1], rnb[:], ALU.mult, ALU.mult, [PR[6], r_gonT, r_rnb], [r_o1])
                stg, r_stg = stg_rot.next()
                tt("dve", stg[:], o1[:], dzT[:].rearrange("p c t -> p (c t)"), ALU.mult, [r_o1, r_dz], [r_stg])
                stq(OGs[s][:, ccols].rearrange("(h p) c -> p h c", p=128), stg[:].rearrange("p (h c) -> p h c", h=4),
                    [r_stg], [r_OG[s]])
            for t in range(NT + 1):
                if t < NT:
                    g_pre(t)
                if t >= 1:
                    g_scan(t - 1)
            if stages < 6:
                continue
            barrier()
            arena_off[0] = MARK0
            NSLT = (2 * S) // 128 + 32
            Tt = sb("Tt", [128, NT, D], BF16); r_Tt = [R() for _ in range(NT)]
            A12 = sb("A12", [128, NT, 64], F32); r_A12 = [R() for _ in range(NT)]
            RTW = sb("RTW", [128, NT, 2], F32); r_RTW = [R() for _ in range(NT)]
            RK = sb("RK", [128, NT, 32], F32); r_RK = [R() for _ in range(NT)]
            SLf = sb("SLf", [128, NT, 2], F32); r_SLf = R()
            SLi = sb("SLi", [128, NT, 2], mybir.dt.int32); r_SLi = R()
            WIf = sb("WIf", [128, NSLT], F32); r_WIf = R()
            WIi = sb("WIi", [128, NSLT], mybir.dt.int32); r_WIi = R()
            cnt = sb("cnt", [128, 5, 32], F32); r_cnt = R()
            gffn_b = sb("gffn_b", [128, D], F32); r_gffn_b = R()
            ld(gffn_b[:], g_ffn[None, :].to_broadcast([128, D]), [r_gffn_b])
            MARKT = arena_off[0]
            wout_sb, r_wout = wres("wout", w_out, 8, D)
            wofox_sb, r_wofox = wres("wofox", w_o_fox, 4, D)
            wodel_sb, r_wodel = wres("wodel", w_o_delta, 4, D)
            xg = sb("xg", [128, 4, D], F32); r_xg = [R() for _ in range(4)]
            mrg = sb("mrg", [128, 8, 512], BF16); r_mrg = R()
            at_rot = Rot("atr", [128, 4, 512], BF16, 2)
            th_rot = Rot("thr", [128, 8, 512], BF16, 2)
            e512_rot = Rot("e512", [128, 512], F32, 4)
            rt_rot = Rot("rtr", [128, 128], F32, 2)
            tTt_rot = Rot("tTt", [128, 8, 128], BF16, 2)
            zt = sb("zt", [128, D], BF16); r_zt = R()
            ms("dve", zt[:], 0.0, [r_zt])
            for i in range(NSLT):
                stq(Xg[s][i * 128:(i + 1) * 128, :], zt[:], [r_zt], [r_Xgz[s]])
            for g in range(NG):
                cols = slice(g * 512, (g + 1) * 512)
                atT, r_atT = at_rot.next()
                ogT, r_ogT = at_rot.next()
                ld(atT[:], ATs[s][:, cols].rearrange("(k p) t -> p k t", p=128), [r_atT], [r_AT[s]])
                ld(ogT[:], OGs[s][:, cols].rearrange("(k p) t -> p k t", p=128), [r_ogT], [r_OG[s]])
                thf, r_thf = th_rot.next()
                thd, r_thd = th_rot.next()
                ld(thf[:], THF[s][:, cols].rearrange("(k p) t -> p k t", p=128), [r_thf], [r_THF[s]])
                ld(thd[:], THD[s][:, cols].rearrange("(k p) t -> p k t", p=128), [r_thd], [r_THD[s]])
                for j in range(4):
                    r0 = tok0 + g * 512 + j * 128
                    ld(xg[:, j, :], x_d[r0:r0 + 128, :], [r_xg[j]])
                for m in range(8):
                    ms_ = slice(m * 128, (m + 1) * 128)
                    for k in range(4):
                        mm(PS[0][:, :], wofox_sb[:, k, ms_], atT[:, k, :], k == 0, k == 3, [r_wofox, r_atT], [PR[0]])
                    for k in range(4):
                        mm(PS[1][:, :], wodel_sb[:, k, ms_], ogT[:, k, :], k == 0, k == 3, [r_wodel, r_ogT], [PR[1]])
                    m1, r_m1 = e512_rot.next()
                    m2, r_m2 = e512_rot.next()
                    stt(m1[:], thf[:, m, :], 1.0, PS[0][:, :], ALU.add, ALU.mult, [r_thf, PR[0]], [r_m1])
                    stt(m2[:], thd[:, m, :], 1.0, PS[1][:, :], ALU.add, ALU.mult, [r_thd, PR[1]], [r_m2])
                    tt("pool", mrg[:, m, :], m1[:], m2[:], ALU.add, [r_m1, r_m2], [r_mrg])
                for j in range(4):
                    for hf in range(2):
                        b = 2 + hf
                        for k in range(8):
                            mm(PS[b][:, :], mrg[:, k, j * 128:(j + 1) * 128], wout_sb[:, k, hf * 512:(hf + 1) * 512],
                               k == 0, k == 7, [r_mrg, r_wout], [PR[b]])
                        stt(xg[:, j, hf * 512:(hf + 1) * 512], PS[b][:, :], 0.5, xg[:, j, hf * 512:(hf + 1) * 512],
                            ALU.mult, ALU.add, [PR[b], r_xg[j]], [r_xg[j]])
                for j in range(4):
                    n = 4 * g + j
                    stq(X1[s][n * 128:(n + 1) * 128, :], xg[:, j, :], [r_xg[j]], [r_X1[s]])
                    s1, r_s1 = norm_stats(xg[:, j, :], r_xg[j])
                    stt(Tt[:, n, :], xg[:, j, :], s1[:, 1:2], gffn_b[:], ALU.mult, ALU.mult, [r_xg[j], r_s1, r_gffn_b], [r_Tt[n]])
                    for c in range(8):
                        tr(PB[:, c * 128:(c + 1) * 128], Tt[:, n, c * 128:(c + 1) * 128], identB[:], [r_Tt[n], r_identB], [PBR])
                    tTt, r_tTt = tTt_rot.next()
                    cp("act", tTt[:].rearrange("p c t -> p (c t)"), PB[:, :], [PBR], [r_tTt])
                    for k in range(8):
                        mm(PS[4][:, 0:36], tTt[:, k, :], wrt_sb[:, k, :], k == 0, k == 7, [r_tTt, r_wrt], [PR[4]])
                    rt, r_rt = rt_rot.next()
                    lg = rt[:, 0:36]
                    tt("dve", lg, PS[4][:, 0:36], brt[:], ALU.add, [PR[4], r_brt], [r_rt])
                    gmax = rt[:, 40:41]; ngm = rt[:, 41:42]; sg = rt[:, 42:43]; psel = rt[:, 43:44]
                    oh = rt[:, 44:48]; el = rt[:, 48:56]; m8 = rt[:, 56:64]; msk = rt[:, 64:72]; a1 = rt[:, 72:80]
                    nm1 = rt[:, 80:81]; e21 = rt[:, 81:82]; cc = rt[:, 82:83]; eg = rt[:, 84:88]; a2 = rt[:, 88:96]
                    rr_ = [r_rt]
                    kb.op("dve", lambda e, lg=lg, gmax=gmax: e.tensor_reduce(out=gmax, in_=lg[:, 0:4], axis=mybir.AxisListType.X, op=ALU.max),
                          reads=rr_, writes=rr_)
                    ts("dve", oh, lg[:, 0:4], gmax, None, ALU.is_ge, None, rr_, rr_)
                    ts("dve", ngm, gmax, -1.0, None, ALU.mult, None, rr_, rr_)
                    ms("dve", sg, 0.0, rr_)
                    act(eg, lg[:, 0:4], AF.Exp, rr_, rr_, bias=ngm, accum=sg)
                    kb.op("dve", lambda e, psel=psel, sg=sg: e.reciprocal(psel, sg), reads=rr_, writes=rr_)
                    ts("dve", el, lg[:, 4:12], oh[:, 0:1], None, ALU.mult, None, rr_, rr_)
                    for gg in range(1, 4):
                        stt(el, lg[:, 4 + 8 * gg:12 + 8 * gg], oh[:, gg:gg + 1], el, ALU.mult, ALU.add, rr_, rr_)
                    kb.op("dve", lambda e, m8=m8, el=el: e.max(out=m8, in_=el), reads=rr_, writes=rr_)
                    ts("dve", msk, el, m8[:, 1:2], None, ALU.is_ge, None, rr_, rr_)
                    ts("dve", a1, el, m8[:, 0:1], None, ALU.is_ge, None, rr_, rr_)
                    tt("dve", a2, msk, a1, ALU.subtract, rr_, rr_)
                    ts("dve", nm1, m8[:, 0:1], -1.0, None, ALU.mult, None, rr_, rr_)
                    act(e21, m8[:, 1:2], AF.Exp, rr_, rr_, bias=nm1)
                    ts("dve", cc, e21, 1.0, None, ALU.add, None, rr_, rr_)
                    kb.op("dve", lambda e, cc=cc: e.reciprocal(cc, cc), reads=rr_, writes=rr_)
                    tt("dve", RTW[:, n, 0:1], cc, psel, ALU.mult, rr_, [r_RTW[n]])
                    tt("dve", RTW[:, n, 1:2], RTW[:, n, 0:1], e21, ALU.mult, rr_ + [r_RTW[n]], [r_RTW[n]])
                    for gg in range(4):
                        ts("dve", A12[:, n, 8 * gg:8 * gg + 8], a1, oh[:, gg:gg + 1], None, ALU.mult, None, rr_, [r_A12[n]])
                        ts("dve", A12[:, n, 32 + 8 * gg:40 + 8 * gg], a2, oh[:, gg:gg + 1], None, ALU.mult, None, rr_, [r_A12[n]])
            barrier()
            arena_off[0] = MARKT
            asum_rot = Rot("asum", [128, 32], BF16, 3)
            tmp_rot = Rot("tmpr", [128, 64], F32, 3)
            ms("dve", cnt[:, 0, :], 0.0, [r_cnt])
            for n in range(NT):
                asum, r_asum = asum_rot.next()
                tt("dve", asum[:], A12[:, n, 0:32], A12[:, n, 32:64], ALU.add, [r_A12[n]], [r_asum])
                mm(PS[0][:, 0:32], ustrict[:], asum[:], True, True, [r_ustrict, r_asum], [PR[0]])
                mm(PS[1][:, 0:32], onesB[:], asum[:], True, True, [r_onesB, r_asum], [PR[1]])
                tt("dve", RK[:, n, :], PS[0][:, 0:32], cnt[:, 0, :], ALU.add, [PR[0], r_cnt], [r_RK[n]])
                tt("dve", cnt[:, 0, :], cnt[:, 0, :], PS[1][:, 0:32], ALU.add, [PR[1], r_cnt], [r_cnt])
            for e_ in range(32):
                tmp, r_tmp = tmp_rot.next()
                ts("dve", tmp[:, 0:64], tstart[:, 0:64], cnt[:, 0, e_:e_ + 1], None, ALU.is_lt, None, [r_tstart, r_cnt], [r_tmp])
                kb.op("dve", lambda e, tmp=tmp, e_=e_, cnt=cnt: e.tensor_reduce(out=cnt[:, 2, e_:e_ + 1], in_=tmp[:, 0:64],
                                                                        axis=mybir.AxisListType.X, op=ALU.add),
                      reads=[r_tmp], writes=[r_cnt])
            ts("dve", cnt[:, 2, :], cnt[:, 2, :], 128.0, None, ALU.mult, None, [r_cnt], [r_cnt])
            kb.op("dve", lambda e, o_=cnt[:, 3, :], d_=cnt[:, 2, :]: e.tensor_tensor_scan(out=o_, data0=onesS[:, 0:32], data1=d_, initial=0.0,
                                                                             op0=ALU.mult, op1=ALU.add), reads=[r_cnt, r_onesS], writes=[r_cnt])
            tt("dve", cnt[:, 4, :], cnt[:, 3, :], cnt[:, 2, :], ALU.subtract, [r_cnt], [r_cnt])
            ms("dve", WIf[:], 0.0, [r_WIf])
            for e_ in range(32):
                stt(WIf[:], tstart[:, 0:NSLT], cnt[:, 3, e_:e_ + 1], WIf[:], ALU.is_ge, ALU.add, [r_tstart, r_cnt, r_WIf], [r_WIf])
            ts("dve", WIf[:], WIf[:], 31.0, None, ALU.min, None, [r_WIf], [r_WIf])
            ts("dve", WIf[:], WIf[:], 128.0, pcol[:, 0:1], ALU.mult, ALU.add, [r_WIf, r_pcol], [r_WIf])
            cp("dve", WIi[:], WIf[:], [r_WIf], [r_WIi])
            for n in range(NT):
                tmp, r_tmp = tmp_rot.next()
                tt("dve", tmp[:, 0:32], RK[:, n, :], cnt[:, 4, :], ALU.add, [r_RK[n], r_cnt], [r_tmp])
                for k in range(2):
                    tt("dve", tmp[:, 32:64], tmp[:, 0:32], A12[:, n, 32 * k:32 * k + 32], ALU.mult, [r_tmp, r_A12[n]], [r_tmp])
                    kb.op("dve", lambda e, tmp=tmp, n=n, k=k, SLf=SLf: e.tensor_reduce(out=SLf[:, n, k:k + 1], in_=tmp[:, 32:64],
                                                                              axis=mybir.AxisListType.X, op=ALU.add),
                          reads=[r_tmp], writes=[r_SLf])
            cp("dve", SLi[:], SLf[:], [r_SLf], [r_SLi])
            for n in range(NT):
                for k in range(2):
                    kb.dma("pool", "sc", lambda e, o_=Xg[s][:, :], i_=SLi[:, n, k:k + 1], t_=Tt[:, n, :]: e.indirect_dma_start(
                        out=o_, out_offset=bass.IndirectOffsetOnAxis(ap=i_, axis=0),
                        in_=t_, in_offset=None), reads=[r_Tt[n], r_SLi, r_Xgz[s]], writes=[r_Xg[s]])
            wx_rot = Rot("wx", [128, 6144], BF16, 4)
            xs_rot = Rot("xs", [128, D], BF16, 3)
            xsT_rot = Rot("xsT", [128, 8, 128], BF16, 3)
            hs_rot = Rot("hs", [128, 256], F32, 2)
            hb_rot = Rot("hb", [128, 256], BF16, 3)
            hT_rot = Rot("hTr", [128, 2, 128], BF16, 2)
            ys_rot = Rot("ys", [128, D], BF16, 2)
            PB2 = PS[6][:, :].bitcast(BF16)
            cst = {}

            def c_load(i):
                wx, r_wx = wx_rot.next()
                kb.dma("pool", "wxg", lambda e, o_=wx[:], i_=WIi[:, i:i + 1], w_=WX[:, :]: e.indirect_dma_start(
                    out=o_, out_offset=None, in_=w_,
                    in_offset=bass.IndirectOffsetOnAxis(ap=i_, axis=0)), reads=[r_WIi, r_wexp], writes=[r_wx])
                xs, r_xs = xs_rot.next()
                ld(xs[:], Xg[s][i * 128:(i + 1) * 128, :], [r_xs], [r_Xg[s]])
                cst[i] = dict(wx=wx, r_wx=r_wx, xs=xs, r_xs=r_xs)

            def c_tr(i):
                d_ = cst[i]
                for c in range(8):
                    tr(PB[:, c * 128:(c + 1) * 128], d_["xs"][:, c * 128:(c + 1) * 128], identB[:], [d_["r_xs"], r_identB], [PBR])
                xsT, r_xsT = xsT_rot.next()
                cp("dve", xsT[:].rearrange("p c t -> p (c t)"), PB[:, :], [PBR], [r_xsT])
                d_["xsT"], d_["r_xsT"] = xsT, r_xsT

            def c_gu(i):
                d_ = cst[i]
                b0 = 3 * (i % 2)
                for k in range(8):
                    mm(PS[b0][:, :], d_["xsT"][:, k, :], d_["wx"][:, k * 512:(k + 1) * 512], k == 0, k == 7,
                       [d_["r_xsT"], d_["r_wx"]], [PR[b0]])
                hsg, r_hsg = hs_rot.next()
                act(hsg[:], PS[b0][:, 0:256], AF.Silu, [PR[b0]], [r_hsg])
                hb, r_hb = hb_rot.next()
                tt("dve", hb[:], hsg[:], PS[b0][:, 256:512], ALU.mult, [r_hsg, PR[b0]], [r_hb])
                d_["hb"], d_["r_hb"] = hb, r_hb

            def c_down(i):
                d_ = cst.pop(i)
                b0 = 3 * (i % 2)
                for c in range(2):
                    tr(PB2[:, c * 128:(c + 1) * 128], d_["hb"][:, c * 128:(c + 1) * 128], identB[:], [d_["r_hb"], r_identB], [PR[6]])
                hTt, r_hTt = hT_rot.next()
                cp("act", hTt[:].rearrange("p c t -> p (c t)"), PB2[:, 0:256], [PR[6]], [r_hTt])
                ys, r_ys = ys_rot.next()
                wx = d_["wx"]
                for hf in range(2):
                    b = b0 + 1 + hf
                    for c in range(2):
                        mm(PS[b][:, :], hTt[:, c, :], wx[:, 4096 + c * 1024 + hf * 512:4096 + c * 1024 + (hf + 1) * 512],
                           c == 0, c == 1, [r_hTt, d_["r_wx"]], [PR[b]])
                    cp("act" if hf else "dve", ys[:, hf * 512:(hf + 1) * 512], PS[b][:, :], [PR[b]], [r_ys])
                stq(Yg[s][i * 128:(i + 1) * 128, :], ys[:], [r_ys], [r_Yg[s]])
            for t in range(NSLT + 3):
                if t < NSLT:
                    c_load(t)
                if 0 <= t - 1 < NSLT:
                    c_tr(t - 1)
                if 0 <= t - 2 < NSLT:
                    c_gu(t - 2)
                if 0 <= t - 3 < NSLT:
                    c_down(t - 3)
            barrier()
            arena_off[0] = MARKT
            wpg_sb, r_wpg = wres("wpg", w_ple_gate, 8, D)
            wpp_sb, r_wpp = wres("wpp", w_ple_proj, 2, D)
            xw_rot = Rot("xw", [128, D], F32, 6)
            yg_rot = Rot("yg", [128, D], BF16, 8)
            e512_rot = Rot("e512", [128, 512], F32, 4)
            nT_rot = Rot("nTr", [128, 8, 128], BF16, 3)
            xn_rot = Rot("xn", [128, D], BF16, 2)
            stg_rot = Rot("stg", [128, 512], BF16, 6)
            f32_rot = Rot("f32w", [128, 520], F32, 4)
            yt_rot = Rot("yt", [128, D], F32, 2)
            dst = {}

            def d_0(n):
                r0 = tok0 + n * 128
                xw, r_xw = xw_rot.next()
                ld(xw[:], X1[s][n * 128:(n + 1) * 128, :], [r_xw], [r_X1[s]])
                pt_, r_pt_ = f32_rot.next()
                ld(pt_[:, 0:256], p_d[r0:r0 + 128, :], [r_pt_])
                ygs = []
                for k in range(2):
                    yg, r_yg = yg_rot.next()
                    kb.dma("pool", "yg", lambda e, o_=yg[:], y_=Yg[s][:, :], i_=SLi[:, n, k:k + 1]: e.indirect_dma_start(
                        out=o_, out_offset=None, in_=y_,
                        in_offset=bass.IndirectOffsetOnAxis(ap=i_, axis=0)), reads=[r_SLi, r_Yg[s]], writes=[r_yg])
                    ygs.append((yg, r_yg))
                dst[n] = dict(xw=xw, r_xw=r_xw, pt_=pt_, r_pt_=r_pt_, ygs=ygs)

            def d_a(n):
                d_ = dst[n]
                xw, r_xw, pt_, r_pt_ = d_["xw"], d_["r_xw"], d_["pt_"], d_["r_pt_"]
                for k in range(2):
                    yg, r_yg = d_["ygs"][k]
                    stt(xw[:], yg[:], RTW[:, n, k:k + 1], xw[:], ALU.mult, ALU.add, [r_yg, r_RTW[n], r_xw], [r_xw])
                pbf, r_pbf = stg_rot.next()
                cp("dve", pbf[:, 0:256], pt_[:, 0:256], [r_pt_], [r_pbf])
                s1, r_s1 = norm_stats(xw[:], r_xw)
                d_.update(pbf=pbf, r_pbf=r_pbf, s1=s1, r_s1=r_s1)

            def d_b(n):
                d_ = dst[n]
                xw, r_xw, s1, r_s1 = d_["xw"], d_["r_xw"], d_["s1"], d_["r_s1"]
                xn, r_xn = xn_rot.next()
                ts("dve", xn[:], xw[:], s1[:, 1:2], None, ALU.mult, None, [r_xw, r_s1], [r_xn])
                for c in range(8):
                    tr(PB[:, c * 128:(c + 1) * 128], xn[:, c * 128:(c + 1) * 128], identB[:], [r_xn, r_identB], [PBR])
                nT, r_nT = nT_rot.next()
                for c in range(8):
                    if c % 2 == 0:
                        act(nT[:, c, :], PB[:, c * 128:(c + 1) * 128], AF.Copy, [PBR, r_gpleT], [r_nT], scale=gpleT[:, c:c + 1])
                    else:
                        ts("dve", nT[:, c, :], PB[:, c * 128:(c + 1) * 128], gpleT[:, c:c + 1], None, ALU.mult, None,
                           [PBR, r_gpleT], [r_nT])
                PB2 = PS[6][:, :].bitcast(BF16)
                for k in range(2):
                    tr(PB2[:, k * 128:(k + 1) * 128], d_["pbf"][:, k * 128:(k + 1) * 128], identB[:], [d_["r_pbf"], r_identB], [PR[6]])
                pT, r_pT = stg_rot.next()
                cp("act", pT[:, 0:256], PB2[:, 0:256], [PR[6]], [r_pT])
                d_.update(nT=nT, r_nT=r_nT, pT=pT, r_pT=r_pT)

            def d_c(n):
                d_ = dst.pop(n)
                r0 = tok0 + n * 128
                xw, r_xw, nT, r_nT, pT, r_pT = d_["xw"], d_["r_xw"], d_["nT"], d_["r_nT"], d_["pT"], d_["r_pT"]
                for hf in range(2):
                    hs_ = slice(hf * 512, (hf + 1) * 512)
                    for k in range(8):
                        mm(PS[0 + 2 * hf][:, :], nT[:, k, :], wpg_sb[:, k, hs_], k == 0, k == 7, [r_nT, r_wpg], [PR[0 + 2 * hf]])
                    for k in range(2):
                        mm(PS[1 + 2 * hf][:, :], pT[:, k * 128:(k + 1) * 128], wpp_sb[:, k, hs_], k == 0, k == 1, [r_pT, r_wpp], [PR[1 + 2 * hf]])
                    th, r_th = e512_rot.next()
                    act(th[:], PS[0 + 2 * hf][:, :], AF.Tanh, [PR[0 + 2 * hf]], [r_th], scale=0.5)
                    ts("dve", th[:], th[:], 0.5, 0.5, ALU.mult, ALU.add, [r_th], [r_th])
                    tt("dve", th[:], th[:], PS[1 + 2 * hf][:, :], ALU.mult, [r_th, PR[1 + 2 * hf]], [r_th])
                    tt("dve", xw[:, hs_], xw[:, hs_], th[:], ALU.add, [r_xw, r_th], [r_xw])
                s1, r_s1 = norm_stats(xw[:], r_xw)
                yt, r_yt = yt_rot.next()
                stt(yt[:], xw[:], s1[:, 1:2], gfin[:], ALU.mult, ALU.mult, [r_xw, r_s1, r_gfin], [r_yt])
                stq(out_d[r0:r0 + 128, :], yt[:], [r_yt], [r_out], stream="sto")
            for t in range(NT + 4):
                if t < NT:
                    d_0(t)
                if 0 <= t - 2 < NT:
                    d_a(t - 2)
                if 0 <= t - 3 < NT:
                    d_b(t - 3)
                if 0 <= t - 4 < NT:
                    d_c(t - 4)
        barrier()
        kb.emit()
    nc.dbg_names = dbg_names
    nc.arena_hw = arena_hw[0]
    return nc


WKEYS = ['g_mix', 'w_in', 'b_forget', 'conv_w', 'a_log', 'dt_bias', 'g_onorm', 'w_o_fox', 'w_o_delta', 'w_out', 'g_ffn',
         'w_group', 'b_group', 'w_router', 'b_router', 'w_gate', 'w_up', 'w_down', 'g_ple', 'w_ple_gate', 'w_ple_proj']
_NC_CACHE = {}


def kernel(**inputs):
    x = np.ascontiguousarray(np.asarray(inputs['x'], dtype=np.float32))
    p = np.ascontiguousarray(np.asarray(inputs['p'], dtype=np.float32))
    B, S, _ = x.shape
    nseq = B // NCORES
    shared = {k: np.ascontiguousarray(np.asarray(inputs[k], dtype=np.float32)[0]) for k in WKEYS}
    shared['g_final'] = np.ascontiguousarray(np.asarray(inputs['g_final'], dtype=np.float32))
    shared.update(host_consts())
    key = (nseq, S)
    if key not in _NC_CACHE:
        _NC_CACHE[key] = build(nseq, S)
    nc = _NC_CACHE[key]
    in_maps = []
    for c in range(NCORES):
        m = dict(shared)
        m['x'] = x[c * nseq:(c + 1) * nseq].reshape(nseq * S, D)
        m['p'] = p[0, c * nseq:(c + 1) * nseq].reshape(nseq * S, 256)
        in_maps.append(m)
    res = run_bass_kernel_spmd(nc, in_maps, core_ids=list(range(NCORES)))
    out = np.concatenate([np.asarray(r['out']).reshape(nseq, S, D) for r in res.results], axis=0)
    return out.astype(np.float32)
```

```python
import numpy as np
from contextlib import ExitStack
import concourse.bass as bass
import concourse.mybir as mybir
from concourse.bass_utils import run_bass_kernel_spmd

F32 = mybir.dt.float32
BF16 = mybir.dt.bfloat16
AF = mybir.ActivationFunctionType
ALU = mybir.AluOpType

NCORES = 8
D = 1024
EPS = 1e-6
NEG = -60000.0
SAME_ENGINE_SYNC = True
FOX_FILL = False
FOX_K = 128
FOX_M = 128
FOX_FILL_N = 256


class Res:
    __slots__ = ("name", "w", "r", "nowaw")

    def __init__(self, name="", nowaw=False):
        self.name = name
        self.nowaw = nowaw
        self.w = {}
        self.r = {}


class KB:
    ENGS = ("pe", "act", "dve", "pool", "sp")
    NDSEM = 20

    def __init__(self, nc, stack):
        self.nc = nc
        self.stack = stack
        self.sem = {}
        self.count = {}
        self.prog = {e: [] for e in self.ENGS}
        self.waited = {e: {} for e in self.ENGS}
        for e in self.ENGS:
            self.sem[e] = stack.enter_context(nc.semaphore("s_" + e))
            self.count[e] = 0
        self.dpool = {}
        self.dnext = {}
        for q in ("sp", "pool"):
            self.dpool[q] = []
            self.dnext[q] = 0
            for i in range(self.NDSEM):
                k = f"d{q}{i}"
                self.sem[k] = stack.enter_context(nc.semaphore(k))
                self.count[k] = 0
                self.dpool[q].append(k)

    def res(self, name=""):
        return Res(name)

    def _need(self, eng, reads, writes, extra=()):
        need = {}

        def add(k, v):
            if need.get(k, 0) < v:
                need[k] = v
        for k, v in extra:
            add(k, v)
        for r in reads:
            for k, v in r.w.items():
                add(k, v)
        for w in writes:
            if not w.nowaw:
                for k, v in w.w.items():
                    add(k, v)
            for k, v in w.r.items():
                add(k, v)
        out = []
        for k, v in need.items():
            if k == eng and (not SAME_ENGINE_SYNC or eng == "pe" or v > self.count[eng]):
                continue
            if self.waited[eng].get(k, 0) >= v:
                continue
            self.waited[eng][k] = v
            out.append((k, v))
        return out

    def _commit(self, ticket, reads, writes):
        k, v = ticket
        for w in writes:
            if w.w.get(k, 0) < v:
                w.w[k] = v
            w.r = {}
        for r in reads:
            if r.r.get(k, 0) < v:
                r.r[k] = v

    def op(self, eng, fn, reads=(), writes=(), sig=True):
        for k, v in self._need(eng, reads, writes):
            self.prog[eng].append(("wait", k, v))
        if sig:
            self.count[eng] += 1
            ticket = (eng, self.count[eng])
            self.prog[eng].append(("op", fn, eng, 1))
        else:
            ticket = (eng, self.count[eng] + 1)
            self.prog[eng].append(("op", fn, None, 0))
        self._commit(ticket, reads, writes)

    def dma(self, queue, stream, fn, reads=(), writes=()):
        pool = self.dpool[queue]
        k = pool[self.dnext[queue] % len(pool)]
        self.dnext[queue] += 1
        extra = [(k, self.count[k])] if self.count[k] > 0 else []
        for kk, v in self._need(queue, reads, writes, extra):
            self.prog[queue].append(("wait", kk, v))
        self.count[k] += 16
        ticket = (k, self.count[k])
        self.prog[queue].append(("op", fn, k, 16))
        self._commit(ticket, reads, writes)

    def emit(self):
        nc = self.nc
        with nc.Block() as block:
            def run(engname, e):
                for item in self.prog[engname]:
                    if item[0] == "wait":
                        e.wait_ge(self.sem[item[1]], item[2])
                    else:
                        _, fn, semk, inc = item
                        ins = fn(e)
                        if semk is not None:
                            ins.then_inc(self.sem[semk], inc)

            @block.tensor
            def _(e):
                run("pe", e)

            @block.scalar
            def _(e):
                run("act", e)

            @block.vector
            def _(e):
                run("dve", e)

            @block.gpsimd
            def _(e):
                run("pool", e)

            @block.sync
            def _(e):
                run("sp", e)


def host_consts():
    c = {}
    c["identF"] = np.eye(128, dtype=np.float32)
    p = np.arange(128)[:, None]
    f = np.arange(128)[None, :]
    c["mask_u"] = np.where(f >= p, 0.0, NEG).astype(np.float32)
    c["mask_l"] = np.where(f >= p, -NEG, 0.0).astype(np.float32)
    sel = np.zeros((128, 4, 128), np.float32)
    for h in range(4):
        sel[32 + h, h, :] = 1.0
    c["selA"] = sel.reshape(128, 512)
    c["tstart"] = np.tile((np.arange(160, dtype=np.float32) * 128.0)[None, :], (128, 1))
    c["pcol"] = np.arange(128, dtype=np.float32)[:, None]
    c["ustrict"] = (p < f).astype(np.float32)
    return c


def build(NSEQ, S, stages=99, debug=False):
    T = NSEQ * S
    NT = S // 128
    NG = S // 512
    nc = bass.Bass("TRN2", target_bir_lowering=False)

    def din(name, shape, dt=F32):
        return nc.dram_tensor(name, list(shape), dt, kind="ExternalInput").ap()

    def dscr(name, shape, dt=BF16):
        return nc.dram_tensor(name, list(shape), dt, kind="ExternalOutput" if debug else "Internal").ap()

    x_d = din("x", [T, D])
    p_d = din("p", [T, 256])
    g_mix = din("g_mix", [D]); w_in = din("w_in", [D, 5648]); b_forget = din("b_forget", [8])
    conv_w = din("conv_w", [4, 1536]); a_log = din("a_log", [4]); dt_bias = din("dt_bias", [4])
    g_onorm = din("g_onorm", [128]); w_o_fox = din("w_o_fox", [512, D]); w_o_delta = din("w_o_delta", [512, D])
    w_out = din("w_out", [D, D]); g_ffn = din("g_ffn", [D]); w_group = din("w_group", [D, 4])
    b_group = din("b_group", [4]); w_router = din("w_router", [D, 32]); b_router = din("b_router", [32])
    w_gate = din("w_gate", [32, D, 256]); w_up = din("w_up", [32, D, 256]); w_down = din("w_down", [32, 256, D])
    g_ple = din("g_ple", [D]); w_ple_gate = din("w_ple_gate", [D, D]); w_ple_proj = din("w_ple_proj", [256, D])
    g_final = din("g_final", [D])
    identF_d = din("identF", [128, 128]); mask_u_d = din("mask_u", [128, 128]); mask_l_d = din("mask_l", [128, 128])
    selA_d = din("selA", [128, 512])
    out_d = nc.dram_tensor("out", [T, D], F32, kind="ExternalOutput").ap()

    WX = dscr("WX", [32 * 128, 6144])
    NSLT_ = (2 * S) // 128 + 32
    Xg = [dscr(f"Xg{s}", [NSLT_ * 128, D]) for s in range(NSEQ)]
    Yg = [dscr(f"Yg{s}", [NSLT_ * 128, D]) for s in range(NSEQ)]
    X1 = [dscr(f"X1{s}", [S, D], F32) for s in range(NSEQ)]
    tstart_d = din("tstart", [128, 160]); pcol_d = din("pcol", [128, 1]); ustrict_d = din("ustrict", [128, 128])
    QT = [dscr(f"QT{s}", [512, S]) for s in range(NSEQ)]
    KT = [dscr(f"KT{s}", [512, S]) for s in range(NSEQ)]
    GQKV = [dscr(f"GQKV{s}", [1536, S]) for s in range(NSEQ)]
    DZ = [dscr(f"DZ{s}", [512, S]) for s in range(NSEQ)]
    THF = [dscr(f"THF{s}", [D, S]) for s in range(NSEQ)]
    THD = [dscr(f"THD{s}", [D, S]) for s in range(NSEQ)]
    AUGQ = [dscr(f"AUGQ{s}", [8, 6, S]) for s in range(NSEQ)]
    AUGK = [dscr(f"AUGK{s}", [8, 6, S]) for s in range(NSEQ)]
    ATs = [dscr(f"AT{s}", [512, S]) for s in range(NSEQ)]
    OGs = [dscr(f"OG{s}", [512, S]) for s in range(NSEQ)]
    GTs = [dscr(f"GT{s}", [32, S]) for s in range(NSEQ)]

    with ExitStack() as st:
        kb = KB(nc, st)

        ARENA_BYTES = 209920
        arena_t = st.enter_context(nc.sbuf_tensor("arena", [128, ARENA_BYTES // 4], F32))
        arena_off = [0]
        arena_hw = [0]

        def sb(name, shape, dt):
            esz = 2 if dt == BF16 else 4
            n = 1
            for d_ in shape[1:]:
                n *= d_
            nbytes = (n * esz + 31) // 32 * 32
            o = arena_off[0]
            assert o + nbytes <= ARENA_BYTES, (name, o, nbytes)
            arena_off[0] = o + nbytes
            arena_hw[0] = max(arena_hw[0], o + nbytes)
            v = arena_t[:, o // 4:(o + nbytes) // 4]
            if dt != F32:
                v = v.bitcast(dt)
            v = v[:, 0:n]
            if len(shape) == 3:
                v = v.rearrange("p (a b) -> p a b", a=shape[1])
            elif len(shape) == 4:
                v = v.rearrange("p (a b c) -> p a b c", a=shape[1], b=shape[2])
            return v

        def R(name=""):
            return kb.res(name)

        def mm(out, lhsT, rhs, start, stop, reads, writes, sig=None):
            if sig is None:
                sig = stop
            kb.op("pe", lambda e: e.matmul(out, lhsT=lhsT, rhs=rhs, start=start, stop=stop),
                  reads=reads, writes=writes, sig=sig)

        def tr(out, in_, ident, reads, writes):
            kb.op("pe", lambda e: e.transpose(out, in_, ident), reads=reads, writes=writes)

        def act(out, in_, func, reads, writes, bias=None, scale=None, accum=None):
            kw = {}
            if bias is not None:
                kw["bias"] = bias
            if scale is not None:
                kw["scale"] = scale
            if accum is not None:
                kw["accum_out"] = accum
            kb.op("act", lambda e: e.activation(out=out, in_=in_, func=func, **kw), reads=reads, writes=writes)

        def ts(eng, out, in0, s1, s2, op0, op1, reads, writes):
            if s2 is None:
                kb.op(eng, lambda e: e.tensor_scalar(out=out, in0=in0, scalar1=s1, scalar2=None, op0=op0),
                      reads=reads, writes=writes)
            else:
                kb.op(eng, lambda e: e.tensor_scalar(out=out, in0=in0, scalar1=s1, scalar2=s2, op0=op0, op1=op1),
                      reads=reads, writes=writes)

        def tt(eng, out, in0, in1, op, reads, writes):
            kb.op(eng, lambda e: e.tensor_tensor(out=out, in0=in0, in1=in1, op=op), reads=reads, writes=writes)

        def stt(out, in0, scalar, in1, op0, op1, reads, writes, eng="dve"):
            kb.op(eng, lambda e: e.scalar_tensor_tensor(out=out, in0=in0, scalar=scalar, in1=in1, op0=op0, op1=op1),
                  reads=reads, writes=writes)

        def cp(eng, out, in_, reads, writes):
            if eng == "act":
                kb.op("act", lambda e: e.copy(out, in_), reads=reads, writes=writes)
            else:
                kb.op(eng, lambda e: e.tensor_copy(out, in_), reads=reads, writes=writes)

        def ms(eng, ap, val, writes):
            kb.op(eng, lambda e: e.memset(ap, val), writes=writes)

        def ld(out, in_, writes, reads=(), stream="ld", q="sp", nonc=False):
            if nonc:
                kb.dma(q, stream, lambda e: e.dma_start(out=out, in_=in_, allow_slow_non_contiguous=True),
                       reads=reads, writes=writes)
            else:
                kb.dma(q, stream, lambda e: e.dma_start(out=out, in_=in_), reads=reads, writes=writes)

        def ldc(out, in_, writes, reads=(), stream="ldc", nonc=False):
            ld(out, in_, writes, reads, stream=stream, q="pool", nonc=nonc)

        def stq(out, in_, reads, writes=(), stream="st"):
            kb.dma("sp", stream, lambda e: e.dma_start(out=out, in_=in_), reads=reads, writes=writes)

        dbg_names = []

        def dbg(name, ap, reads, dt=F32):
            if not debug:
                return
            shp = list(ap.shape)
            d_ = nc.dram_tensor("dbg_" + name, shp, dt, kind="ExternalOutput").ap()
            dbg_names.append("dbg_" + name)
            idx = tuple(slice(None) for _ in shp)
            stq(d_[idx], ap, reads)

        class Rot:
            def __init__(self, name, shape, dt, n):
                self.t = [sb(f"{name}{i}", shape, dt) for i in range(n)]
                self.r = [R(f"{name}{i}") for i in range(n)]
                self.i = 0
                self.n = n

            def next(self):
                k = self.i % self.n
                self.i += 1
                return self.t[k], self.r[k]

        def barrier():
            keys = list(kb.count.keys())
            for e_ in KB.ENGS:
                for k in keys:
                    v = kb.count[k]
                    if v > 0 and kb.waited[e_].get(k, 0) < v and k != e_:
                        kb.waited[e_][k] = v
                        kb.prog[e_].append(("wait", k, v))

        PS = [st.enter_context(nc.psum_tensor(f"ps{i}", [128, 512], F32)) for i in range(7)]
        PR = [R(f"ps{i}") for i in range(7)]
        PB = st.enter_context(nc.psum_tensor("psb", [128, 1024], BF16))
        PBR = R("psb")

        identF = sb("identF", [128, 128], F32); r_identF = R()
        identB = sb("identB", [128, 128], BF16); r_identB = R()
        ident4F = sb("ident4F", [128, 512], F32); r_ident4F = R()
        masku = sb("masku", [128, 128], BF16); r_masku = R()
        maskl = sb("maskl", [128, 128], BF16); r_maskl = R()
        selA = sb("selA", [128, 512], F32); r_selA = R()
        onesF = sb("onesF", [128, 128], F32); r_onesF = R()
        onesB = sb("onesB", [128, 128], BF16); r_onesB = R()
        nhalf = sb("nhalf", [128, 512], F32); r_nhalf = R()
        onesS = sb("onesS", [128, 1024], BF16); r_onesS = R()
        ld(identF[:], identF_d[:, :], [r_identF])
        ldc(identB[:], identF_d[:, :], [r_identB])
        for h in range(4):
            ld(ident4F[:, h * 128:(h + 1) * 128], identF_d[:, :], [r_ident4F])
        ldc(masku[:], mask_u_d[:, :], [r_masku])
        ldc(maskl[:], mask_l_d[:, :], [r_maskl])
        ld(selA[:], selA_d[:, :], [r_selA])
        ms("dve", onesF[:], 1.0, [r_onesF])
        ms("dve", onesB[:], 1.0, [r_onesB])
        ms("dve", nhalf[:], -0.5, [r_nhalf])
        ms("dve", onesS[:], 1.0, [r_onesS])

        def colvec(name, src, nch):
            t = sb(name, [128, nch], F32); r = R()
            ld(t[:], src.rearrange("(c p) -> p c", p=128), [r], nonc=True)
            return t, r
        gmixT, r_gmixT = colvec("gmixT", g_mix, 8)
        gffnT, r_gffnT = colvec("gffnT", g_ffn, 8)
        gpleT, r_gpleT = colvec("gpleT", g_ple, 8)
        gonT, r_gonT = colvec("gonT", g_onorm, 1)
        gfin = sb("gfin", [128, D], F32); r_gfin = R()
        ld(gfin[:], g_final[None, :].to_broadcast([128, D]), [r_gfin])
        cw = sb("cw", [128, 12, 4], F32); r_cw = R()
        for j in range(4):
            ld(cw[:, :, j], conv_w[j, :].rearrange("(c p) -> p c", p=128), [r_cw], nonc=True)
        wrt_sb = sb("wrt", [128, 8, 36], BF16); r_wrt = R()
        ldc(wrt_sb[:, :, 0:4], w_group.rearrange("(k p) c -> p k c", p=128), [r_wrt], nonc=True)
        ldc(wrt_sb[:, :, 4:36], w_router.rearrange("(k p) c -> p k c", p=128), [r_wrt], nonc=True)
        brt = sb("brt", [128, 36], F32); r_brt = R()
        ld(brt[:, 0:4], b_group[None, :].to_broadcast([128, 4]), [r_brt])
        ld(brt[:, 4:36], b_router[None, :].to_broadcast([128, 32]), [r_brt])
        tots = sb("tots", [128, max(NT, 8)], F32); r_tots = R()
        C_FF, C_QKV, C_DA, C_DB, C_DZ, C_GF, C_GD = 1536, 1544, 3080, 3084, 3088, 3600, 4624

        def wcols(c0, n):
            return w_in[:, c0:c0 + n].rearrange("(k p) c -> p k c", p=128)

        def wres(name, src, kch, ncol):
            t = sb(name, [128, kch, ncol], BF16); r = R()
            ldc(t[:], src.rearrange("(k p) c -> p k c", p=128), [r])
            return t, r
        prmA = sb("prmA", [128, 2], F32); r_prmA = R()
        prmB = sb("prmB", [128, 2], F32); r_prmB = R()
        ms("dve", prmA[:], 0.0, [r_prmA])
        ms("dve", prmB[:], 0.0, [r_prmB])
        ld(prmA[0:8, 0:1], b_forget[:, None], [r_prmA], nonc=True)
        for o in (32, 64, 96):
            ld(prmA[o:o + 4, 0:1], dt_bias[:, None], [r_prmA], nonc=True)
            ld(prmA[o:o + 4, 1:2], a_log[:, None], [r_prmA], nonc=True)
        ld(prmB[64:68, 0:1], dt_bias[:, None], [r_prmB], nonc=True)
        ld(prmB[64:68, 1:2], a_log[:, None], [r_prmB], nonc=True)
        ts("dve", prmA[0:8, 0:1], prmA[0:8, 0:1], -1.0, None, ALU.mult, None, [r_prmA], [r_prmA])
        act(prmA[:, 1:2], prmA[:, 1:2], AF.Exp, [r_prmA], [r_prmA])
        ts("dve", prmA[:, 1:2], prmA[:, 1:2], -1.0, None, ALU.mult, None, [r_prmA], [r_prmA])
        act(prmB[:, 1:2], prmB[:, 1:2], AF.Exp, [r_prmB], [r_prmB])
        ts("dve", prmB[:, 1:2], prmB[:, 1:2], -1.0, None, ALU.mult, None, [r_prmB], [r_prmB])

        r_wexp = Res("wexp", nowaw=True)
        if stages >= 7:
            for e_ in range(32):
                rows = WX[e_ * 128:(e_ + 1) * 128, :]
                gu = rows[:, 0:4096].rearrange("p (k t f) -> p k t f", k=8, t=2)
                ldc(gu[:, :, 0, :], w_gate[e_].rearrange("(k p) f -> p k f", p=128), [r_wexp], stream="wx")
                ldc(gu[:, :, 1, :], w_up[e_].rearrange("(k p) f -> p k f", p=128), [r_wexp], stream="wx")
                ldc(rows[:, 4096:6144].rearrange("p (c f) -> p c f", c=2), w_down[e_].rearrange("(c p) f -> p c f", p=128),
                    [r_wexp], stream="wx")

        def RD():
            return Res("dram", nowaw=True)
        r_QT = [RD() for _ in range(NSEQ)]; r_KT = [RD() for _ in range(NSEQ)]; r_GQKV = [RD() for _ in range(NSEQ)]
        r_DZ = [RD() for _ in range(NSEQ)]; r_THF = [RD() for _ in range(NSEQ)]; r_THD = [RD() for _ in range(NSEQ)]
        r_aug = [RD() for _ in range(NSEQ)]; r_AT = [RD() for _ in range(NSEQ)]; r_OG = [RD() for _ in range(NSEQ)]
        r_GT = [RD() for _ in range(NSEQ)]; r_out = RD()
        r_Xg = [RD() for _ in range(NSEQ)]; r_Yg = [RD() for _ in range(NSEQ)]; r_X1 = [RD() for _ in range(NSEQ)]
        r_Xgz = [RD() for _ in range(NSEQ)]
        tstart = sb("tstart", [128, 160], F32); r_tstart = R()
        pcol = sb("pcol", [128, 1], F32); r_pcol = R()
        ustrict = sb("ustrict", [128, 128], BF16); r_ustrict = R()
        ld(tstart[:], tstart_d[:, :], [r_tstart])
        ld(pcol[:], pcol_d[:, :], [r_pcol])
        ldc(ustrict[:], ustrict_d[:, :], [r_ustrict])

        junk = sb("junk", [128, D], BF16); r_junk = R()
        st1_rot = Rot("st1", [128, 2], F32, 10)
        MARK0 = arena_off[0]

        def norm_stats(src, r_src):
            s1, r_s1 = st1_rot.next()
            ms("dve", s1[:, 0:1], 0.0, [r_s1])
            act(junk[:], src, AF.Square, [r_src, r_s1], [r_junk, r_s1], accum=s1[:, 0:1])
            ts("dve", s1[:, 0:1], s1[:, 0:1], 1.0 / D, EPS, ALU.mult, ALU.add, [r_s1], [r_s1])
            tt("pool", s1[:, 1:2], s1[:, 0:1], nhalf[:, 0:1], ALU.pow, [r_s1, r_nhalf], [r_s1])
            return s1, r_s1

        def norm_transpose(src, r_src, gT, r_gT, dst_fn, r_dst, xn_rot):
            s1, r_s1 = norm_stats(src, r_src)
            xn, r_xn = xn_rot.next()
            ts("dve", xn[:], src, s1[:, 1:2], None, ALU.mult, None, [r_src, r_s1], [r_xn])
            for c in range(8):
                tr(PB[:, c * 128:(c + 1) * 128], xn[:, c * 128:(c + 1) * 128], identB[:], [r_xn, r_identB], [PBR])
            for c in range(8):
                if c % 2 == 0:
                    act(dst_fn(c), PB[:, c * 128:(c + 1) * 128], AF.Copy, [PBR, r_gT], [r_dst], scale=gT[:, c:c + 1])
                else:
                    ts("dve", dst_fn(c), PB[:, c * 128:(c + 1) * 128], gT[:, c:c + 1], None, ALU.mult, None,
                       [PBR, r_gT], [r_dst])

        scale_q = 64 ** -0.5
        scale_gq = 128 ** -0.5

        for s in range(NSEQ):
            tok0 = s * S
            barrier()
            arena_off[0] = MARK0
            ZA = sb("ZA", [128, S], F32); r_ZA = R("ZA")
            ZB = sb("ZB", [128, S], F32); r_ZB = R("ZB")
            MARK1 = arena_off[0]
            Vall = sb("Vall", [128, NT, 8, 65], BF16); r_V = [R(f"V{i}") for i in range(NT)]
            MARK2 = arena_off[0]
            ms("dve", Vall[:], 1.0, r_V)
            hT = sb("hT", [128, 8, S], BF16)
            r_hT = [R(f"hT{i}") for i in range(NT)]
            wv_sb, r_wv = wres("wv", w_in[:, 1024:1536], 8, 512)
            wgA = sb("wgA", [128, 8, 128], BF16); r_wgA = R()
            wgB = sb("wgB", [128, 8, 128], BF16); r_wgB = R()
            ms("dve", wgA[:], 0.0, [r_wgA])
            ms("dve", wgB[:], 0.0, [r_wgB])
            ldc(wgA[:, :, 0:8], wcols(C_FF, 8), [r_wgA], nonc=True)
            for o in (32, 64, 96):
                ldc(wgA[:, :, o:o + 4], wcols(C_DA, 4), [r_wgA], nonc=True)
            ldc(wgB[:, :, 0:4], wcols(C_DB, 4), [r_wgB], nonc=True)
            ldc(wgB[:, :, 32:36], wcols(C_DB, 4), [r_wgB], nonc=True)
            ldc(wgB[:, :, 64:68], wcols(C_DA, 4), [r_wgB], nonc=True)
            xt_rot = Rot("xt", [128, D], F32, 2)
            xn_rot = Rot("xn", [128, D], BF16, 4)
            stg_rot = Rot("stg", [128, 512], BF16, 6)
            f32_rot = Rot("f32w", [128, 520], F32, 5)
            win_rot = Rot("win", [128, 8, 128], BF16, 3)
            p1st = {}

            def p1_a(i):
                xt, r_xt = xt_rot.next()
                ld(xt[:], x_d[tok0 + i * 128: tok0 + (i + 1) * 128, :], [r_xt])
                s1, r_s1 = norm_stats(xt[:], r_xt)
                xn, r_xn = xn_rot.next()
                ts("dve", xn[:], xt[:], s1[:, 1:2], None, ALU.mult, None, [r_xt, r_s1], [r_xn])
                p1st[i] = (xn, r_xn)

            def p1_b(i):
                xn, r_xn = p1st.pop(i)
                for c in range(8):
                    tr(PB[:, c * 128:(c + 1) * 128], xn[:, c * 128:(c + 1) * 128], identB[:], [r_xn, r_identB], [PBR])
                for c in range(8):
                    dstc = hT[:, c, i * 128:(i + 1) * 128]
                    if c % 2 == 0:
                        act(dstc, PB[:, c * 128:(c + 1) * 128], AF.Copy, [PBR, r_gmixT], [r_hT[i]], scale=gmixT[:, c:c + 1])
                    else:
                        ts("dve", dstc, PB[:, c * 128:(c + 1) * 128], gmixT[:, c:c + 1], None, ALU.mult, None,
                           [PBR, r_gmixT], [r_hT[i]])
            for t in range(NT + 2):
                if t < NT:
                    p1_a(t)
                if 0 <= t - 2 < NT:
                    p1_b(t - 2)
            if stages < 1.1:
                continue
            for i in range(NT):
                b = i % 2
                for k in range(8):
                    mm(PS[b][:, :], hT[:, k, i * 128:(i + 1) * 128], wv_sb[:, k, :], k == 0, k == 7,
                       [r_hT[i], r_wv], [PR[b]])
                cp("act" if i % 2 else "dve", Vall[:, i, :, 0:64],
                   PS[b][:, :].rearrange("p (h d) -> p h d", h=8), [PR[b]], [r_V[i]])
            if stages < 1.2:
                continue
            for g in range(NG):
                gt = [r_hT[4 * g + j] for j in range(4)]
                for (wg_, r_wg_, Z, r_Z, b) in ((wgA, r_wgA, ZA, r_ZA, 2), (wgB, r_wgB, ZB, r_ZB, 3)):
                    for k in range(8):
                        mm(PS[b][:, :], wg_[:, k, :], hT[:, k, g * 512:(g + 1) * 512], k == 0, k == 7,
                           gt + [r_wg_], [PR[b]])
                    cp("act", Z[:, g * 512:(g + 1) * 512], PS[b][:, :], [PR[b]], [r_Z])
            if stages < 1.3:
                continue
            chunks = []
            for c in range(4):
                chunks.append((c * 128, "q", c))
            for c in range(4):
                chunks.append((512 + c * 128, "k", c))
            for c in range(12):
                chunks.append((C_QKV + c * 128, "gdn", c))
            for c in range(4):
                chunks.append((C_DZ + c * 128, "dz", c))
            for c in range(8):
                chunks.append((C_GF + c * 128, "gf", c))
            for c in range(8):
                chunks.append((C_GD + c * 128, "gd", c))
            bsel = 0
            p2cnt = [0]
            p2pend = [None]
            if stages < 1.4:
                chunks = chunks[0:8]
            elif stages < 1.5:
                chunks = chunks[0:20]
            for (c0, kind, ci) in chunks:
                wt, r_wt = win_rot.next()
                ldc(wt[:], wcols(c0, 128), [r_wt], stream="ldw")
                halo, r_halo = None, None
                for g in range(NG):
                    gt = [r_hT[4 * g + j] for j in range(4)]
                    b = 4 + (bsel % 2)
                    bsel += 1
                    for k in range(8):
                        mm(PS[b][:, :], wt[:, k, :], hT[:, k, g * 512:(g + 1) * 512], k == 0, k == 7,
                           gt + [r_wt], [PR[b]])
                    cols = slice(g * 512, (g + 1) * 512)
                    if not (kind == "gdn" and ci < 8) and p2pend[0] is not None:
                        p2pend[0]()
                        p2pend[0] = None
                    stg, r_stg = stg_rot.next()
                    if kind == "q":
                        act(stg[:], PS[b][:, :], AF.Copy, [PR[b]], [r_stg], scale=scale_q)
                        stq(QT[s][ci * 128:(ci + 1) * 128, cols], stg[:], [r_stg], [r_QT[s]])
                    elif kind == "k":
                        cp("dve", stg[:], PS[b][:, :], [PR[b]], [r_stg])
                        stq(KT[s][ci * 128:(ci + 1) * 128, cols], stg[:], [r_stg], [r_KT[s]])
                    elif kind == "dz":
                        act(stg[:], PS[b][:, :], AF.Silu, [PR[b]], [r_stg])
                        stq(DZ[s][ci * 128:(ci + 1) * 128, cols], stg[:], [r_stg], [r_DZ[s]])
                    elif kind in ("gf", "gd"):
                        act(stg[:], PS[b][:, :], AF.Tanh, [PR[b]], [r_stg], scale=0.5)
                        dst, r_d = (THF[s], r_THF[s]) if kind == "gf" else (THD[s], r_THD[s])
                        stq(dst[ci * 128:(ci + 1) * 128, cols], stg[:], [r_stg], [r_d])
                    else:
                        zb, r_zb = f32_rot.next()
                        if g == 0:
                            ms("dve", zb[:, 0:3], 0.0, [r_zb])
                        else:
                            cp("dve", zb[:, 0:3], halo[:, 512:515], [r_halo], [r_zb])
                        cp("act", zb[:, 3:515], PS[b][:, :], [PR[b]], [r_zb])
                        halo, r_halo = zb, r_zb
                        cv, r_cv = f32_rot.next()
                        ts("dve", cv[:, 0:512], zb[:, 3:515], cw[:, ci, 3:4], None, ALU.mult, None, [r_zb, r_cw], [r_cv])
                        for j in range(3):
                            stt(cv[:, 0:512], zb[:, j:j + 512], cw[:, ci, j:j + 1], cv[:, 0:512], ALU.mult, ALU.add,
                                [r_zb, r_cw, r_cv], [r_cv])
                        if ci >= 8:
                            act(stg[:], cv[:, 0:512], AF.Silu, [r_cv], [r_stg])
                            stq(GQKV[s][ci * 128:(ci + 1) * 128, cols], stg[:], [r_stg], [r_GQKV[s]])
                        else:
                            act(cv[:, 0:512], cv[:, 0:512], AF.Silu, [r_cv], [r_cv])
                            sq, r_sq = stg_rot.next()
                            act(sq[:], cv[:, 0:512], AF.Square, [r_cv], [r_sq])
                            pss = p2cnt[0] % 2
                            p2cnt[0] += 1
                            for j in range(4):
                                mm(PS[pss][:, j:j + 1], sq[:, j * 128:(j + 1) * 128], onesB[:, 0:1], True, True,
                                   [r_onesB, r_sq], [PR[pss]], sig=(j == 3))

                            def g2(cv=cv, r_cv=r_cv, stg=stg, r_stg=r_stg, pss=pss, ci=ci, cols=cols):
                                rw, r_rw = f32_rot.next()
                                ts("dve", rw[:, 0:4], PS[pss][:, 0:4], EPS, None, ALU.add, None, [PR[pss]], [r_rw])
                                tt("pool", rw[:, 4:8], rw[:, 0:4], nhalf[:, 0:4], ALU.pow, [r_rw, r_nhalf], [r_rw])
                                for j in range(4):
                                    ts("dve", rw[:, 8 + j * 128:8 + (j + 1) * 128], identF[:], rw[:, 4 + j:5 + j], None, ALU.mult, None,
                                       [r_identF, r_rw], [r_rw])
                                for j in range(4):
                                    mm(PS[6][:, j * 128:(j + 1) * 128], onesF[:], rw[:, 8 + j * 128:8 + (j + 1) * 128], True, True,
                                       [r_onesF, r_rw], [PR[6]], sig=(j == 3))
                                stt(stg[:], cv[:, 0:512], scale_gq if ci < 4 else 1.0, PS[6][:, :], ALU.mult, ALU.mult,
                                    [r_cv, PR[6]], [r_stg])
                                stq(GQKV[s][ci * 128:(ci + 1) * 128, cols], stg[:], [r_stg], [r_GQKV[s]])
                            if p2pend[0] is not None:
                                p2pend[0]()
                            p2pend[0] = g2
            if p2pend[0] is not None:
                p2pend[0]()
                p2pend[0] = None
            if stages < 3:
                continue
            barrier()
            arena_off[0] = MARK2
            augb_rot = Rot("augb", [128, 6, 1024], BF16, 1)
            augf_rot = Rot("augf", [128, 2, 1024], F32, 1)
            def softplus_rows(Z, r_Z, prm, r_prm, lo, n, escale):
                rows = slice(lo, lo + n)
                act(Z[rows, :], Z[rows, :], AF.Exp, [r_Z, r_prm], [r_Z], bias=prm[rows, 0:1], scale=escale)
                act(Z[rows, :], Z[rows, :], AF.Ln, [r_Z], [r_Z], bias=1.0)
                ts("dve", Z[rows, :], Z[rows, :], prm[rows, 1:2], None, ALU.mult, None, [r_Z, r_prm], [r_Z])
            softplus_rows(ZA, r_ZA, prmA, r_prmA, 0, 8, -1.0)
            for o in (32, 64, 96):
                softplus_rows(ZA, r_ZA, prmA, r_prmA, o, 4, 1.0)
            softplus_rows(ZB, r_ZB, prmB, r_prmB, 0, 4, -1.0)
            softplus_rows(ZB, r_ZB, prmB, r_prmB, 32, 4, -1.0)
            softplus_rows(ZB, r_ZB, prmB, r_prmB, 64, 4, 1.0)

            def scan(Z, r_Z, rows, c0, n, init):
                kb.op("dve", lambda e: e.tensor_tensor_scan(out=Z[rows, c0:c0 + n], data0=onesS[rows, 0:n],
                                                            data1=Z[rows, c0:c0 + n], initial=init,
                                                            op0=ALU.mult, op1=ALU.add),
                      reads=[r_Z, r_onesS], writes=[r_Z])
            AB = min(1024, S)
            for blk in range(S // AB):
                scan(ZA, r_ZA, slice(0, 8), blk * AB, AB, 0.0 if blk == 0 else ZA[0:8, blk * AB - 1:blk * AB])
            for blk in range(S // AB):
                cs = slice(blk * AB, (blk + 1) * AB)
                ab, r_ab = augb_rot.next()
                af, r_af = augf_rot.next()
                cp("dve", ab[0:8, 0, 0:AB], ZA[0:8, cs], [r_ZA], [r_ab])
                tt("dve", af[0:8, 0, 0:AB], ZA[0:8, cs], ab[0:8, 0, 0:AB], ALU.subtract, [r_ZA, r_ab], [r_af])
                cp("dve", ab[0:8, 1, 0:AB], af[0:8, 0, 0:AB], [r_af], [r_ab])
                tt("dve", af[0:8, 1, 0:AB], af[0:8, 0, 0:AB], ab[0:8, 1, 0:AB], ALU.subtract, [r_af, r_ab], [r_af])
                cp("dve", ab[0:8, 2, 0:AB], af[0:8, 1, 0:AB], [r_af], [r_ab])
                for j in range(3):
                    ts("dve", ab[0:8, 3 + j, 0:AB], ab[0:8, j, 0:AB], -1.0, None, ALU.mult, None, [r_ab], [r_ab])
                for j in range(3):
                    stq(AUGQ[s][:, j, cs], ab[0:8, j, 0:AB], [r_ab], [r_aug[s]])
                    stq(AUGQ[s][:, 3 + j, cs], onesS[0:8, 0:AB], [r_onesS], [r_aug[s]])
                    stq(AUGK[s][:, j, cs], onesS[0:8, 0:AB], [r_onesS], [r_aug[s]])
                    stq(AUGK[s][:, 3 + j, cs], ab[0:8, 3 + j, 0:AB], [r_ab], [r_aug[s]])
            for n in range(NT):
                c0 = n * 128
                for o in (32, 64, 96):
                    scan(ZA, r_ZA, slice(o, o + 4), c0, 128, 0.0)
                scan(ZB, r_ZB, slice(64, 68), c0, 128, 0.0)
                for o in (64, 96):
                    cp("dve", tots[o:o + 4, n:n + 1], ZA[o:o + 4, c0 + 127:c0 + 128], [r_ZA], [r_tots])
                ts("dve", ZA[64:68, c0:c0 + 128], ZA[64:68, c0:c0 + 128], -1.0, tots[64:68, n:n + 1], ALU.mult, ALU.add,
                   [r_ZA, r_tots], [r_ZA])
                ts("dve", ZA[96:100, c0:c0 + 128], ZA[96:100, c0:c0 + 128], 0.0, tots[96:100, n:n + 1], ALU.mult, ALU.add,
                   [r_ZA, r_tots], [r_ZA])
            act(ZA[64:68, :], ZA[64:68, :], AF.Exp, [r_ZA], [r_ZA])
            act(ZA[96:100, :], ZA[96:100, :], AF.Exp, [r_ZA], [r_ZA])
            act(ZB[0:4, :], ZB[0:4, :], AF.Exp, [r_ZB], [r_ZB])
            tt("dve", ZB[32:36, :], ZB[32:36, :], ZA[32:36, :], ALU.add, [r_ZB, r_ZA], [r_ZB])
            act(ZB[32:36, :], ZB[32:36, :], AF.Exp, [r_ZB], [r_ZB])
            ts("dve", ZB[64:68, :], ZB[64:68, :], -1.0, None, ALU.mult, None, [r_ZB], [r_ZB])
            if stages < 4:
                continue
            barrier()
            arena_off[0] = MARK2
            qa_rot = Rot("qa", [128, S], BF16, 2)
            ka_rot = Rot("ka", [128, S], BF16, 2)
            vh_rot = Rot("vh", [128, NT, 128], BF16, 2)
            for i_ in range(2):
                ms("pool", qa_rot.t[i_][64:128, :], 0.0, [qa_rot.r[i_]])
                ms("pool", ka_rot.t[i_][64:128, :], 0.0, [ka_rot.r[i_]])
                ms("pool", vh_rot.t[i_][:], 0.0, [vh_rot.r[i_]])
            pt_rot = Rot("pt", [128, 512], BF16, 3)
            f32_rot = Rot("f32w", [128, 520], F32, 4)
            stg_rot = Rot("stg", [128, 512], BF16, 4)
            fox_pend = []
            fox_cnt = [0]
            for h in range(8):
                QA, r_QA = qa_rot.next()
                KA, r_KA = ka_rot.next()
                ld(QA[0:64, 0:S], QT[s][h * 64:(h + 1) * 64, :], [r_QA], [r_QT[s]])
                ld(QA[64:70, 0:S], AUGQ[s][h], [r_QA], [r_aug[s]])
                ld(KA[0:64, 0:S], KT[s][h * 64:(h + 1) * 64, :], [r_KA], [r_KT[s]])
                ld(KA[64:70, 0:S], AUGK[s][h], [r_KA], [r_aug[s]])
                Vh, r_Vh = vh_rot.next()
                cp("pool", Vh[:, :, 0:65], Vall[:, :, h, :], r_V, [r_Vh])
                for g in range(NG):
                    last = 4 * g + 3
                    po = 2 + 2 * (fox_cnt[0] % 2)
                    fox_cnt[0] += 1
                    pbc = po + 1

                    def scores(j):
                        m = j - 4 * g
                        c0 = max(m, 0) * 128
                        N = 512 - c0
                        b = j % 2
                        mm(PS[b][:, 0:N], KA[0:FOX_K, j * 128:(j + 1) * 128], QA[0:FOX_K, g * 512 + c0:(g + 1) * 512],
                           True, m < 0, [r_KA, r_QA], [PR[b]], sig=(m < 0))
                        if m >= 0:
                            mm(PS[b][:, 0:128], identB[:], masku[:], False, True, [r_identB, r_masku], [PR[b]])
                    scores(0)
                    for j in range(last + 1):
                        m = j - 4 * g
                        c0 = max(m, 0) * 128
                        N = 512 - c0
                        b = j % 2
                        if j + 1 <= last:
                            scores(j + 1)
                        if j == min(1, last) and len(fox_pend) > 0:
                            fox_pend.pop(0)()
                        pt, r_pt = pt_rot.next()
                        act(pt[:, 0:N], PS[b][:, 0:N], AF.Exp, [PR[b]], [r_pt])
                        mm(PS[po][0:FOX_M, c0:512], Vh[:, j, 0:FOX_M], pt[:, 0:N], j == 0, j == last,
                           [r_Vh, r_pt], [PR[po]])
                        if FOX_FILL:
                            kb.op("pe", lambda e: e.matmul(PS[6][:, 0:FOX_FILL_N], lhsT=identB[:], rhs=onesS[:, 0:FOX_FILL_N], start=True, stop=True),
                                  sig=False)
                    def epilogue(po=po, pbc=pbc, g=g, h=h):
                        ob, r_ob = f32_rot.next()
                        cp("act", ob[0:65, 0:512], PS[po][0:65, :], [PR[po]], [r_ob])
                        mm(PS[pbc][0:64, :], onesF[64:65, 0:64], ob[64:65, 0:512], True, True, [r_onesF, r_ob], [PR[pbc]])
                        rb, r_rb = f32_rot.next()
                        kb.op("dve", lambda e, rb=rb, pbc=pbc: e.reciprocal(rb[0:64, 0:512], PS[pbc][0:64, :]), reads=[PR[pbc]], writes=[r_rb])
                        stg, r_stg = stg_rot.next()
                        tt("dve", stg[0:64, :], ob[0:64, 0:512], rb[0:64, 0:512], ALU.mult, [r_ob, r_rb], [r_stg])
                        stq(ATs[s][h * 64:(h + 1) * 64, g * 512:(g + 1) * 512], stg[0:64, :], [r_stg], [r_AT[s]])
                    fox_pend.append(epilogue)
            while fox_pend:
                fox_pend.pop(0)()
            if stages < 5:
                continue
            barrier()
            arena_off[0] = MARK1
            qkv_rot = Rot("qkv", [128, 12, 128], BF16, 2)
            dz_rot = Rot("dzr", [128, 4, 128], BF16, 2)
            tok_rot = Rot("tok", [128, 128], F32, 4)
            e512_rot = Rot("e512", [128, 512], F32, 12)
            wl_rot = Rot("wl", [128, 512], BF16, 18)
            ws_rot = Rot("ws", [128, 512], F32, 8)
            f32_rot = Rot("f32w", [128, 520], F32, 2)
            stg_rot = Rot("stg", [128, 512], BF16, 3)
            Sst = sb("Sst", [128, 512], F32); r_Sst = R()
            Sbf = sb("Sbf", [128, 512], BF16); r_Sbf = R()
            ms("dve", Sst[:], 0.0, [r_Sst])
            ms("dve", Sbf[:], 0.0, [r_Sbf])
            gst_ = {}

            def g_pre(n):
                c0 = n * 128
                ccols = slice(c0, c0 + 128)
                qkvT, r_qkv = qkv_rot.next()
                ld(qkvT[:], GQKV[s][:, ccols].rearrange("(c p) t -> p c t", p=128), [r_qkv], [r_GQKV[s]])
                dzT, r_dz = dz_rot.next()
                ld(dzT[:], DZ[s][:, ccols].rearrange("(c p) t -> p c t", p=128), [r_dz], [r_DZ[s]])
                tokA, r_tokA = tok_rot.next()
                tokB, r_tokB = tok_rot.next()
                tr(PS[3][:, 0:128], ZA[:, ccols], identF[:], [r_ZA, r_identF], [PR[3]])
                cp("act", tokA[:], PS[3][:, 0:128], [PR[3]], [r_tokA])
                tr(PS[4][:, 0:128], ZB[:, ccols], identF[:], [r_ZB, r_identF], [PR[4]])
                cp("dve", tokB[:], PS[4][:, 0:128], [PR[4]], [r_tokB])
                HS = [slice(h * 128, (h + 1) * 128) for h in range(4)]
                for h in range(4):
                    kTh = qkvT[:, 4 + h, :]
                    mm(PS[0][:, HS[h]], kTh, kTh, True, True, [r_qkv], [PR[0]], sig=(h == 3))
                for h in range(4):
                    mm(PS[1][:, HS[h]], qkvT[:, 4 + h, :], qkvT[:, h, :], True, True, [r_qkv], [PR[1]], sig=(h == 3))
                for h in range(4):
                    mm(PS[2][:, HS[h]], selA[:, HS[h]], ZA[:, ccols], True, False, [r_selA, r_ZA], [PR[2]], sig=False)
                    mm(PS[2][:, HS[h]], identB[:], masku[:], False, True, [r_identB, r_masku], [PR[2]], sig=(h == 3))
                for h in range(4):
                    mm(PS[3][:, HS[h]], selA[:, HS[h]], ZA[:, ccols], True, False, [r_selA, r_ZA], [PR[3]], sig=False)
                    mm(PS[3][:, HS[h]], identB[:], maskl[:], False, True, [r_identB, r_maskl], [PR[3]], sig=(h == 3))
                for h in range(4):
                    mm(PS[4][:, HS[h]], selA[:, HS[h]], ZA[:, ccols], True, True, [r_selA, r_ZA], [PR[4]], sig=(h == 3))
                Eu, r_Eu = e512_rot.next()
                El, r_El = e512_rot.next()
                Ep, r_Ep = e512_rot.next()
                for h in range(4):
                    act(Eu[:, HS[h]], PS[2][:, HS[h]], AF.Exp, [PR[2], r_tokB], [r_Eu], bias=tokB[:, 64 + h:65 + h])
                    act(El[:, HS[h]], PS[3][:, HS[h]], AF.Exp, [PR[3], r_tokA], [r_El], bias=tokA[:, 32 + h:33 + h], scale=-1.0)
                act(Ep[:], PS[4][:, :], AF.Exp, [PR[4]], [r_Ep])
                ATt, r_ATt = wl_rot.next()
                tt("dve", ATt[:], PS[1][:, :], Eu[:], ALU.mult, [PR[1], r_Eu], [r_ATt])
                Lt, r_Lt = ws_rot.next()
                for h in range(4):
                    stt(Lt[:, HS[h]], PS[0][:, HS[h]], tokB[:, h:h + 1], El[:, HS[h]], ALU.mult, ALU.mult,
                        [PR[0], r_tokB, r_El], [r_Lt])
                qdec, r_qdec = wl_rot.next()
                tt("dve", qdec[:], qkvT[:, 0:4, :].rearrange("p c t -> p (c t)"), Ep[:], ALU.mult, [r_qkv, r_Ep], [r_qdec])
                for h in range(4):
                    tr(PB[:, HS[h]], qkvT[:, 4 + h, :], identB[:], [r_qkv, r_identB], [PBR])
                    tr(PB[:, 512 + h * 128:512 + (h + 1) * 128], qkvT[:, 8 + h, :], identB[:], [r_qkv, r_identB], [PBR])
                kbg, r_kbg = wl_rot.next()
                kdec, r_kdec = wl_rot.next()
                vb, r_vb = wl_rot.next()
                for h in range(4):
                    ts("dve", kbg[:, HS[h]], PB[:, HS[h]], tokB[:, 32 + h:33 + h], None, ALU.mult, None, [PBR, r_tokB], [r_kbg])
                    act(kdec[:, HS[h]], PB[:, HS[h]], AF.Copy, [PBR, r_tokA], [r_kdec], scale=tokA[:, 64 + h:65 + h])
                    ts("dve", vb[:, HS[h]], PB[:, 512 + h * 128:512 + (h + 1) * 128], tokB[:, h:h + 1], None, ALU.mult, None,
                       [PBR, r_tokB], [r_vb])
                for h in range(4):
                    tr(PS[5][:, HS[h]], Lt[:, HS[h]], identF[:], [r_Lt, r_identF], [PR[5]])
                Mt, r_Mt = ws_rot.next()
                cp("act", Mt[:], PS[5][:, :], [PR[5]], [r_Mt])
                Rt, r_Rt = ws_rot.next()
                tt("dve", Rt[:], ident4F[:], PS[5][:, :], ALU.subtract, [r_ident4F, PR[5]], [r_Rt])
                Pc, r_Pc, Qc, r_Qc = Lt, r_Lt, Mt, r_Mt
                for lvl in range(6):
                    for h in range(4):
                        mm(PS[0][:, HS[h]], Qc[:, HS[h]], Pc[:, HS[h]], True, True, [r_Qc, r_Pc], [PR[0]], sig=(h == 3))
                    if lvl < 5:
                        for h in range(4):
                            mm(PS[1][:, HS[h]], Pc[:, HS[h]], Qc[:, HS[h]], True, True, [r_Qc, r_Pc], [PR[1]], sig=(h == 3))
                    Pn, r_Pn = ws_rot.next()
                    cp("act", Pn[:], PS[0][:, :], [PR[0]], [r_Pn])
                    if lvl < 5:
                        Qn, r_Qn = ws_rot.next()
                        cp("dve", Qn[:], PS[1][:, :], [PR[1]], [r_Qn])
                    for h in range(4):
                        mm(PS[2][:, HS[h]], Pn[:, HS[h]], Rt[:, HS[h]], True, True, [r_Pn, r_Rt], [PR[2]], sig=(h == 3))
                    Rn, r_Rn = ws_rot.next()
                    tt("dve", Rn[:], Rt[:], PS[2][:, :], ALU.add, [r_Rt, PR[2]], [r_Rn])
                    Rt, r_Rt = Rn, r_Rn
                    Pc, r_Pc = Pn, r_Pn
                    if lvl < 5:
                        Qc, r_Qc = Qn, r_Qn
                Rf, r_Rf = Rt, r_Rt
                Rt, r_Rt = wl_rot.next()
                cp("act", Rt[:], Rf[:], [r_Rf], [r_Rt])
                for h in range(4):
                    mm(PS[3][:, HS[h]], kbg[:, HS[h]], Rt[:, HS[h]], True, True, [r_kbg, r_Rt], [PR[3]], sig=(h == 3))
                for h in range(4):
                    mm(PS[4][:, HS[h]], Rt[:, HS[h]], vb[:, HS[h]], True, True, [r_vb, r_Rt], [PR[4]], sig=(h == 3))
                wT, r_wT = wl_rot.next()
                cp("act", wT[:], PS[3][:, :], [PR[3]], [r_wT])
                uu, r_uu = e512_rot.next()
                cp("dve", uu[:], PS[4][:, :], [PR[4]], [r_uu])
                gst_[n] = dict(ccols=ccols, HS=HS, tokA=tokA, r_tokA=r_tokA, dzT=dzT, r_dz=r_dz, ATt=ATt, r_ATt=r_ATt,
                               qdec=qdec, r_qdec=r_qdec, kdec=kdec, r_kdec=r_kdec, wT=wT, r_wT=r_wT, uu=uu, r_uu=r_uu)

            def g_scan(n):
                d_ = gst_.pop(n)
                ccols, HS, tokA, r_tokA, dzT, r_dz = d_['ccols'], d_['HS'], d_['tokA'], d_['r_tokA'], d_['dzT'], d_['r_dz']
                ATt, r_ATt, qdec, r_qdec, kdec, r_kdec = d_['ATt'], d_['r_ATt'], d_['qdec'], d_['r_qdec'], d_['kdec'], d_['r_kdec']
                wT, r_wT, uu, r_uu = d_['wT'], d_['r_wT'], d_['uu'], d_['r_uu']
                for h in range(4):
                    mm(PS[5][:, HS[h]], wT[:, HS[h]], Sbf[:, HS[h]], True, True, [r_wT, r_Sbf], [PR[5]], sig=(h == 3))
                vnew, r_vnew = wl_rot.next()
                tt("dve", vnew[:], uu[:], PS[5][:, :], ALU.subtract, [r_uu, PR[5]], [r_vnew])
                for h in range(4):
                    mm(PS[6][:, HS[h]], Sbf[:, HS[h]], qdec[:, HS[h]], True, False, [r_Sbf, r_qdec], [PR[6]], sig=False)
                    mm(PS[6][:, HS[h]], vnew[:, HS[h]], ATt[:, HS[h]], False, True, [r_vnew, r_ATt], [PR[6]], sig=(h == 3))
                for h in range(4):
                    mm(PS[0][:, HS[h]], kdec[:, HS[h]], vnew[:, HS[h]], True, True, [r_kdec, r_vnew], [PR[0]], sig=(h == 3))
                for h in range(4):
                    stt(Sst[:, HS[h]], Sst[:, HS[h]], tokA[:, 96 + h:97 + h], PS[0][:, HS[h]], ALU.mult, ALU.add,
                        [r_Sst, r_tokA, PR[0]], [r_Sst])
                cp("act", Sbf[:], Sst[:], [r_Sst], [r_Sbf])
                sq, r_sq = wl_rot.next()
                act(sq[:], PS[6][:, :], AF.Square, [PR[6]], [r_sq])
                for h in range(4):
                    mm(PS[1][:, h:h + 1], sq[:, HS[h]], onesB[:, 0:1], True, True, [r_onesB, r_sq], [PR[1]], sig=(h == 3))
                rw, r_rw = f32_rot.next()
                ts("dve", rw[:, 0:4], PS[1][:, 0:4], 1.0 / 128, EPS, ALU.mult, ALU.add, [PR[1]], [r_rw])
                tt("pool", rw[:, 4:8], rw[:, 0:4], nhalf[:, 0:4], ALU.pow, [r_rw, r_nhalf], [r_rw])
                Dg, r_Dg = e512_rot.next()
                for h in range(4):
                    ts("dve", Dg[:, HS[h]], identF[:], rw[:, 4 + h:5 + h], None, ALU.mult, None, [r_identF, r_rw], [r_Dg])
                for h in range(4):
                    mm(PS[2][:, HS[h]], onesF[:], Dg[:, HS[h]], True, True, [r_onesF, r_Dg], [PR[2]], sig=(h == 3))
                rnb, r_rnb = e512_rot.next()
                cp("act", rnb[:], PS[2][:, :], [PR[2]], [r_rnb])
                o1, r_o1 = e512_rot.next()
                stt(o1[:], PS[6][:, :], gonT[:, 0:1], rnb[:], ALU.mult, ALU.mult, [PR[6], r_gonT, r_rnb], [r_o1])
                stg, r_stg = stg_rot.next()
                tt("dve", stg[:], o1[:], dzT[:].rearrange("p c t -> p (c t)"), ALU.mult, [r_o1, r_dz], [r_stg])
                stq(OGs[s][:, ccols].rearrange("(h p) c -> p h c", p=128), stg[:].rearrange("p (h c) -> p h c", h=4),
                    [r_stg], [r_OG[s]])
            for t in range(NT + 1):
                if t < NT:
                    g_pre(t)
                if t >= 1:
                    g_scan(t - 1)
            if stages < 6:
                continue
            barrier()
            arena_off[0] = MARK0
            NSLT = (2 * S) // 128 + 32
            Tt = sb("Tt", [128, NT, D], BF16); r_Tt = [R() for _ in range(NT)]
            A12 = sb("A12", [128, NT, 64], F32); r_A12 = [R() for _ in range(NT)]
            RTW = sb("RTW", [128, NT, 2], F32); r_RTW = [R() for _ in range(NT)]
            RK = sb("RK", [128, NT, 32], F32); r_RK = [R() for _ in range(NT)]
            SLf = sb("SLf", [128, NT, 2], F32); r_SLf = R()
            SLi = sb("SLi", [128, NT, 2], mybir.dt.int32); r_SLi = R()
            WIf = sb("WIf", [128, NSLT], F32); r_WIf = R()
            WIi = sb("WIi", [128, NSLT], mybir.dt.int32); r_WIi = R()
            cnt = sb("cnt", [128, 5, 32], F32); r_cnt = R()
            gffn_b = sb("gffn_b", [128, D], F32); r_gffn_b = R()
            ld(gffn_b[:], g_ffn[None, :].to_broadcast([128, D]), [r_gffn_b])
            MARKT = arena_off[0]
            wout_sb, r_wout = wres("wout", w_out, 8, D)
            wofox_sb, r_wofox = wres("wofox", w_o_fox, 4, D)
            wodel_sb, r_wodel = wres("wodel", w_o_delta, 4, D)
            xg = sb("xg", [128, 4, D], F32); r_xg = [R() for _ in range(4)]
            mrg = sb("mrg", [128, 8, 512], BF16); r_mrg = R()
            at_rot = Rot("atr", [128, 4, 512], BF16, 2)
            th_rot = Rot("thr", [128, 8, 512], BF16, 2)
            e512_rot = Rot("e512", [128, 512], F32, 4)
            rt_rot = Rot("rtr", [128, 128], F32, 2)
            tTt_rot = Rot("tTt", [128, 8, 128], BF16, 2)
            zt = sb("zt", [128, D], BF16); r_zt = R()
            ms("dve", zt[:], 0.0, [r_zt])
            for i in range(NSLT):
                stq(Xg[s][i * 128:(i + 1) * 128, :], zt[:], [r_zt], [r_Xgz[s]])
            for g in range(NG):
                cols = slice(g * 512, (g + 1) * 512)
                atT, r_atT = at_rot.next()
                ogT, r_ogT = at_rot.next()
                ld(atT[:], ATs[s][:, cols].rearrange("(k p) t -> p k t", p=128), [r_atT], [r_AT[s]])
                ld(ogT[:], OGs[s][:, cols].rearrange("(k p) t -> p k t", p=128), [r_ogT], [r_OG[s]])
                thf, r_thf = th_rot.next()
                thd, r_thd = th_rot.next()
                ld(thf[:], THF[s][:, cols].rearrange("(k p) t -> p k t", p=128), [r_thf], [r_THF[s]])
                ld(thd[:], THD[s][:, cols].rearrange("(k p) t -> p k t", p=128), [r_thd], [r_THD[s]])
                for j in range(4):
                    r0 = tok0 + g * 512 + j * 128
                    ld(xg[:, j, :], x_d[r0:r0 + 128, :], [r_xg[j]])
                for m in range(8):
                    ms_ = slice(m * 128, (m + 1) * 128)
                    for k in range(4):
                        mm(PS[0][:, :], wofox_sb[:, k, ms_], atT[:, k, :], k == 0, k == 3, [r_wofox, r_atT], [PR[0]])
                    for k in range(4):
                        mm(PS[1][:, :], wodel_sb[:, k, ms_], ogT[:, k, :], k == 0, k == 3, [r_wodel, r_ogT], [PR[1]])
                    m1, r_m1 = e512_rot.next()
                    m2, r_m2 = e512_rot.next()
                    stt(m1[:], thf[:, m, :], 1.0, PS[0][:, :], ALU.add, ALU.mult, [r_thf, PR[0]], [r_m1])
                    stt(m2[:], thd[:, m, :], 1.0, PS[1][:, :], ALU.add, ALU.mult, [r_thd, PR[1]], [r_m2])
                    tt("pool", mrg[:, m, :], m1[:], m2[:], ALU.add, [r_m1, r_m2], [r_mrg])
                for j in range(4):
                    for hf in range(2):
                        b = 2 + hf
                        for k in range(8):
                            mm(PS[b][:, :], mrg[:, k, j * 128:(j + 1) * 128], wout_sb[:, k, hf * 512:(hf + 1) * 512],
                               k == 0, k == 7, [r_mrg, r_wout], [PR[b]])
                        stt(xg[:, j, hf * 512:(hf + 1) * 512], PS[b][:, :], 0.5, xg[:, j, hf * 512:(hf + 1) * 512],
                            ALU.mult, ALU.add, [PR[b], r_xg[j]], [r_xg[j]])
                for j in range(4):
                    n = 4 * g + j
                    stq(X1[s][n * 128:(n + 1) * 128, :], xg[:, j, :], [r_xg[j]], [r_X1[s]])
                    s1, r_s1 = norm_stats(xg[:, j, :], r_xg[j])
                    stt(Tt[:, n, :], xg[:, j, :], s1[:, 1:2], gffn_b[:], ALU.mult, ALU.mult, [r_xg[j], r_s1, r_gffn_b], [r_Tt[n]])
                    for c in range(8):
                        tr(PB[:, c * 128:(c + 1) * 128], Tt[:, n, c * 128:(c + 1) * 128], identB[:], [r_Tt[n], r_identB], [PBR])
                    tTt, r_tTt = tTt_rot.next()
                    cp("act", tTt[:].rearrange("p c t -> p (c t)"), PB[:, :], [PBR], [r_tTt])
                    for k in range(8):
                        mm(PS[4][:, 0:36], tTt[:, k, :], wrt_sb[:, k, :], k == 0, k == 7, [r_tTt, r_wrt], [PR[4]])
                    rt, r_rt = rt_rot.next()
                    lg = rt[:, 0:36]
                    tt("dve", lg, PS[4][:, 0:36], brt[:], ALU.add, [PR[4], r_brt], [r_rt])
                    gmax = rt[:, 40:41]; ngm = rt[:, 41:42]; sg = rt[:, 42:43]; psel = rt[:, 43:44]
                    oh = rt[:, 44:48]; el = rt[:, 48:56]; m8 = rt[:, 56:64]; msk = rt[:, 64:72]; a1 = rt[:, 72:80]
                    nm1 = rt[:, 80:81]; e21 = rt[:, 81:82]; cc = rt[:, 82:83]; eg = rt[:, 84:88]; a2 = rt[:, 88:96]
                    rr_ = [r_rt]
                    kb.op("dve", lambda e, lg=lg, gmax=gmax: e.tensor_reduce(out=gmax, in_=lg[:, 0:4], axis=mybir.AxisListType.X, op=ALU.max),
                          reads=rr_, writes=rr_)
                    ts("dve", oh, lg[:, 0:4], gmax, None, ALU.is_ge, None, rr_, rr_)
                    ts("dve", ngm, gmax, -1.0, None, ALU.mult, None, rr_, rr_)
                    ms("dve", sg, 0.0, rr_)
                    act(eg, lg[:, 0:4], AF.Exp, rr_, rr_, bias=ngm, accum=sg)
                    kb.op("dve", lambda e, psel=psel, sg=sg: e.reciprocal(psel, sg), reads=rr_, writes=rr_)
                    ts("dve", el, lg[:, 4:12], oh[:, 0:1], None, ALU.mult, None, rr_, rr_)
                    for gg in range(1, 4):
                        stt(el, lg[:, 4 + 8 * gg:12 + 8 * gg], oh[:, gg:gg + 1], el, ALU.mult, ALU.add, rr_, rr_)
                    kb.op("dve", lambda e, m8=m8, el=el: e.max(out=m8, in_=el), reads=rr_, writes=rr_)
                    ts("dve", msk, el, m8[:, 1:2], None, ALU.is_ge, None, rr_, rr_)
                    ts("dve", a1, el, m8[:, 0:1], None, ALU.is_ge, None, rr_, rr_)
                    tt("dve", a2, msk, a1, ALU.subtract, rr_, rr_)
                    ts("dve", nm1, m8[:, 0:1], -1.0, None, ALU.mult, None, rr_, rr_)
                    act(e21, m8[:, 1:2], AF.Exp, rr_, rr_, bias=nm1)
                    ts("dve", cc, e21, 1.0, None, ALU.add, None, rr_, rr_)
                    kb.op("dve", lambda e, cc=cc: e.reciprocal(cc, cc), reads=rr_, writes=rr_)
                    tt("dve", RTW[:, n, 0:1], cc, psel, ALU.mult, rr_, [r_RTW[n]])
                    tt("dve", RTW[:, n, 1:2], RTW[:, n, 0:1], e21, ALU.mult, rr_ + [r_RTW[n]], [r_RTW[n]])
                    for gg in range(4):
                        ts("dve", A12[:, n, 8 * gg:8 * gg + 8], a1, oh[:, gg:gg + 1], None, ALU.mult, None, rr_, [r_A12[n]])
                        ts("dve", A12[:, n, 32 + 8 * gg:40 + 8 * gg], a2, oh[:, gg:gg + 1], None, ALU.mult, None, rr_, [r_A12[n]])
            barrier()
            arena_off[0] = MARKT
            asum_rot = Rot("asum", [128, 32], BF16, 3)
            tmp_rot = Rot("tmpr", [128, 64], F32, 3)
            ms("dve", cnt[:, 0, :], 0.0, [r_cnt])
            for n in range(NT):
                asum, r_asum = asum_rot.next()
                tt("dve", asum[:], A12[:, n, 0:32], A12[:, n, 32:64], ALU.add, [r_A12[n]], [r_asum])
                mm(PS[0][:, 0:32], ustrict[:], asum[:], True, True, [r_ustrict, r_asum], [PR[0]])
                mm(PS[1][:, 0:32], onesB[:], asum[:], True, True, [r_onesB, r_asum], [PR[1]])
                tt("dve", RK[:, n, :], PS[0][:, 0:32], cnt[:, 0, :], ALU.add, [PR[0], r_cnt], [r_RK[n]])
                tt("dve", cnt[:, 0, :], cnt[:, 0, :], PS[1][:, 0:32], ALU.add, [PR[1], r_cnt], [r_cnt])
            for e_ in range(32):
                tmp, r_tmp = tmp_rot.next()
                ts("dve", tmp[:, 0:64], tstart[:, 0:64], cnt[:, 0, e_:e_ + 1], None, ALU.is_lt, None, [r_tstart, r_cnt], [r_tmp])
                kb.op("dve", lambda e, tmp=tmp, e_=e_, cnt=cnt: e.tensor_reduce(out=cnt[:, 2, e_:e_ + 1], in_=tmp[:, 0:64],
                                                                        axis=mybir.AxisListType.X, op=ALU.add),
                      reads=[r_tmp], writes=[r_cnt])
            ts("dve", cnt[:, 2, :], cnt[:, 2, :], 128.0, None, ALU.mult, None, [r_cnt], [r_cnt])
            kb.op("dve", lambda e, o_=cnt[:, 3, :], d_=cnt[:, 2, :]: e.tensor_tensor_scan(out=o_, data0=onesS[:, 0:32], data1=d_, initial=0.0,
                                                                             op0=ALU.mult, op1=ALU.add), reads=[r_cnt, r_onesS], writes=[r_cnt])
            tt("dve", cnt[:, 4, :], cnt[:, 3, :], cnt[:, 2, :], ALU.subtract, [r_cnt], [r_cnt])
            ms("dve", WIf[:], 0.0, [r_WIf])
            for e_ in range(32):
                stt(WIf[:], tstart[:, 0:NSLT], cnt[:, 3, e_:e_ + 1], WIf[:], ALU.is_ge, ALU.add, [r_tstart, r_cnt, r_WIf], [r_WIf])
            ts("dve", WIf[:], WIf[:], 31.0, None, ALU.min, None, [r_WIf], [r_WIf])
            ts("dve", WIf[:], WIf[:], 128.0, pcol[:, 0:1], ALU.mult, ALU.add, [r_WIf, r_pcol], [r_WIf])
            cp("dve", WIi[:], WIf[:], [r_WIf], [r_WIi])
            for n in range(NT):
                tmp, r_tmp = tmp_rot.next()
                tt("dve", tmp[:, 0:32], RK[:, n, :], cnt[:, 4, :], ALU.add, [r_RK[n], r_cnt], [r_tmp])
                for k in range(2):
                    tt("dve", tmp[:, 32:64], tmp[:, 0:32], A12[:, n, 32 * k:32 * k + 32], ALU.mult, [r_tmp, r_A12[n]], [r_tmp])
                    kb.op("dve", lambda e, tmp=tmp, n=n, k=k, SLf=SLf: e.tensor_reduce(out=SLf[:, n, k:k + 1], in_=tmp[:, 32:64],
                                                                              axis=mybir.AxisListType.X, op=ALU.add),
                          reads=[r_tmp], writes=[r_SLf])
            cp("dve", SLi[:], SLf[:], [r_SLf], [r_SLi])
            for n in range(NT):
                for k in range(2):
                    kb.dma("pool", "sc", lambda e, o_=Xg[s][:, :], i_=SLi[:, n, k:k + 1], t_=Tt[:, n, :]: e.indirect_dma_start(
                        out=o_, out_offset=bass.IndirectOffsetOnAxis(ap=i_, axis=0),
                        in_=t_, in_offset=None), reads=[r_Tt[n], r_SLi, r_Xgz[s]], writes=[r_Xg[s]])
            wx_rot = Rot("wx", [128, 6144], BF16, 4)
            xs_rot = Rot("xs", [128, D], BF16, 3)
            xsT_rot = Rot("xsT", [128, 8, 128], BF16, 3)
            hs_rot = Rot("hs", [128, 256], F32, 2)
            hb_rot = Rot("hb", [128, 256], BF16, 3)
            hT_rot = Rot("hTr", [128, 2, 128], BF16, 2)
            ys_rot = Rot("ys", [128, D], BF16, 2)
            PB2 = PS[6][:, :].bitcast(BF16)
            cst = {}

            def c_load(i):
                wx, r_wx = wx_rot.next()
                kb.dma("pool", "wxg", lambda e, o_=wx[:], i_=WIi[:, i:i + 1], w_=WX[:, :]: e.indirect_dma_start(
                    out=o_, out_offset=None, in_=w_,
                    in_offset=bass.IndirectOffsetOnAxis(ap=i_, axis=0)), reads=[r_WIi, r_wexp], writes=[r_wx])
                xs, r_xs = xs_rot.next()
                ld(xs[:], Xg[s][i * 128:(i + 1) * 128, :], [r_xs], [r_Xg[s]])
                cst[i] = dict(wx=wx, r_wx=r_wx, xs=xs, r_xs=r_xs)

            def c_tr(i):
                d_ = cst[i]
                for c in range(8):
                    tr(PB[:, c * 128:(c + 1) * 128], d_["xs"][:, c * 128:(c + 1) * 128], identB[:], [d_["r_xs"], r_identB], [PBR])
                xsT, r_xsT = xsT_rot.next()
                cp("dve", xsT[:].rearrange("p c t -> p (c t)"), PB[:, :], [PBR], [r_xsT])
                d_["xsT"], d_["r_xsT"] = xsT, r_xsT

            def c_gu(i):
                d_ = cst[i]
                b0 = 3 * (i % 2)
                for k in range(8):
                    mm(PS[b0][:, :], d_["xsT"][:, k, :], d_["wx"][:, k * 512:(k + 1) * 512], k == 0, k == 7,
                       [d_["r_xsT"], d_["r_wx"]], [PR[b0]])
                hsg, r_hsg = hs_rot.next()
                act(hsg[:], PS[b0][:, 0:256], AF.Silu, [PR[b0]], [r_hsg])
                hb, r_hb = hb_rot.next()
                tt("dve", hb[:], hsg[:], PS[b0][:, 256:512], ALU.mult, [r_hsg, PR[b0]], [r_hb])
                d_["hb"], d_["r_hb"] = hb, r_hb

            def c_down(i):
                d_ = cst.pop(i)
                b0 = 3 * (i % 2)
                for c in range(2):
                    tr(PB2[:, c * 128:(c + 1) * 128], d_["hb"][:, c * 128:(c + 1) * 128], identB[:], [d_["r_hb"], r_identB], [PR[6]])
                hTt, r_hTt = hT_rot.next()
                cp("act", hTt[:].rearrange("p c t -> p (c t)"), PB2[:, 0:256], [PR[6]], [r_hTt])
                ys, r_ys = ys_rot.next()
                wx = d_["wx"]
                for hf in range(2):
                    b = b0 + 1 + hf
                    for c in range(2):
                        mm(PS[b][:, :], hTt[:, c, :], wx[:, 4096 + c * 1024 + hf * 512:4096 + c * 1024 + (hf + 1) * 512],
                           c == 0, c == 1, [r_hTt, d_["r_wx"]], [PR[b]])
                    cp("act" if hf else "dve", ys[:, hf * 512:(hf + 1) * 512], PS[b][:, :], [PR[b]], [r_ys])
                stq(Yg[s][i * 128:(i + 1) * 128, :], ys[:], [r_ys], [r_Yg[s]])
            for t in range(NSLT + 3):
                if t < NSLT:
                    c_load(t)
                if 0 <= t - 1 < NSLT:
                    c_tr(t - 1)
                if 0 <= t - 2 < NSLT:
                    c_gu(t - 2)
                if 0 <= t - 3 < NSLT:
                    c_down(t - 3)
            barrier()
            arena_off[0] = MARKT
            wpg_sb, r_wpg = wres("wpg", w_ple_gate, 8, D)
            wpp_sb, r_wpp = wres("wpp", w_ple_proj, 2, D)
            xw_rot = Rot("xw", [128, D], F32, 6)
            yg_rot = Rot("yg", [128, D], BF16, 8)
            e512_rot = Rot("e512", [128, 512], F32, 4)
            nT_rot = Rot("nTr", [128, 8, 128], BF16, 3)
            xn_rot = Rot("xn", [128, D], BF16, 2)
            stg_rot = Rot("stg", [128, 512], BF16, 6)
            f32_rot = Rot("f32w", [128, 520], F32, 4)
            yt_rot = Rot("yt", [128, D], F32, 2)
            dst = {}

            def d_0(n):
                r0 = tok0 + n * 128
                xw, r_xw = xw_rot.next()
                ld(xw[:], X1[s][n * 128:(n + 1) * 128, :], [r_xw], [r_X1[s]])
                pt_, r_pt_ = f32_rot.next()
                ld(pt_[:, 0:256], p_d[r0:r0 + 128, :], [r_pt_])
                ygs = []
                for k in range(2):
                    yg, r_yg = yg_rot.next()
                    kb.dma("pool", "yg", lambda e, o_=yg[:], y_=Yg[s][:, :], i_=SLi[:, n, k:k + 1]: e.indirect_dma_start(
                        out=o_, out_offset=None, in_=y_,
                        in_offset=bass.IndirectOffsetOnAxis(ap=i_, axis=0)), reads=[r_SLi, r_Yg[s]], writes=[r_yg])
                    ygs.append((yg, r_yg))
                dst[n] = dict(xw=xw, r_xw=r_xw, pt_=pt_, r_pt_=r_pt_, ygs=ygs)

            def d_a(n):
                d_ = dst[n]
                xw, r_xw, pt_, r_pt_ = d_["xw"], d_["r_xw"], d_["pt_"], d_["r_pt_"]
                for k in range(2):
                    yg, r_yg = d_["ygs"][k]
                    stt(xw[:], yg[:], RTW[:, n, k:k + 1], xw[:], ALU.mult, ALU.add, [r_yg, r_RTW[n], r_xw], [r_xw])
                pbf, r_pbf = stg_rot.next()
                cp("dve", pbf[:, 0:256], pt_[:, 0:256], [r_pt_], [r_pbf])
                s1, r_s1 = norm_stats(xw[:], r_xw)
                d_.update(pbf=pbf, r_pbf=r_pbf, s1=s1, r_s1=r_s1)

            def d_b(n):
                d_ = dst[n]
                xw, r_xw, s1, r_s1 = d_["xw"], d_["r_xw"], d_["s1"], d_["r_s1"]
                xn, r_xn = xn_rot.next()
                ts("dve", xn[:], xw[:], s1[:, 1:2], None, ALU.mult, None, [r_xw, r_s1], [r_xn])
                for c in range(8):
                    tr(PB[:, c * 128:(c + 1) * 128], xn[:, c * 128:(c + 1) * 128], identB[:], [r_xn, r_identB], [PBR])
                nT, r_nT = nT_rot.next()
                for c in range(8):
                    if c % 2 == 0:
                        act(nT[:, c, :], PB[:, c * 128:(c + 1) * 128], AF.Copy, [PBR, r_gpleT], [r_nT], scale=gpleT[:, c:c + 1])
                    else:
                        ts("dve", nT[:, c, :], PB[:, c * 128:(c + 1) * 128], gpleT[:, c:c + 1], None, ALU.mult, None,
                           [PBR, r_gpleT], [r_nT])
                PB2 = PS[6][:, :].bitcast(BF16)
                for k in range(2):
                    tr(PB2[:, k * 128:(k + 1) * 128], d_["pbf"][:, k * 128:(k + 1) * 128], identB[:], [d_["r_pbf"], r_identB], [PR[6]])
                pT, r_pT = stg_rot.next()
                cp("act", pT[:, 0:256], PB2[:, 0:256], [PR[6]], [r_pT])
                d_.update(nT=nT, r_nT=r_nT, pT=pT, r_pT=r_pT)

            def d_c(n):
                d_ = dst.pop(n)
                r0 = tok0 + n * 128
                xw, r_xw, nT, r_nT, pT, r_pT = d_["xw"], d_["r_xw"], d_["nT"], d_["r_nT"], d_["pT"], d_["r_pT"]
                for hf in range(2):
                    hs_ = slice(hf * 512, (hf + 1) * 512)
                    for k in range(8):
                        mm(PS[0 + 2 * hf][:, :], nT[:, k, :], wpg_sb[:, k, hs_], k == 0, k == 7, [r_nT, r_wpg], [PR[0 + 2 * hf]])
                    for k in range(2):
                        mm(PS[1 + 2 * hf][:, :], pT[:, k * 128:(k + 1) * 128], wpp_sb[:, k, hs_], k == 0, k == 1, [r_pT, r_wpp], [PR[1 + 2 * hf]])
                    th, r_th = e512_rot.next()
                    act(th[:], PS[0 + 2 * hf][:, :], AF.Tanh, [PR[0 + 2 * hf]], [r_th], scale=0.5)
                    ts("dve", th[:], th[:], 0.5, 0.5, ALU.mult, ALU.add, [r_th], [r_th])
                    tt("dve", th[:], th[:], PS[1 + 2 * hf][:, :], ALU.mult, [r_th, PR[1 + 2 * hf]], [r_th])
                    tt("dve", xw[:, hs_], xw[:, hs_], th[:], ALU.add, [r_xw, r_th], [r_xw])
                s1, r_s1 = norm_stats(xw[:], r_xw)
                yt, r_yt = yt_rot.next()
                stt(yt[:], xw[:], s1[:, 1:2], gfin[:], ALU.mult, ALU.mult, [r_xw, r_s1, r_gfin], [r_yt])
                stq(out_d[r0:r0 + 128, :], yt[:], [r_yt], [r_out], stream="sto")
            for t in range(NT + 4):
                if t < NT:
                    d_0(t)
                if 0 <= t - 2 < NT:
                    d_a(t - 2)
                if 0 <= t - 3 < NT:
                    d_b(t - 3)
                if 0 <= t - 4 < NT:
                    d_c(t - 4)
        barrier()
        kb.emit()
    nc.dbg_names = dbg_names
    nc.arena_hw = arena_hw[0]
    return nc


WKEYS = ['g_mix', 'w_in', 'b_forget', 'conv_w', 'a_log', 'dt_bias', 'g_onorm', 'w_o_fox', 'w_o_delta', 'w_out', 'g_ffn',
         'w_group', 'b_group', 'w_router', 'b_router', 'w_gate', 'w_up', 'w_down', 'g_ple', 'w_ple_gate', 'w_ple_proj']
_NC_CACHE = {}


def kernel(**inputs):
    x = np.ascontiguousarray(np.asarray(inputs['x'], dtype=np.float32))
    p = np.ascontiguousarray(np.asarray(inputs['p'], dtype=np.float32))
    B, S, _ = x.shape
    nseq = B // NCORES
    shared = {k: np.ascontiguousarray(np.asarray(inputs[k], dtype=np.float32)[0]) for k in WKEYS}
    shared['g_final'] = np.ascontiguousarray(np.asarray(inputs['g_final'], dtype=np.float32))
    shared.update(host_consts())
    key = (nseq, S)
    if key not in _NC_CACHE:
        _NC_CACHE[key] = build(nseq, S)
    nc = _NC_CACHE[key]
    in_maps = []
    for c in range(NCORES):
        m = dict(shared)
        m['x'] = x[c * nseq:(c + 1) * nseq].reshape(nseq * S, D)
        m['p'] = p[0, c * nseq:(c + 1) * nseq].reshape(nseq * S, 256)
        in_maps.append(m)
    res = run_bass_kernel_spmd(nc, in_maps, core_ids=list(range(NCORES)))
    out = np.concatenate([np.asarray(r['out']).reshape(nseq, S, D) for r in res.results], axis=0)
    return out.astype(np.float32)
```

```python
import numpy as np
from contextlib import ExitStack
import concourse.bass as bass
import concourse.mybir as mybir
from concourse.bass_utils import run_bass_kernel_spmd

F32 = mybir.dt.float32
BF16 = mybir.dt.bfloat16
AF = mybir.ActivationFunctionType
ALU = mybir.AluOpType

NCORES = 8
D = 1024
EPS = 1e-6
NEG = -60000.0
SAME_ENGINE_SYNC = True
FOX_FILL = False
FOX_K = 128
FOX_M = 128
FOX_FILL_N = 256


class Res:
    __slots__ = ("name", "w", "r", "nowaw")

    def __init__(self, name="", nowaw=False):
        self.name = name
        self.nowaw = nowaw
        self.w = {}
        self.r = {}


class KB:
    ENGS = ("pe", "act", "dve", "pool", "sp")
    NDSEM = 20

    def __init__(self, nc, stack):
        self.nc = nc
        self.stack = stack
        self.sem = {}
        self.count = {}
        self.prog = {e: [] for e in self.ENGS}
        self.waited = {e: {} for e in self.ENGS}
        for e in self.ENGS:
            self.sem[e] = stack.enter_context(nc.semaphore("s_" + e))
            self.count[e] = 0
        self.dpool = {}
        self.dnext = {}
        for q in ("sp", "pool"):
            self.dpool[q] = []
            self.dnext[q] = 0
            for i in range(self.NDSEM):
                k = f"d{q}{i}"
                self.sem[k] = stack.enter_context(nc.semaphore(k))
                self.count[k] = 0
                self.dpool[q].append(k)

    def res(self, name=""):
        return Res(name)

    def _need(self, eng, reads, writes, extra=()):
        need = {}

        def add(k, v):
            if need.get(k, 0) < v:
                need[k] = v
        for k, v in extra:
            add(k, v)
        for r in reads:
            for k, v in r.w.items():
                add(k, v)
        for w in writes:
            if not w.nowaw:
                for k, v in w.w.items():
                    add(k, v)
            for k, v in w.r.items():
                add(k, v)
        out = []
        for k, v in need.items():
            if k == eng and (not SAME_ENGINE_SYNC or eng == "pe" or v > self.count[eng]):
                continue
            if self.waited[eng].get(k, 0) >= v:
                continue
            self.waited[eng][k] = v
            out.append((k, v))
        return out

    def _commit(self, ticket, reads, writes):
        k, v = ticket
        for w in writes:
            if w.w.get(k, 0) < v:
                w.w[k] = v
            w.r = {}
        for r in reads:
            if r.r.get(k, 0) < v:
                r.r[k] = v

    def op(self, eng, fn, reads=(), writes=(), sig=True):
        for k, v in self._need(eng, reads, writes):
            self.prog[eng].append(("wait", k, v))
        if sig:
            self.count[eng] += 1
            ticket = (eng, self.count[eng])
            self.prog[eng].append(("op", fn, eng, 1))
        else:
            ticket = (eng, self.count[eng] + 1)
            self.prog[eng].append(("op", fn, None, 0))
        self._commit(ticket, reads, writes)

    def dma(self, queue, stream, fn, reads=(), writes=()):
        pool = self.dpool[queue]
        k = pool[self.dnext[queue] % len(pool)]
        self.dnext[queue] += 1
        extra = [(k, self.count[k])] if self.count[k] > 0 else []
        for kk, v in self._need(queue, reads, writes, extra):
            self.prog[queue].append(("wait", kk, v))
        self.count[k] += 16
        ticket = (k, self.count[k])
        self.prog[queue].append(("op", fn, k, 16))
        self._commit(ticket, reads, writes)

    def emit(self):
        nc = self.nc
        with nc.Block() as block:
            def run(engname, e):
                for item in self.prog[engname]:
                    if item[0] == "wait":
                        e.wait_ge(self.sem[item[1]], item[2])
                    else:
                        _, fn, semk, inc = item
                        ins = fn(e)
                        if semk is not None:
                            ins.then_inc(self.sem[semk], inc)

            @block.tensor
            def _(e):
                run("pe", e)

            @block.scalar
            def _(e):
                run("act", e)

            @block.vector
            def _(e):
                run("dve", e)

            @block.gpsimd
            def _(e):
                run("pool", e)

            @block.sync
            def _(e):
                run("sp", e)


def host_consts():
    c = {}
    c["identF"] = np.eye(128, dtype=np.float32)
    p = np.arange(128)[:, None]
    f = np.arange(128)[None, :]
    c["mask_u"] = np.where(f >= p, 0.0, NEG).astype(np.float32)
    c["mask_l"] = np.where(f >= p, -NEG, 0.0).astype(np.float32)
    sel = np.zeros((128, 4, 128), np.float32)
    for h in range(4):
        sel[32 + h, h, :] = 1.0
    c["selA"] = sel.reshape(128, 512)
    c["tstart"] = np.tile((np.arange(160, dtype=np.float32) * 128.0)[None, :], (128, 1))
    c["pcol"] = np.arange(128, dtype=np.float32)[:, None]
    c["ustrict"] = (p < f).astype(np.float32)
    return c


def build(NSEQ, S, stages=99, debug=False):
    T = NSEQ * S
    NT = S // 128
    NG = S // 512
    nc = bass.Bass("TRN2", target_bir_lowering=False)

    def din(name, shape, dt=F32):
        return nc.dram_tensor(name, list(shape), dt, kind="ExternalInput").ap()

    def dscr(name, shape, dt=BF16):
        return nc.dram_tensor(name, list(shape), dt, kind="ExternalOutput" if debug else "Internal").ap()

    x_d = din("x", [T, D])
    p_d = din("p", [T, 256])
    g_mix = din("g_mix", [D]); w_in = din("w_in", [D, 5648]); b_forget = din("b_forget", [8])
    conv_w = din("conv_w", [4, 1536]); a_log = din("a_log", [4]); dt_bias = din("dt_bias", [4])
    g_onorm = din("g_onorm", [128]); w_o_fox = din("w_o_fox", [512, D]); w_o_delta = din("w_o_delta", [512, D])
    w_out = din("w_out", [D, D]); g_ffn = din("g_ffn", [D]); w_group = din("w_group", [D, 4])
    b_group = din("b_group", [4]); w_router = din("w_router", [D, 32]); b_router = din("b_router", [32])
    w_gate = din("w_gate", [32, D, 256]); w_up = din("w_up", [32, D, 256]); w_down = din("w_down", [32, 256, D])
    g_ple = din("g_ple", [D]); w_ple_gate = din("w_ple_gate", [D, D]); w_ple_proj = din("w_ple_proj", [256, D])
    g_final = din("g_final", [D])
    identF_d = din("identF", [128, 128]); mask_u_d = din("mask_u", [128, 128]); mask_l_d = din("mask_l", [128, 128])
    selA_d = din("selA", [128, 512])
    out_d = nc.dram_tensor("out", [T, D], F32, kind="ExternalOutput").ap()

    WX = dscr("WX", [32 * 128, 6144])
    NSLT_ = (2 * S) // 128 + 32
    Xg = [dscr(f"Xg{s}", [NSLT_ * 128, D]) for s in range(NSEQ)]
    Yg = [dscr(f"Yg{s}", [NSLT_ * 128, D]) for s in range(NSEQ)]
    X1 = [dscr(f"X1{s}", [S, D], F32) for s in range(NSEQ)]
    tstart_d = din("tstart", [128, 160]); pcol_d = din("pcol", [128, 1]); ustrict_d = din("ustrict", [128, 128])
    QT = [dscr(f"QT{s}", [512, S]) for s in range(NSEQ)]
    KT = [dscr(f"KT{s}", [512, S]) for s in range(NSEQ)]
    GQKV = [dscr(f"GQKV{s}", [1536, S]) for s in range(NSEQ)]
    DZ = [dscr(f"DZ{s}", [512, S]) for s in range(NSEQ)]
    THF = [dscr(f"THF{s}", [D, S]) for s in range(NSEQ)]
    THD = [dscr(f"THD{s}", [D, S]) for s in range(NSEQ)]
    AUGQ = [dscr(f"AUGQ{s}", [8, 6, S]) for s in range(NSEQ)]
    AUGK = [dscr(f"AUGK{s}", [8, 6, S]) for s in range(NSEQ)]
    ATs = [dscr(f"AT{s}", [512, S]) for s in range(NSEQ)]
    OGs = [dscr(f"OG{s}", [512, S]) for s in range(NSEQ)]
    GTs = [dscr(f"GT{s}", [32, S]) for s in range(NSEQ)]

    with ExitStack() as st:
        kb = KB(nc, st)

        ARENA_BYTES = 209920
        arena_t = st.enter_context(nc.sbuf_tensor("arena", [128, ARENA_BYTES // 4], F32))
        arena_off = [0]
        arena_hw = [0]

        def sb(name, shape, dt):
            esz = 2 if dt == BF16 else 4
            n = 1
            for d_ in shape[1:]:
                n *= d_
            nbytes = (n * esz + 31) // 32 * 32
            o = arena_off[0]
            assert o + nbytes <= ARENA_BYTES, (name, o, nbytes)
            arena_off[0] = o + nbytes
            arena_hw[0] = max(arena_hw[0], o + nbytes)
            v = arena_t[:, o // 4:(o + nbytes) // 4]
            if dt != F32:
                v = v.bitcast(dt)
            v = v[:, 0:n]
            if len(shape) == 3:
                v = v.rearrange("p (a b) -> p a b", a=shape[1])
            elif len(shape) == 4:
                v = v.rearrange("p (a b c) -> p a b c", a=shape[1], b=shape[2])
            return v

        def R(name=""):
            return kb.res(name)

        def mm(out, lhsT, rhs, start, stop, reads, writes, sig=None):
            if sig is None:
                sig = stop
            kb.op("pe", lambda e: e.matmul(out, lhsT=lhsT, rhs=rhs, start=start, stop=stop),
                  reads=reads, writes=writes, sig=sig)

        def tr(out, in_, ident, reads, writes):
            kb.op("pe", lambda e: e.transpose(out, in_, ident), reads=reads, writes=writes)

        def act(out, in_, func, reads, writes, bias=None, scale=None, accum=None):
            kw = {}
            if bias is not None:
                kw["bias"] = bias
            if scale is not None:
                kw["scale"] = scale
            if accum is not None:
                kw["accum_out"] = accum
            kb.op("act", lambda e: e.activation(out=out, in_=in_, func=func, **kw), reads=reads, writes=writes)

        def ts(eng, out, in0, s1, s2, op0, op1, reads, writes):
            if s2 is None:
                kb.op(eng, lambda e: e.tensor_scalar(out=out, in0=in0, scalar1=s1, scalar2=None, op0=op0),
                      reads=reads, writes=writes)
            else:
                kb.op(eng, lambda e: e.tensor_scalar(out=out, in0=in0, scalar1=s1, scalar2=s2, op0=op0, op1=op1),
                      reads=reads, writes=writes)

        def tt(eng, out, in0, in1, op, reads, writes):
            kb.op(eng, lambda e: e.tensor_tensor(out=out, in0=in0, in1=in1, op=op), reads=reads, writes=writes)

        def stt(out, in0, scalar, in1, op0, op1, reads, writes, eng="dve"):
            kb.op(eng, lambda e: e.scalar_tensor_tensor(out=out, in0=in0, scalar=scalar, in1=in1, op0=op0, op1=op1),
                  reads=reads, writes=writes)

        def cp(eng, out, in_, reads, writes):
            if eng == "act":
                kb.op("act", lambda e: e.copy(out, in_), reads=reads, writes=writes)
            else:
                kb.op(eng, lambda e: e.tensor_copy(out, in_), reads=reads, writes=writes)

        def ms(eng, ap, val, writes):
            kb.op(eng, lambda e: e.memset(ap, val), writes=writes)

        def ld(out, in_, writes, reads=(), stream="ld", q="sp", nonc=False):
            if nonc:
                kb.dma(q, stream, lambda e: e.dma_start(out=out, in_=in_, allow_slow_non_contiguous=True),
                       reads=reads, writes=writes)
            else:
                kb.dma(q, stream, lambda e: e.dma_start(out=out, in_=in_), reads=reads, writes=writes)

        def ldc(out, in_, writes, reads=(), stream="ldc", nonc=False):
            ld(out, in_, writes, reads, stream=stream, q="pool", nonc=nonc)

        def stq(out, in_, reads, writes=(), stream="st"):
            kb.dma("sp", stream, lambda e: e.dma_start(out=out, in_=in_), reads=reads, writes=writes)

        dbg_names = []

        def dbg(name, ap, reads, dt=F32):
            if not debug:
                return
            shp = list(ap.shape)
            d_ = nc.dram_tensor("dbg_" + name, shp, dt, kind="ExternalOutput").ap()
            dbg_names.append("dbg_" + name)
            idx = tuple(slice(None) for _ in shp)
            stq(d_[idx], ap, reads)

        class Rot:
            def __init__(self, name, shape, dt, n):
                self.t = [sb(f"{name}{i}", shape, dt) for i in range(n)]
                self.r = [R(f"{name}{i}") for i in range(n)]
                self.i = 0
                self.n = n

            def next(self):
                k = self.i % self.n
                self.i += 1
                return self.t[k], self.r[k]

        def barrier():
            keys = list(kb.count.keys())
            for e_ in KB.ENGS:
                for k in keys:
                    v = kb.count[k]
                    if v > 0 and kb.waited[e_].get(k, 0) < v and k != e_:
                        kb.waited[e_][k] = v
                        kb.prog[e_].append(("wait", k, v))

        PS = [st.enter_context(nc.psum_tensor(f"ps{i}", [128, 512], F32)) for i in range(7)]
        PR = [R(f"ps{i}") for i in range(7)]
        PB = st.enter_context(nc.psum_tensor("psb", [128, 1024], BF16))
        PBR = R("psb")

        identF = sb("identF", [128, 128], F32); r_identF = R()
        identB = sb("identB", [128, 128], BF16); r_identB = R()
        ident4F = sb("ident4F", [128, 512], F32); r_ident4F = R()
        masku = sb("masku", [128, 128], BF16); r_masku = R()
        maskl = sb("maskl", [128, 128], BF16); r_maskl = R()
        selA = sb("selA", [128, 512], F32); r_selA = R()
        onesF = sb("onesF", [128, 128], F32); r_onesF = R()
        onesB = sb("onesB", [128, 128], BF16); r_onesB = R()
        nhalf = sb("nhalf", [128, 512], F32); r_nhalf = R()
        onesS = sb("onesS", [128, 1024], BF16); r_onesS = R()
        ld(identF[:], identF_d[:, :], [r_identF])
        ldc(identB[:], identF_d[:, :], [r_identB])
        for h in range(4):
            ld(ident4F[:, h * 128:(h + 1) * 128], identF_d[:, :], [r_ident4F])
        ldc(masku[:], mask_u_d[:, :], [r_masku])
        ldc(maskl[:], mask_l_d[:, :], [r_maskl])
        ld(selA[:], selA_d[:, :], [r_selA])
        ms("dve", onesF[:], 1.0, [r_onesF])
        ms("dve", onesB[:], 1.0, [r_onesB])
        ms("dve", nhalf[:], -0.5, [r_nhalf])
        ms("dve", onesS[:], 1.0, [r_onesS])

        def colvec(name, src, nch):
            t = sb(name, [128, nch], F32); r = R()
            ld(t[:], src.rearrange("(c p) -> p c", p=128), [r], nonc=True)
            return t, r
        gmixT, r_gmixT = colvec("gmixT", g_mix, 8)
        gffnT, r_gffnT = colvec("gffnT", g_ffn, 8)
        gpleT, r_gpleT = colvec("gpleT", g_ple, 8)
        gonT, r_gonT = colvec("gonT", g_onorm, 1)
        gfin = sb("gfin", [128, D], F32); r_gfin = R()
        ld(gfin[:], g_final[None, :].to_broadcast([128, D]), [r_gfin])
        cw = sb("cw", [128, 12, 4], F32); r_cw = R()
        for j in range(4):
            ld(cw[:, :, j], conv_w[j, :].rearrange("(c p) -> p c", p=128), [r_cw], nonc=True)
        wrt_sb = sb("wrt", [128, 8, 36], BF16); r_wrt = R()
        ldc(wrt_sb[:, :, 0:4], w_group.rearrange("(k p) c -> p k c", p=128), [r_wrt], nonc=True)
        ldc(wrt_sb[:, :, 4:36], w_router.rearrange("(k p) c -> p k c", p=128), [r_wrt], nonc=True)
        brt = sb("brt", [128, 36], F32); r_brt = R()
        ld(brt[:, 0:4], b_group[None, :].to_broadcast([128, 4]), [r_brt])
        ld(brt[:, 4:36], b_router[None, :].to_broadcast([128, 32]), [r_brt])
        tots = sb("tots", [128, max(NT, 8)], F32); r_tots = R()
        C_FF, C_QKV, C_DA, C_DB, C_DZ, C_GF, C_GD = 1536, 1544, 3080, 3084, 3088, 3600, 4624

        def wcols(c0, n):
            return w_in[:, c0:c0 + n].rearrange("(k p) c -> p k c", p=128)

        def wres(name, src, kch, ncol):
            t = sb(name, [128, kch, ncol], BF16); r = R()
            ldc(t[:], src.rearrange("(k p) c -> p k c", p=128), [r])
            return t, r
        prmA = sb("prmA", [128, 2], F32); r_prmA = R()
        prmB = sb("prmB", [128, 2], F32); r_prmB = R()
        ms("dve", prmA[:], 0.0, [r_prmA])
        ms("dve", prmB[:], 0.0, [r_prmB])
        ld(prmA[0:8, 0:1], b_forget[:, None], [r_prmA], nonc=True)
        for o in (32, 64, 96):
            ld(prmA[o:o + 4, 0:1], dt_bias[:, None], [r_prmA], nonc=True)
            ld(prmA[o:o + 4, 1:2], a_log[:, None], [r_prmA], nonc=True)
        ld(prmB[64:68, 0:1], dt_bias[:, None], [r_prmB], nonc=True)
        ld(prmB[64:68, 1:2], a_log[:, None], [r_prmB], nonc=True)
        ts("dve", prmA[0:8, 0:1], prmA[0:8, 0:1], -1.0, None, ALU.mult, None, [r_prmA], [r_prmA])
        act(prmA[:, 1:2], prmA[:, 1:2], AF.Exp, [r_prmA], [r_prmA])
        ts("dve", prmA[:, 1:2], prmA[:, 1:2], -1.0, None, ALU.mult, None, [r_prmA], [r_prmA])
        act(prmB[:, 1:2], prmB[:, 1:2], AF.Exp, [r_prmB], [r_prmB])
        ts("dve", prmB[:, 1:2], prmB[:, 1:2], -1.0, None, ALU.mult, None, [r_prmB], [r_prmB])

        r_wexp = Res("wexp", nowaw=True)
        if stages >= 7:
            for e_ in range(32):
                rows = WX[e_ * 128:(e_ + 1) * 128, :]
                gu = rows[:, 0:4096].rearrange("p (k t f) -> p k t f", k=8, t=2)
                ldc(gu[:, :, 0, :], w_gate[e_].rearrange("(k p) f -> p k f", p=128), [r_wexp], stream="wx")
                ldc(gu[:, :, 1, :], w_up[e_].rearrange("(k p) f -> p k f", p=128), [r_wexp], stream="wx")
                ldc(rows[:, 4096:6144].rearrange("p (c f) -> p c f", c=2), w_down[e_].rearrange("(c p) f -> p c f", p=128),
                    [r_wexp], stream="wx")

        def RD():
            return Res("dram", nowaw=True)
        r_QT = [RD() for _ in range(NSEQ)]; r_KT = [RD() for _ in range(NSEQ)]; r_GQKV = [RD() for _ in range(NSEQ)]
        r_DZ = [RD() for _ in range(NSEQ)]; r_THF = [RD() for _ in range(NSEQ)]; r_THD = [RD() for _ in range(NSEQ)]
        r_aug = [RD() for _ in range(NSEQ)]; r_AT = [RD() for _ in range(NSEQ)]; r_OG = [RD() for _ in range(NSEQ)]
        r_GT = [RD() for _ in range(NSEQ)]; r_out = RD()
        r_Xg = [RD() for _ in range(NSEQ)]; r_Yg = [RD() for _ in range(NSEQ)]; r_X1 = [RD() for _ in range(NSEQ)]
        r_Xgz = [RD() for _ in range(NSEQ)]
        tstart = sb("tstart", [128, 160], F32); r_tstart = R()
        pcol = sb("pcol", [128, 1], F32); r_pcol = R()
        ustrict = sb("ustrict", [128, 128], BF16); r_ustrict = R()
        ld(tstart[:], tstart_d[:, :], [r_tstart])
        ld(pcol[:], pcol_d[:, :], [r_pcol])
        ldc(ustrict[:], ustrict_d[:, :], [r_ustrict])

        junk = sb("junk", [128, D], BF16); r_junk = R()
        st1_rot = Rot("st1", [128, 2], F32, 10)
        MARK0 = arena_off[0]

        def norm_stats(src, r_src):
            s1, r_s1 = st1_rot.next()
            ms("dve", s1[:, 0:1], 0.0, [r_s1])
            act(junk[:], src, AF.Square, [r_src, r_s1], [r_junk, r_s1], accum=s1[:, 0:1])
            ts("dve", s1[:, 0:1], s1[:, 0:1], 1.0 / D, EPS, ALU.mult, ALU.add, [r_s1], [r_s1])
            tt("pool", s1[:, 1:2], s1[:, 0:1], nhalf[:, 0:1], ALU.pow, [r_s1, r_nhalf], [r_s1])
            return s1, r_s1

        def norm_transpose(src, r_src, gT, r_gT, dst_fn, r_dst, xn_rot):
            s1, r_s1 = norm_stats(src, r_src)
            xn, r_xn = xn_rot.next()
            ts("dve", xn[:], src, s1[:, 1:2], None, ALU.mult, None, [r_src, r_s1], [r_xn])
            for c in range(8):
                tr(PB[:, c * 128:(c + 1) * 128], xn[:, c * 128:(c + 1) * 128], identB[:], [r_xn, r_identB], [PBR])
            for c in range(8):
                if c % 2 == 0:
                    act(dst_fn(c), PB[:, c * 128:(c + 1) * 128], AF.Copy, [PBR, r_gT], [r_dst], scale=gT[:, c:c + 1])
                else:
                    ts("dve", dst_fn(c), PB[:, c * 128:(c + 1) * 128], gT[:, c:c + 1], None, ALU.mult, None,
                       [PBR, r_gT], [r_dst])

        scale_q = 64 ** -0.5
        scale_gq = 128 ** -0.5

        for s in range(NSEQ):
            tok0 = s * S
            barrier()
            arena_off[0] = MARK0
            ZA = sb("ZA", [128, S], F32); r_ZA = R("ZA")
            ZB = sb("ZB", [128, S], F32); r_ZB = R("ZB")
            MARK1 = arena_off[0]
            Vall = sb("Vall", [128, NT, 8, 65], BF16); r_V = [R(f"V{i}") for i in range(NT)]
            MARK2 = arena_off[0]
            ms("dve", Vall[:], 1.0, r_V)
            hT = sb("hT", [128, 8, S], BF16)
            r_hT = [R(f"hT{i}") for i in range(NT)]
            wv_sb, r_wv = wres("wv", w_in[:, 1024:1536], 8, 512)
            wgA = sb("wgA", [128, 8, 128], BF16); r_wgA = R()
            wgB = sb("wgB", [128, 8, 128], BF16); r_wgB = R()
            ms("dve", wgA[:], 0.0, [r_wgA])
            ms("dve", wgB[:], 0.0, [r_wgB])
            ldc(wgA[:, :, 0:8], wcols(C_FF, 8), [r_wgA], nonc=True)
            for o in (32, 64, 96):
                ldc(wgA[:, :, o:o + 4], wcols(C_DA, 4), [r_wgA], nonc=True)
            ldc(wgB[:, :, 0:4], wcols(C_DB, 4), [r_wgB], nonc=True)
            ldc(wgB[:, :, 32:36], wcols(C_DB, 4), [r_wgB], nonc=True)
            ldc(wgB[:, :, 64:68], wcols(C_DA, 4), [r_wgB], nonc=True)
            xt_rot = Rot("xt", [128, D], F32, 2)
            xn_rot = Rot("xn", [128, D], BF16, 4)
            stg_rot = Rot("stg", [128, 512], BF16, 6)
            f32_rot = Rot("f32w", [128, 520], F32, 5)
            win_rot = Rot("win", [128, 8, 128], BF16, 3)
            p1st = {}

            def p1_a(i):
                xt, r_xt = xt_rot.next()
                ld(xt[:], x_d[tok0 + i * 128: tok0 + (i + 1) * 128, :], [r_xt])
                s1, r_s1 = norm_stats(xt[:], r_xt)
                xn, r_xn = xn_rot.next()
                ts("dve", xn[:], xt[:], s1[:, 1:2], None, ALU.mult, None, [r_xt, r_s1], [r_xn])
                p1st[i] = (xn, r_xn)

            def p1_b(i):
                xn, r_xn = p1st.pop(i)
                for c in range(8):
                    tr(PB[:, c * 128:(c + 1) * 128], xn[:, c * 128:(c + 1) * 128], identB[:], [r_xn, r_identB], [PBR])
                for c in range(8):
                    dstc = hT[:, c, i * 128:(i + 1) * 128]
                    if c % 2 == 0:
                        act(dstc, PB[:, c * 128:(c + 1) * 128], AF.Copy, [PBR, r_gmixT], [r_hT[i]], scale=gmixT[:, c:c + 1])
                    else:
                        ts("dve", dstc, PB[:, c * 128:(c + 1) * 128], gmixT[:, c:c + 1], None, ALU.mult, None,
                           [PBR, r_gmixT], [r_hT[i]])
            for t in range(NT + 2):
                if t < NT:
                    p1_a(t)
                if 0 <= t - 2 < NT:
                    p1_b(t - 2)
            if stages < 1.1:
                continue
            for i in range(NT):
                b = i % 2
                for k in range(8):
                    mm(PS[b][:, :], hT[:, k, i * 128:(i + 1) * 128], wv_sb[:, k, :], k == 0, k == 7,
                       [r_hT[i], r_wv], [PR[b]])
                cp("act" if i % 2 else "dve", Vall[:, i, :, 0:64],
                   PS[b][:, :].rearrange("p (h d) -> p h d", h=8), [PR[b]], [r_V[i]])
            if stages < 1.2:
                continue
            for g in range(NG):
                gt = [r_hT[4 * g + j] for j in range(4)]
                for (wg_, r_wg_, Z, r_Z, b) in ((wgA, r_wgA, ZA, r_ZA, 2), (wgB, r_wgB, ZB, r_ZB, 3)):
                    for k in range(8):
                        mm(PS[b][:, :], wg_[:, k, :], hT[:, k, g * 512:(g + 1) * 512], k == 0, k == 7,
                           gt + [r_wg_], [PR[b]])
                    cp("act", Z[:, g * 512:(g + 1) * 512], PS[b][:, :], [PR[b]], [r_Z])
            if stages < 1.3:
                continue
            chunks = []
            for c in range(4):
                chunks.append((c * 128, "q", c))
            for c in range(4):
                chunks.append((512 + c * 128, "k", c))
            for c in range(12):
                chunks.append((C_QKV + c * 128, "gdn", c))
            for c in range(4):
                chunks.append((C_DZ + c * 128, "dz", c))
            for c in range(8):
                chunks.append((C_GF + c * 128, "gf", c))
            for c in range(8):
                chunks.append((C_GD + c * 128, "gd", c))
            bsel = 0
            p2cnt = [0]
            p2pend = [None]
            if stages < 1.4:
                chunks = chunks[0:8]
            elif stages < 1.5:
                chunks = chunks[0:20]
            for (c0, kind, ci) in chunks:
                wt, r_wt = win_rot.next()
                ldc(wt[:], wcols(c0, 128), [r_wt], stream="ldw")
                halo, r_halo = None, None
                for g in range(NG):
                    gt = [r_hT[4 * g + j] for j in range(4)]
                    b = 4 + (bsel % 2)
                    bsel += 1
                    for k in range(8):
                        mm(PS[b][:, :], wt[:, k, :], hT[:, k, g * 512:(g + 1) * 512], k == 0, k == 7,
                           gt + [r_wt], [PR[b]])
                    cols = slice(g * 512, (g + 1) * 512)
                    if not (kind == "gdn" and ci < 8) and p2pend[0] is not None:
                        p2pend[0]()
                        p2pend[0] = None
                    stg, r_stg = stg_rot.next()
                    if kind == "q":
                        act(stg[:], PS[b][:, :], AF.Copy, [PR[b]], [r_stg], scale=scale_q)
                        stq(QT[s][ci * 128:(ci + 1) * 128, cols], stg[:], [r_stg], [r_QT[s]])
                    elif kind == "k":
                        cp("dve", stg[:], PS[b][:, :], [PR[b]], [r_stg])
                        stq(KT[s][ci * 128:(ci + 1) * 128, cols], stg[:], [r_stg], [r_KT[s]])
                    elif kind == "dz":
                        act(stg[:], PS[b][:, :], AF.Silu, [PR[b]], [r_stg])
                        stq(DZ[s][ci * 128:(ci + 1) * 128, cols], stg[:], [r_stg], [r_DZ[s]])
                    elif kind in ("gf", "gd"):
                        act(stg[:], PS[b][:, :], AF.Tanh, [PR[b]], [r_stg], scale=0.5)
                        dst, r_d = (THF[s], r_THF[s]) if kind == "gf" else (THD[s], r_THD[s])
                        stq(dst[ci * 128:(ci + 1) * 128, cols], stg[:], [r_stg], [r_d])
                    else:
                        zb, r_zb = f32_rot.next()
                        if g == 0:
                            ms("dve", zb[:, 0:3], 0.0, [r_zb])
                        else:
                            cp("dve", zb[:, 0:3], halo[:, 512:515], [r_halo], [r_zb])
                        cp("act", zb[:, 3:515], PS[b][:, :], [PR[b]], [r_zb])
                        halo, r_halo = zb, r_zb
                        cv, r_cv = f32_rot.next()
                        ts("dve", cv[:, 0:512], zb[:, 3:515], cw[:, ci, 3:4], None, ALU.mult, None, [r_zb, r_cw], [r_cv])
                        for j in range(3):
                            stt(cv[:, 0:512], zb[:, j:j + 512], cw[:, ci, j:j + 1], cv[:, 0:512], ALU.mult, ALU.add,
                                [r_zb, r_cw, r_cv], [r_cv])
                        if ci >= 8:
                            act(stg[:], cv[:, 0:512], AF.Silu, [r_cv], [r_stg])
                            stq(GQKV[s][ci * 128:(ci + 1) * 128, cols], stg[:], [r_stg], [r_GQKV[s]])
                        else:
                            act(cv[:, 0:512], cv[:, 0:512], AF.Silu, [r_cv], [r_cv])
                            sq, r_sq = stg_rot.next()
                            act(sq[:], cv[:, 0:512], AF.Square, [r_cv], [r_sq])
                            pss = p2cnt[0] % 2
                            p2cnt[0] += 1
                            for j in range(4):
                                mm(PS[pss][:, j:j + 1], sq[:, j * 128:(j + 1) * 128], onesB[:, 0:1], True, True,
                                   [r_onesB, r_sq], [PR[pss]], sig=(j == 3))

                            def g2(cv=cv, r_cv=r_cv, stg=stg, r_stg=r_stg, pss=pss, ci=ci, cols=cols):
                                rw, r_rw = f32_rot.next()
                                ts("dve", rw[:, 0:4], PS[pss][:, 0:4], EPS, None, ALU.add, None, [PR[pss]], [r_rw])
                                tt("pool", rw[:, 4:8], rw[:, 0:4], nhalf[:, 0:4], ALU.pow, [r_rw, r_nhalf], [r_rw])
                                for j in range(4):
                                    ts("dve", rw[:, 8 + j * 128:8 + (j + 1) * 128], identF[:], rw[:, 4 + j:5 + j], None, ALU.mult, None,
                                       [r_identF, r_rw], [r_rw])
                                for j in range(4):
                                    mm(PS[6][:, j * 128:(j + 1) * 128], onesF[:], rw[:, 8 + j * 128:8 + (j + 1) * 128], True, True,
                                       [r_onesF, r_rw], [PR[6]], sig=(j == 3))
                                stt(stg[:], cv[:, 0:512], scale_gq if ci < 4 else 1.0, PS[6][:, :], ALU.mult, ALU.mult,
                                    [r_cv, PR[6]], [r_stg])
                                stq(GQKV[s][ci * 128:(ci + 1) * 128, cols], stg[:], [r_stg], [r_GQKV[s]])
                            if p2pend[0] is not None:
                                p2pend[0]()
                            p2pend[0] = g2
            if p2pend[0] is not None:
                p2pend[0]()
                p2pend[0] = None
            if stages < 3:
                continue
            barrier()
            arena_off[0] = MARK2
            augb_rot = Rot("augb", [128, 6, 1024], BF16, 1)
            augf_rot = Rot("augf", [128, 2, 1024], F32, 1)
            def softplus_rows(Z, r_Z, prm, r_prm, lo, n, escale):
                rows = slice(lo, lo + n)
                act(Z[rows, :], Z[rows, :], AF.Exp, [r_Z, r_prm], [r_Z], bias=prm[rows, 0:1], scale=escale)
                act(Z[rows, :], Z[rows, :], AF.Ln, [r_Z], [r_Z], bias=1.0)
                ts("dve", Z[rows, :], Z[rows, :], prm[rows, 1:2], None, ALU.mult, None, [r_Z, r_prm], [r_Z])
            softplus_rows(ZA, r_ZA, prmA, r_prmA, 0, 8, -1.0)
            for o in (32, 64, 96):
                softplus_rows(ZA, r_ZA, prmA, r_prmA, o, 4, 1.0)
            softplus_rows(ZB, r_ZB, prmB, r_prmB, 0, 4, -1.0)
            softplus_rows(ZB, r_ZB, prmB, r_prmB, 32, 4, -1.0)
            softplus_rows(ZB, r_ZB, prmB, r_prmB, 64, 4, 1.0)

            def scan(Z, r_Z, rows, c0, n, init):
                kb.op("dve", lambda e: e.tensor_tensor_scan(out=Z[rows, c0:c0 + n], data0=onesS[rows, 0:n],
                                                            data1=Z[rows, c0:c0 + n], initial=init,
                                                            op0=ALU.mult, op1=ALU.add),
                      reads=[r_Z, r_onesS], writes=[r_Z])
            AB = min(1024, S)
            for blk in range(S // AB):
                scan(ZA, r_ZA, slice(0, 8), blk * AB, AB, 0.0 if blk == 0 else ZA[0:8, blk * AB - 1:blk * AB])
            for blk in range(S // AB):
                cs = slice(blk * AB, (blk + 1) * AB)
                ab, r_ab = augb_rot.next()
                af, r_af = augf_rot.next()
                cp("dve", ab[0:8, 0, 0:AB], ZA[0:8, cs], [r_ZA], [r_ab])
                tt("dve", af[0:8, 0, 0:AB], ZA[0:8, cs], ab[0:8, 0, 0:AB], ALU.subtract, [r_ZA, r_ab], [r_af])
                cp("dve", ab[0:8, 1, 0:AB], af[0:8, 0, 0:AB], [r_af], [r_ab])
                tt("dve", af[0:8, 1, 0:AB], af[0:8, 0, 0:AB], ab[0:8, 1, 0:AB], ALU.subtract, [r_af, r_ab], [r_af])
                cp("dve", ab[0:8, 2, 0:AB], af[0:8, 1, 0:AB], [r_af], [r_ab])
                for j in range(3):
                    ts("dve", ab[0:8, 3 + j, 0:AB], ab[0:8, j, 0:AB], -1.0, None, ALU.mult, None, [r_ab], [r_ab])
                for j in range(3):
                    stq(AUGQ[s][:, j, cs], ab[0:8, j, 0:AB], [r_ab], [r_aug[s]])
                    stq(AUGQ[s][:, 3 + j, cs], onesS[0:8, 0:AB], [r_onesS], [r_aug[s]])
                    stq(AUGK[s][:, j, cs], onesS[0:8, 0:AB], [r_onesS], [r_aug[s]])
                    stq(AUGK[s][:, 3 + j, cs], ab[0:8, 3 + j, 0:AB], [r_ab], [r_aug[s]])
            for n in range(NT):
                c0 = n * 128
                for o in (32, 64, 96):
                    scan(ZA, r_ZA, slice(o, o + 4), c0, 128, 0.0)
                scan(ZB, r_ZB, slice(64, 68), c0, 128, 0.0)
                for o in (64, 96):
                    cp("dve", tots[o:o + 4, n:n + 1], ZA[o:o + 4, c0 + 127:c0 + 128], [r_ZA], [r_tots])
                ts("dve", ZA[64:68, c0:c0 + 128], ZA[64:68, c0:c0 + 128], -1.0, tots[64:68, n:n + 1], ALU.mult, ALU.add,
                   [r_ZA, r_tots], [r_ZA])
                ts("dve", ZA[96:100, c0:c0 + 128], ZA[96:100, c0:c0 + 128], 0.0, tots[96:100, n:n + 1], ALU.mult, ALU.add,
                   [r_ZA, r_tots], [r_ZA])
            act(ZA[64:68, :], ZA[64:68, :], AF.Exp, [r_ZA], [r_ZA])
            act(ZA[96:100, :], ZA[96:100, :], AF.Exp, [r_ZA], [r_ZA])
            act(ZB[0:4, :], ZB[0:4, :], AF.Exp, [r_ZB], [r_ZB])
            tt("dve", ZB[32:36, :], ZB[32:36, :], ZA[32:36, :], ALU.add, [r_ZB, r_ZA], [r_ZB])
            act(ZB[32:36, :], ZB[32:36, :], AF.Exp, [r_ZB], [r_ZB])
            ts("dve", ZB[64:68, :], ZB[64:68, :], -1.0, None, ALU.mult, None, [r_ZB], [r_ZB])
            if stages < 4:
                continue
            barrier()
            arena_off[0] = MARK2
            qa_rot = Rot("qa", [128, S], BF16, 2)
            ka_rot = Rot("ka", [128, S], BF16, 2)
            vh_rot = Rot("vh", [128, NT, 128], BF16, 2)
            for i_ in range(2):
                ms("pool", qa_rot.t[i_][64:128, :], 0.0, [qa_rot.r[i_]])
                ms("pool", ka_rot.t[i_][64:128, :], 0.0, [ka_rot.r[i_]])
                ms("pool", vh_rot.t[i_][:], 0.0, [vh_rot.r[i_]])
            pt_rot = Rot("pt", [128, 512], BF16, 3)
            f32_rot = Rot("f32w", [128, 520], F32, 4)
            stg_rot = Rot("stg", [128, 512], BF16, 4)
            fox_pend = []
            fox_cnt = [0]
            for h in range(8):
                QA, r_QA = qa_rot.next()
                KA, r_KA = ka_rot.next()
                ld(QA[0:64, 0:S], QT[s][h * 64:(h + 1) * 64, :], [r_QA], [r_QT[s]])
                ld(QA[64:70, 0:S], AUGQ[s][h], [r_QA], [r_aug[s]])
                ld(KA[0:64, 0:S], KT[s][h * 64:(h + 1) * 64, :], [r_KA], [r_KT[s]])
                ld(KA[64:70, 0:S], AUGK[s][h], [r_KA], [r_aug[s]])
                Vh, r_Vh = vh_rot.next()
                cp("pool", Vh[:, :, 0:65], Vall[:, :, h, :], r_V, [r_Vh])
                for g in range(NG):
                    last = 4 * g + 3
                    po = 2 + 2 * (fox_cnt[0] % 2)
                    fox_cnt[0] += 1
                    pbc = po + 1

                    def scores(j):
                        m = j - 4 * g
                        c0 = max(m, 0) * 128
                        N = 512 - c0
                        b = j % 2
                        mm(PS[b][:, 0:N], KA[0:FOX_K, j * 128:(j + 1) * 128], QA[0:FOX_K, g * 512 + c0:(g + 1) * 512],
                           True, m < 0, [r_KA, r_QA], [PR[b]], sig=(m < 0))
                        if m >= 0:
                            mm(PS[b][:, 0:128], identB[:], masku[:], False, True, [r_identB, r_masku], [PR[b]])
                    scores(0)
                    for j in range(last + 1):
                        m = j - 4 * g
                        c0 = max(m, 0) * 128
                        N = 512 - c0
                        b = j % 2
                        if j + 1 <= last:
                            scores(j + 1)
                        if j == min(1, last) and len(fox_pend) > 0:
                            fox_pend.pop(0)()
                        pt, r_pt = pt_rot.next()
                        act(pt[:, 0:N], PS[b][:, 0:N], AF.Exp, [PR[b]], [r_pt])
                        mm(PS[po][0:FOX_M, c0:512], Vh[:, j, 0:FOX_M], pt[:, 0:N], j == 0, j == last,
                           [r_Vh, r_pt], [PR[po]])
                        if FOX_FILL:
                            kb.op("pe", lambda e: e.matmul(PS[6][:, 0:FOX_FILL_N], lhsT=identB[:], rhs=onesS[:, 0:FOX_FILL_N], start=True, stop=True),
                                  sig=False)
                    def epilogue(po=po, pbc=pbc, g=g, h=h):
                        ob, r_ob = f32_rot.next()
                        cp("act", ob[0:65, 0:512], PS[po][0:65, :], [PR[po]], [r_ob])
                        mm(PS[pbc][0:64, :], onesF[64:65, 0:64], ob[64:65, 0:512], True, True, [r_onesF, r_ob], [PR[pbc]])
                        rb, r_rb = f32_rot.next()
                        kb.op("dve", lambda e, rb=rb, pbc=pbc: e.reciprocal(rb[0:64, 0:512], PS[pbc][0:64, :]), reads=[PR[pbc]], writes=[r_rb])
                        stg, r_stg = stg_rot.next()
                        tt("dve", stg[0:64, :], ob[0:64, 0:512], rb[0:64, 0:512], ALU.mult, [r_ob, r_rb], [r_stg])
                        stq(ATs[s][h * 64:(h + 1) * 64, g * 512:(g + 1) * 512], stg[0:64, :], [r_stg], [r_AT[s]])
                    fox_pend.append(epilogue)
            while fox_pend:
                fox_pend.pop(0)()
            if stages < 5:
                continue
            barrier()
            arena_off[0] = MARK1
            qkv_rot = Rot("qkv", [128, 12, 128], BF16, 2)
            dz_rot = Rot("dzr", [128, 4, 128], BF16, 3)
            tok_rot = Rot("tok", [128, 128], F32, 4)
            e512_rot = Rot("e512", [128, 512], F32, 16)
            wl_rot = Rot("wl", [128, 512], BF16, 18)
            ws_rot = Rot("ws", [128, 512], F32, 8)
            f32_rot = Rot("f32w", [128, 520], F32, 2)
            stg_rot = Rot("stg", [128, 512], BF16, 3)
            Sst = sb("Sst", [128, 512], F32); r_Sst = R()
            Sbf = sb("Sbf", [128, 512], BF16); r_Sbf = R()
            ms("dve", Sst[:], 0.0, [r_Sst])
            ms("dve", Sbf[:], 0.0, [r_Sbf])
            gst_ = {}
            gst2_ = {}

            def g_pre(n):
                c0 = n * 128
                ccols = slice(c0, c0 + 128)
                qkvT, r_qkv = qkv_rot.next()
                ld(qkvT[:], GQKV[s][:, ccols].rearrange("(c p) t -> p c t", p=128), [r_qkv], [r_GQKV[s]])
                dzT, r_dz = dz_rot.next()
                ld(dzT[:], DZ[s][:, ccols].rearrange("(c p) t -> p c t", p=128), [r_dz], [r_DZ[s]])
                tokA, r_tokA = tok_rot.next()
                tokB, r_tokB = tok_rot.next()
                tr(PS[3][:, 0:128], ZA[:, ccols], identF[:], [r_ZA, r_identF], [PR[3]])
                cp("act", tokA[:], PS[3][:, 0:128], [PR[3]], [r_tokA])
                tr(PS[4][:, 0:128], ZB[:, ccols], identF[:], [r_ZB, r_identF], [PR[4]])
                cp("dve", tokB[:], PS[4][:, 0:128], [PR[4]], [r_tokB])
                HS = [slice(h * 128, (h + 1) * 128) for h in range(4)]
                for h in range(4):
                    kTh = qkvT[:, 4 + h, :]
                    mm(PS[0][:, HS[h]], kTh, kTh, True, True, [r_qkv], [PR[0]], sig=(h == 3))
                for h in range(4):
                    mm(PS[1][:, HS[h]], qkvT[:, 4 + h, :], qkvT[:, h, :], True, True, [r_qkv], [PR[1]], sig=(h == 3))
                for h in range(4):
                    mm(PS[2][:, HS[h]], selA[:, HS[h]], ZA[:, ccols], True, False, [r_selA, r_ZA], [PR[2]], sig=False)
                    mm(PS[2][:, HS[h]], identB[:], masku[:], False, True, [r_identB, r_masku], [PR[2]], sig=(h == 3))
                for h in range(4):
                    mm(PS[3][:, HS[h]], selA[:, HS[h]], ZA[:, ccols], True, False, [r_selA, r_ZA], [PR[3]], sig=False)
                    mm(PS[3][:, HS[h]], identB[:], maskl[:], False, True, [r_identB, r_maskl], [PR[3]], sig=(h == 3))
                for h in range(4):
                    mm(PS[4][:, HS[h]], selA[:, HS[h]], ZA[:, ccols], True, True, [r_selA, r_ZA], [PR[4]], sig=(h == 3))
                Eu, r_Eu = e512_rot.next()
                El, r_El = e512_rot.next()
                Ep, r_Ep = e512_rot.next()
                for h in range(4):
                    act(Eu[:, HS[h]], PS[2][:, HS[h]], AF.Exp, [PR[2], r_tokB], [r_Eu], bias=tokB[:, 64 + h:65 + h])
                    act(El[:, HS[h]], PS[3][:, HS[h]], AF.Exp, [PR[3], r_tokA], [r_El], bias=tokA[:, 32 + h:33 + h], scale=-1.0)
                act(Ep[:], PS[4][:, :], AF.Exp, [PR[4]], [r_Ep])
                ATt, r_ATt = wl_rot.next()
                tt("dve", ATt[:], PS[1][:, :], Eu[:], ALU.mult, [PR[1], r_Eu], [r_ATt])
                Lt, r_Lt = ws_rot.next()
                for h in range(4):
                    stt(Lt[:, HS[h]], PS[0][:, HS[h]], tokB[:, h:h + 1], El[:, HS[h]], ALU.mult, ALU.mult,
                        [PR[0], r_tokB, r_El], [r_Lt])
                qdec, r_qdec = wl_rot.next()
                tt("dve", qdec[:], qkvT[:, 0:4, :].rearrange("p c t -> p (c t)"), Ep[:], ALU.mult, [r_qkv, r_Ep], [r_qdec])
                for h in range(4):
                    tr(PB[:, HS[h]], qkvT[:, 4 + h, :], identB[:], [r_qkv, r_identB], [PBR])
                    tr(PB[:, 512 + h * 128:512 + (h + 1) * 128], qkvT[:, 8 + h, :], identB[:], [r_qkv, r_identB], [PBR])
                kbg, r_kbg = wl_rot.next()
                kdec, r_kdec = wl_rot.next()
                vb, r_vb = wl_rot.next()
                for h in range(4):
                    ts("dve", kbg[:, HS[h]], PB[:, HS[h]], tokB[:, 32 + h:33 + h], None, ALU.mult, None, [PBR, r_tokB], [r_kbg])
                    act(kdec[:, HS[h]], PB[:, HS[h]], AF.Copy, [PBR, r_tokA], [r_kdec], scale=tokA[:, 64 + h:65 + h])
                    ts("dve", vb[:, HS[h]], PB[:, 512 + h * 128:512 + (h + 1) * 128], tokB[:, h:h + 1], None, ALU.mult, None,
                       [PBR, r_tokB], [r_vb])
                for h in range(4):
                    tr(PS[5][:, HS[h]], Lt[:, HS[h]], identF[:], [r_Lt, r_identF], [PR[5]])
                Mt, r_Mt = ws_rot.next()
                cp("act", Mt[:], PS[5][:, :], [PR[5]], [r_Mt])
                Rt, r_Rt = ws_rot.next()
                tt("dve", Rt[:], ident4F[:], PS[5][:, :], ALU.subtract, [r_ident4F, PR[5]], [r_Rt])
                Pc, r_Pc, Qc, r_Qc = Lt, r_Lt, Mt, r_Mt
                for lvl in range(6):
                    for h in range(4):
                        mm(PS[0][:, HS[h]], Qc[:, HS[h]], Pc[:, HS[h]], True, True, [r_Qc, r_Pc], [PR[0]], sig=(h == 3))
                    if lvl < 5:
                        for h in range(4):
                            mm(PS[1][:, HS[h]], Pc[:, HS[h]], Qc[:, HS[h]], True, True, [r_Qc, r_Pc], [PR[1]], sig=(h == 3))
                    Pn, r_Pn = ws_rot.next()
                    cp("act", Pn[:], PS[0][:, :], [PR[0]], [r_Pn])
                    if lvl < 5:
                        Qn, r_Qn = ws_rot.next()
                        cp("dve", Qn[:], PS[1][:, :], [PR[1]], [r_Qn])
                    for h in range(4):
                        mm(PS[2][:, HS[h]], Pn[:, HS[h]], Rt[:, HS[h]], True, True, [r_Pn, r_Rt], [PR[2]], sig=(h == 3))
                    Rn, r_Rn = ws_rot.next()
                    tt("dve", Rn[:], Rt[:], PS[2][:, :], ALU.add, [r_Rt, PR[2]], [r_Rn])
                    Rt, r_Rt = Rn, r_Rn
                    Pc, r_Pc = Pn, r_Pn
                    if lvl < 5:
                        Qc, r_Qc = Qn, r_Qn
                Rf, r_Rf = Rt, r_Rt
                Rt, r_Rt = wl_rot.next()
                cp("act", Rt[:], Rf[:], [r_Rf], [r_Rt])
                for h in range(4):
                    mm(PS[3][:, HS[h]], kbg[:, HS[h]], Rt[:, HS[h]], True, True, [r_kbg, r_Rt], [PR[3]], sig=(h == 3))
                for h in range(4):
                    mm(PS[4][:, HS[h]], Rt[:, HS[h]], vb[:, HS[h]], True, True, [r_vb, r_Rt], [PR[4]], sig=(h == 3))
                wT, r_wT = wl_rot.next()
                cp("act", wT[:], PS[3][:, :], [PR[3]], [r_wT])
                uu, r_uu = e512_rot.next()
                cp("dve", uu[:], PS[4][:, :], [PR[4]], [r_uu])
                gst_[n] = dict(ccols=ccols, HS=HS, tokA=tokA, r_tokA=r_tokA, dzT=dzT, r_dz=r_dz, ATt=ATt, r_ATt=r_ATt,
                               qdec=qdec, r_qdec=r_qdec, kdec=kdec, r_kdec=r_kdec, wT=wT, r_wT=r_wT, uu=uu, r_uu=r_uu)

            def g_scan(n):
                d_ = gst_.pop(n)
                ccols, HS, tokA, r_tokA, dzT, r_dz = d_['ccols'], d_['HS'], d_['tokA'], d_['r_tokA'], d_['dzT'], d_['r_dz']
                ATt, r_ATt, qdec, r_qdec, kdec, r_kdec = d_['ATt'], d_['r_ATt'], d_['qdec'], d_['r_qdec'], d_['kdec'], d_['r_kdec']
                wT, r_wT, uu, r_uu = d_['wT'], d_['r_wT'], d_['uu'], d_['r_uu']
                for h in range(4):
                    mm(PS[5][:, HS[h]], wT[:, HS[h]], Sbf[:, HS[h]], True, True, [r_wT, r_Sbf], [PR[5]], sig=(h == 3))
                vnew, r_vnew = wl_rot.next()
                tt("dve", vnew[:], uu[:], PS[5][:, :], ALU.subtract, [r_uu, PR[5]], [r_vnew])
                for h in range(4):
                    mm(PS[6][:, HS[h]], Sbf[:, HS[h]], qdec[:, HS[h]], True, False, [r_Sbf, r_qdec], [PR[6]], sig=False)
                    mm(PS[6][:, HS[h]], vnew[:, HS[h]], ATt[:, HS[h]], False, True, [r_vnew, r_ATt], [PR[6]], sig=(h == 3))
                for h in range(4):
                    mm(PS[0][:, HS[h]], kdec[:, HS[h]], vnew[:, HS[h]], True, True, [r_kdec, r_vnew], [PR[0]], sig=(h == 3))
                for h in range(4):
                    stt(Sst[:, HS[h]], Sst[:, HS[h]], tokA[:, 96 + h:97 + h], PS[0][:, HS[h]], ALU.mult, ALU.add,
                        [r_Sst, r_tokA, PR[0]], [r_Sst])
                cp("act", Sbf[:], Sst[:], [r_Sst], [r_Sbf])
                osb, r_osb = e512_rot.next()
                cp("act", osb[:], PS[6][:, :], [PR[6]], [r_osb])
                sq, r_sq = wl_rot.next()
                act(sq[:], osb[:], AF.Square, [r_osb], [r_sq])
                for h in range(4):
                    mm(PS[1][:, h:h + 1], sq[:, HS[h]], onesB[:, 0:1], True, True, [r_onesB, r_sq], [PR[1]], sig=(h == 3))
                rw, r_rw = f32_rot.next()
                ts("dve", rw[:, 0:4], PS[1][:, 0:4], 1.0 / 128, EPS, ALU.mult, ALU.add, [PR[1]], [r_rw])
                tt("pool", rw[:, 4:8], rw[:, 0:4], nhalf[:, 0:4], ALU.pow, [r_rw, r_nhalf], [r_rw])
                gst2_[n] = dict(ccols=ccols, HS=HS, dzT=dzT, r_dz=r_dz, osb=osb, r_osb=r_osb, rw=rw, r_rw=r_rw)

            def g_scanb(n):
                d_ = gst2_.pop(n)
                ccols, HS, dzT, r_dz, osb, r_osb, rw, r_rw = (d_['ccols'], d_['HS'], d_['dzT'], d_['r_dz'], d_['osb'], d_['r_osb'],
                                                              d_['rw'], d_['r_rw'])
                Dg, r_Dg = e512_rot.next()
                for h in range(4):
                    ts("dve", Dg[:, HS[h]], identF[:], rw[:, 4 + h:5 + h], None, ALU.mult, None, [r_identF, r_rw], [r_Dg])
                for h in range(4):
                    mm(PS[2][:, HS[h]], onesF[:], Dg[:, HS[h]], True, True, [r_onesF, r_Dg], [PR[2]], sig=(h == 3))
                o1, r_o1 = e512_rot.next()
                stt(o1[:], osb[:], gonT[:, 0:1], PS[2][:, :], ALU.mult, ALU.mult, [r_osb, r_gonT, PR[2]], [r_o1])
                stg, r_stg = stg_rot.next()
                tt("dve", stg[:], o1[:], dzT[:].rearrange("p c t -> p (c t)"), ALU.mult, [r_o1, r_dz], [r_stg])
                stq(OGs[s][:, ccols].rearrange("(h p) c -> p h c", p=128), stg[:].rearrange("p (h c) -> p h c", h=4),
                    [r_stg], [r_OG[s]])
            for t in range(NT + 2):
                if t < NT:
                    g_pre(t)
                if 0 <= t - 1 < NT:
                    g_scan(t - 1)
                if 0 <= t - 2 < NT:
                    g_scanb(t - 2)
            if stages < 6:
                continue
            barrier()
            arena_off[0] = MARK0
            NSLT = (2 * S) // 128 + 32
            Tt = sb("Tt", [128, NT, D], BF16); r_Tt = [R() for _ in range(NT)]
            A12 = sb("A12", [128, NT, 64], F32); r_A12 = [R() for _ in range(NT)]
            RTW = sb("RTW", [128, NT, 2], F32); r_RTW = [R() for _ in range(NT)]
            RK = sb("RK", [128, NT, 32], F32); r_RK = [R() for _ in range(NT)]
            SLf = sb("SLf", [128, NT, 2], F32); r_SLf = R()
            SLi = sb("SLi", [128, NT, 2], mybir.dt.int32); r_SLi = R()
            WIf = sb("WIf", [128, NSLT], F32); r_WIf = R()
            WIi = sb("WIi", [128, NSLT], mybir.dt.int32); r_WIi = R()
            cnt = sb("cnt", [128, 5, 32], F32); r_cnt = R()
            gffn_b = sb("gffn_b", [128, D], F32); r_gffn_b = R()
            ld(gffn_b[:], g_ffn[None, :].to_broadcast([128, D]), [r_gffn_b])
            MARKT = arena_off[0]
            wout_sb, r_wout = wres("wout", w_out, 8, D)
            wofox_sb, r_wofox = wres("wofox", w_o_fox, 4, D)
            wodel_sb, r_wodel = wres("wodel", w_o_delta, 4, D)
            xg = sb("xg", [128, 4, D], F32); r_xg = [R() for _ in range(4)]
            mrg = sb("mrg", [128, 8, 512], BF16); r_mrg = R()
            at_rot = Rot("atr", [128, 4, 512], BF16, 2)
            th_rot = Rot("thr", [128, 8, 512], BF16, 2)
            e512_rot = Rot("e512", [128, 512], F32, 4)
            rt_rot = Rot("rtr", [128, 128], F32, 2)
            tTt_rot = Rot("tTt", [128, 8, 128], BF16, 2)
            zt = sb("zt", [128, D], BF16); r_zt = R()
            ms("dve", zt[:], 0.0, [r_zt])
            for i in range(NSLT):
                stq(Xg[s][i * 128:(i + 1) * 128, :], zt[:], [r_zt], [r_Xgz[s]])
            for g in range(NG):
                cols = slice(g * 512, (g + 1) * 512)
                atT, r_atT = at_rot.next()
                ogT, r_ogT = at_rot.next()
                ld(atT[:], ATs[s][:, cols].rearrange("(k p) t -> p k t", p=128), [r_atT], [r_AT[s]])
                ld(ogT[:], OGs[s][:, cols].rearrange("(k p) t -> p k t", p=128), [r_ogT], [r_OG[s]])
                thf, r_thf = th_rot.next()
                thd, r_thd = th_rot.next()
                ld(thf[:], THF[s][:, cols].rearrange("(k p) t -> p k t", p=128), [r_thf], [r_THF[s]])
                ld(thd[:], THD[s][:, cols].rearrange("(k p) t -> p k t", p=128), [r_thd], [r_THD[s]])
                for j in range(4):
                    r0 = tok0 + g * 512 + j * 128
                    ld(xg[:, j, :], x_d[r0:r0 + 128, :], [r_xg[j]])
                for m in range(8):
                    ms_ = slice(m * 128, (m + 1) * 128)
                    for k in range(4):
                        mm(PS[0][:, :], wofox_sb[:, k, ms_], atT[:, k, :], k == 0, k == 3, [r_wofox, r_atT], [PR[0]])
                    for k in range(4):
                        mm(PS[1][:, :], wodel_sb[:, k, ms_], ogT[:, k, :], k == 0, k == 3, [r_wodel, r_ogT], [PR[1]])
                    m1, r_m1 = e512_rot.next()
                    m2, r_m2 = e512_rot.next()
                    stt(m1[:], thf[:, m, :], 1.0, PS[0][:, :], ALU.add, ALU.mult, [r_thf, PR[0]], [r_m1])
                    stt(m2[:], thd[:, m, :], 1.0, PS[1][:, :], ALU.add, ALU.mult, [r_thd, PR[1]], [r_m2])
                    tt("pool", mrg[:, m, :], m1[:], m2[:], ALU.add, [r_m1, r_m2], [r_mrg])
                for j in range(4):
                    for hf in range(2):
                        b = 2 + hf
                        for k in range(8):
                            mm(PS[b][:, :], mrg[:, k, j * 128:(j + 1) * 128], wout_sb[:, k, hf * 512:(hf + 1) * 512],
                               k == 0, k == 7, [r_mrg, r_wout], [PR[b]])
                        stt(xg[:, j, hf * 512:(hf + 1) * 512], PS[b][:, :], 0.5, xg[:, j, hf * 512:(hf + 1) * 512],
                            ALU.mult, ALU.add, [PR[b], r_xg[j]], [r_xg[j]])
                for j in range(4):
                    n = 4 * g + j
                    stq(X1[s][n * 128:(n + 1) * 128, :], xg[:, j, :], [r_xg[j]], [r_X1[s]])
                    s1, r_s1 = norm_stats(xg[:, j, :], r_xg[j])
                    stt(Tt[:, n, :], xg[:, j, :], s1[:, 1:2], gffn_b[:], ALU.mult, ALU.mult, [r_xg[j], r_s1, r_gffn_b], [r_Tt[n]])
                    for c in range(8):
                        tr(PB[:, c * 128:(c + 1) * 128], Tt[:, n, c * 128:(c + 1) * 128], identB[:], [r_Tt[n], r_identB], [PBR])
                    tTt, r_tTt = tTt_rot.next()
                    cp("act", tTt[:].rearrange("p c t -> p (c t)"), PB[:, :], [PBR], [r_tTt])
                    for k in range(8):
                        mm(PS[4][:, 0:36], tTt[:, k, :], wrt_sb[:, k, :], k == 0, k == 7, [r_tTt, r_wrt], [PR[4]])
                    rt, r_rt = rt_rot.next()
                    lg = rt[:, 0:36]
                    tt("dve", lg, PS[4][:, 0:36], brt[:], ALU.add, [PR[4], r_brt], [r_rt])
                    gmax = rt[:, 40:41]; ngm = rt[:, 41:42]; sg = rt[:, 42:43]; psel = rt[:, 43:44]
                    oh = rt[:, 44:48]; el = rt[:, 48:56]; m8 = rt[:, 56:64]; msk = rt[:, 64:72]; a1 = rt[:, 72:80]
                    nm1 = rt[:, 80:81]; e21 = rt[:, 81:82]; cc = rt[:, 82:83]; eg = rt[:, 84:88]; a2 = rt[:, 88:96]
                    rr_ = [r_rt]
                    kb.op("dve", lambda e, lg=lg, gmax=gmax: e.tensor_reduce(out=gmax, in_=lg[:, 0:4], axis=mybir.AxisListType.X, op=ALU.max),
                          reads=rr_, writes=rr_)
                    ts("dve", oh, lg[:, 0:4], gmax, None, ALU.is_ge, None, rr_, rr_)
                    ts("dve", ngm, gmax, -1.0, None, ALU.mult, None, rr_, rr_)
                    ms("dve", sg, 0.0, rr_)
                    act(eg, lg[:, 0:4], AF.Exp, rr_, rr_, bias=ngm, accum=sg)
                    kb.op("dve", lambda e, psel=psel, sg=sg: e.reciprocal(psel, sg), reads=rr_, writes=rr_)
                    ts("dve", el, lg[:, 4:12], oh[:, 0:1], None, ALU.mult, None, rr_, rr_)
                    for gg in range(1, 4):
                        stt(el, lg[:, 4 + 8 * gg:12 + 8 * gg], oh[:, gg:gg + 1], el, ALU.mult, ALU.add, rr_, rr_)
                    kb.op("dve", lambda e, m8=m8, el=el: e.max(out=m8, in_=el), reads=rr_, writes=rr_)
                    ts("dve", msk, el, m8[:, 1:2], None, ALU.is_ge, None, rr_, rr_)
                    ts("dve", a1, el, m8[:, 0:1], None, ALU.is_ge, None, rr_, rr_)
                    tt("dve", a2, msk, a1, ALU.subtract, rr_, rr_)
                    ts("dve", nm1, m8[:, 0:1], -1.0, None, ALU.mult, None, rr_, rr_)
                    act(e21, m8[:, 1:2], AF.Exp, rr_, rr_, bias=nm1)
                    ts("dve", cc, e21, 1.0, None, ALU.add, None, rr_, rr_)
                    kb.op("dve", lambda e, cc=cc: e.reciprocal(cc, cc), reads=rr_, writes=rr_)
                    tt("dve", RTW[:, n, 0:1], cc, psel, ALU.mult, rr_, [r_RTW[n]])
                    tt("dve", RTW[:, n, 1:2], RTW[:, n, 0:1], e21, ALU.mult, rr_ + [r_RTW[n]], [r_RTW[n]])
                    for gg in range(4):
                        ts("dve", A12[:, n, 8 * gg:8 * gg + 8], a1, oh[:, gg:gg + 1], None, ALU.mult, None, rr_, [r_A12[n]])
                        ts("dve", A12[:, n, 32 + 8 * gg:40 + 8 * gg], a2, oh[:, gg:gg + 1], None, ALU.mult, None, rr_, [r_A12[n]])
            barrier()
            arena_off[0] = MARKT
            asum_rot = Rot("asum", [128, 32], BF16, 3)
            tmp_rot = Rot("tmpr", [128, 64], F32, 3)
            ms("dve", cnt[:, 0, :], 0.0, [r_cnt])
            for n in range(NT):
                asum, r_asum = asum_rot.next()
                tt("dve", asum[:], A12[:, n, 0:32], A12[:, n, 32:64], ALU.add, [r_A12[n]], [r_asum])
                mm(PS[0][:, 0:32], ustrict[:], asum[:], True, True, [r_ustrict, r_asum], [PR[0]])
                mm(PS[1][:, 0:32], onesB[:], asum[:], True, True, [r_onesB, r_asum], [PR[1]])
                tt("dve", RK[:, n, :], PS[0][:, 0:32], cnt[:, 0, :], ALU.add, [PR[0], r_cnt], [r_RK[n]])
                tt("dve", cnt[:, 0, :], cnt[:, 0, :], PS[1][:, 0:32], ALU.add, [PR[1], r_cnt], [r_cnt])
            for e_ in range(32):
                tmp, r_tmp = tmp_rot.next()
                ts("dve", tmp[:, 0:64], tstart[:, 0:64], cnt[:, 0, e_:e_ + 1], None, ALU.is_lt, None, [r_tstart, r_cnt], [r_tmp])
                kb.op("dve", lambda e, tmp=tmp, e_=e_, cnt=cnt: e.tensor_reduce(out=cnt[:, 2, e_:e_ + 1], in_=tmp[:, 0:64],
                                                                        axis=mybir.AxisListType.X, op=ALU.add),
                      reads=[r_tmp], writes=[r_cnt])
            ts("dve", cnt[:, 2, :], cnt[:, 2, :], 128.0, None, ALU.mult, None, [r_cnt], [r_cnt])
            kb.op("dve", lambda e, o_=cnt[:, 3, :], d_=cnt[:, 2, :]: e.tensor_tensor_scan(out=o_, data0=onesS[:, 0:32], data1=d_, initial=0.0,
                                                                             op0=ALU.mult, op1=ALU.add), reads=[r_cnt, r_onesS], writes=[r_cnt])
            tt("dve", cnt[:, 4, :], cnt[:, 3, :], cnt[:, 2, :], ALU.subtract, [r_cnt], [r_cnt])
            ms("dve", WIf[:], 0.0, [r_WIf])
            for e_ in range(32):
                stt(WIf[:], tstart[:, 0:NSLT], cnt[:, 3, e_:e_ + 1], WIf[:], ALU.is_ge, ALU.add, [r_tstart, r_cnt, r_WIf], [r_WIf])
            ts("dve", WIf[:], WIf[:], 31.0, None, ALU.min, None, [r_WIf], [r_WIf])
            ts("dve", WIf[:], WIf[:], 128.0, pcol[:, 0:1], ALU.mult, ALU.add, [r_WIf, r_pcol], [r_WIf])
            cp("dve", WIi[:], WIf[:], [r_WIf], [r_WIi])
            for n in range(NT):
                tmp, r_tmp = tmp_rot.next()
                tt("dve", tmp[:, 0:32], RK[:, n, :], cnt[:, 4, :], ALU.add, [r_RK[n], r_cnt], [r_tmp])
                for k in range(2):
                    tt("dve", tmp[:, 32:64], tmp[:, 0:32], A12[:, n, 32 * k:32 * k + 32], ALU.mult, [r_tmp, r_A12[n]], [r_tmp])
                    kb.op("dve", lambda e, tmp=tmp, n=n, k=k, SLf=SLf: e.tensor_reduce(out=SLf[:, n, k:k + 1], in_=tmp[:, 32:64],
                                                                              axis=mybir.AxisListType.X, op=ALU.add),
                          reads=[r_tmp], writes=[r_SLf])
            cp("dve", SLi[:], SLf[:], [r_SLf], [r_SLi])
            for n in range(NT):
                for k in range(2):
                    kb.dma("pool", "sc", lambda e, o_=Xg[s][:, :], i_=SLi[:, n, k:k + 1], t_=Tt[:, n, :]: e.indirect_dma_start(
                        out=o_, out_offset=bass.IndirectOffsetOnAxis(ap=i_, axis=0),
                        in_=t_, in_offset=None), reads=[r_Tt[n], r_SLi, r_Xgz[s]], writes=[r_Xg[s]])
            wx_rot = Rot("wx", [128, 6144], BF16, 4)
            xs_rot = Rot("xs", [128, D], BF16, 3)
            xsT_rot = Rot("xsT", [128, 8, 128], BF16, 3)
            hs_rot = Rot("hs", [128, 256], F32, 2)
            hb_rot = Rot("hb", [128, 256], BF16, 3)
            hT_rot = Rot("hTr", [128, 2, 128], BF16, 2)
            ys_rot = Rot("ys", [128, D], BF16, 2)
            PB2 = PS[6][:, :].bitcast(BF16)
            cst = {}

            def c_load(i):
                wx, r_wx = wx_rot.next()
                kb.dma("pool", "wxg", lambda e, o_=wx[:], i_=WIi[:, i:i + 1], w_=WX[:, :]: e.indirect_dma_start(
                    out=o_, out_offset=None, in_=w_,
                    in_offset=bass.IndirectOffsetOnAxis(ap=i_, axis=0)), reads=[r_WIi, r_wexp], writes=[r_wx])
                xs, r_xs = xs_rot.next()
                ld(xs[:], Xg[s][i * 128:(i + 1) * 128, :], [r_xs], [r_Xg[s]])
                cst[i] = dict(wx=wx, r_wx=r_wx, xs=xs, r_xs=r_xs)

            def c_tr(i):
                d_ = cst[i]
                for c in range(8):
                    tr(PB[:, c * 128:(c + 1) * 128], d_["xs"][:, c * 128:(c + 1) * 128], identB[:], [d_["r_xs"], r_identB], [PBR])
                xsT, r_xsT = xsT_rot.next()
                cp("dve", xsT[:].rearrange("p c t -> p (c t)"), PB[:, :], [PBR], [r_xsT])
                d_["xsT"], d_["r_xsT"] = xsT, r_xsT

            def c_gu(i):
                d_ = cst[i]
                b0 = 3 * (i % 2)
                for k in range(8):
                    mm(PS[b0][:, :], d_["xsT"][:, k, :], d_["wx"][:, k * 512:(k + 1) * 512], k == 0, k == 7,
                       [d_["r_xsT"], d_["r_wx"]], [PR[b0]])
                hsg, r_hsg = hs_rot.next()
                act(hsg[:], PS[b0][:, 0:256], AF.Silu, [PR[b0]], [r_hsg])
                hb, r_hb = hb_rot.next()
                tt("dve", hb[:], hsg[:], PS[b0][:, 256:512], ALU.mult, [r_hsg, PR[b0]], [r_hb])
                d_["hb"], d_["r_hb"] = hb, r_hb

            def c_down(i):
                d_ = cst.pop(i)
                b0 = 3 * (i % 2)
                for c in range(2):
                    tr(PB2[:, c * 128:(c + 1) * 128], d_["hb"][:, c * 128:(c + 1) * 128], identB[:], [d_["r_hb"], r_identB], [PR[6]])
                hTt, r_hTt = hT_rot.next()
                cp("act", hTt[:].rearrange("p c t -> p (c t)"), PB2[:, 0:256], [PR[6]], [r_hTt])
                ys, r_ys = ys_rot.next()
                wx = d_["wx"]
                for hf in range(2):
                    b = b0 + 1 + hf
                    for c in range(2):
                        mm(PS[b][:, :], hTt[:, c, :], wx[:, 4096 + c * 1024 + hf * 512:4096 + c * 1024 + (hf + 1) * 512],
                           c == 0, c == 1, [r_hTt, d_["r_wx"]], [PR[b]])
                    cp("act" if hf else "dve", ys[:, hf * 512:(hf + 1) * 512], PS[b][:, :], [PR[b]], [r_ys])
                stq(Yg[s][i * 128:(i + 1) * 128, :], ys[:], [r_ys], [r_Yg[s]])
            for t in range(NSLT + 3):
                if t < NSLT:
                    c_load(t)
                if 0 <= t - 1 < NSLT:
                    c_tr(t - 1)
                if 0 <= t - 2 < NSLT:
                    c_gu(t - 2)
                if 0 <= t - 3 < NSLT:
                    c_down(t - 3)
            barrier()
            arena_off[0] = MARKT
            wpg_sb, r_wpg = wres("wpg", w_ple_gate, 8, D)
            wpp_sb, r_wpp = wres("wpp", w_ple_proj, 2, D)
            xw_rot = Rot("xw", [128, D], F32, 6)
            yg_rot = Rot("yg", [128, D], BF16, 8)
            e512_rot = Rot("e512", [128, 512], F32, 4)
            nT_rot = Rot("nTr", [128, 8, 128], BF16, 3)
            xn_rot = Rot("xn", [128, D], BF16, 2)
            stg_rot = Rot("stg", [128, 512], BF16, 6)
            f32_rot = Rot("f32w", [128, 520], F32, 4)
            yt_rot = Rot("yt", [128, D], F32, 2)
            dst = {}

            def d_0(n):
                r0 = tok0 + n * 128
                xw, r_xw = xw_rot.next()
                ld(xw[:], X1[s][n * 128:(n + 1) * 128, :], [r_xw], [r_X1[s]])
                pt_, r_pt_ = f32_rot.next()
                ld(pt_[:, 0:256], p_d[r0:r0 + 128, :], [r_pt_])
                ygs = []
                for k in range(2):
                    yg, r_yg = yg_rot.next()
                    kb.dma("pool", "yg", lambda e, o_=yg[:], y_=Yg[s][:, :], i_=SLi[:, n, k:k + 1]: e.indirect_dma_start(
                        out=o_, out_offset=None, in_=y_,
                        in_offset=bass.IndirectOffsetOnAxis(ap=i_, axis=0)), reads=[r_SLi, r_Yg[s]], writes=[r_yg])
                    ygs.append((yg, r_yg))
                dst[n] = dict(xw=xw, r_xw=r_xw, pt_=pt_, r_pt_=r_pt_, ygs=ygs)

            def d_a(n):
                d_ = dst[n]
                xw, r_xw, pt_, r_pt_ = d_["xw"], d_["r_xw"], d_["pt_"], d_["r_pt_"]
                for k in range(2):
                    yg, r_yg = d_["ygs"][k]
                    stt(xw[:], yg[:], RTW[:, n, k:k + 1], xw[:], ALU.mult, ALU.add, [r_yg, r_RTW[n], r_xw], [r_xw])
                pbf, r_pbf = stg_rot.next()
                cp("dve", pbf[:, 0:256], pt_[:, 0:256], [r_pt_], [r_pbf])
                s1, r_s1 = norm_stats(xw[:], r_xw)
                d_.update(pbf=pbf, r_pbf=r_pbf, s1=s1, r_s1=r_s1)

            def d_b(n):
                d_ = dst[n]
                xw, r_xw, s1, r_s1 = d_["xw"], d_["r_xw"], d_["s1"], d_["r_s1"]
                xn, r_xn = xn_rot.next()
                ts("dve", xn[:], xw[:], s1[:, 1:2], None, ALU.mult, None, [r_xw, r_s1], [r_xn])
                for c in range(8):
                    tr(PB[:, c * 128:(c + 1) * 128], xn[:, c * 128:(c + 1) * 128], identB[:], [r_xn, r_identB], [PBR])
                nT, r_nT = nT_rot.next()
                for c in range(8):
                    if c % 2 == 0:
                        act(nT[:, c, :], PB[:, c * 128:(c + 1) * 128], AF.Copy, [PBR, r_gpleT], [r_nT], scale=gpleT[:, c:c + 1])
                    else:
                        ts("dve", nT[:, c, :], PB[:, c * 128:(c + 1) * 128], gpleT[:, c:c + 1], None, ALU.mult, None,
                           [PBR, r_gpleT], [r_nT])
                PB2 = PS[6][:, :].bitcast(BF16)
                for k in range(2):
                    tr(PB2[:, k * 128:(k + 1) * 128], d_["pbf"][:, k * 128:(k + 1) * 128], identB[:], [d_["r_pbf"], r_identB], [PR[6]])
                pT, r_pT = stg_rot.next()
                cp("act", pT[:, 0:256], PB2[:, 0:256], [PR[6]], [r_pT])
                d_.update(nT=nT, r_nT=r_nT, pT=pT, r_pT=r_pT)

            def d_c(n):
                d_ = dst.pop(n)
                r0 = tok0 + n * 128
                xw, r_xw, nT, r_nT, pT, r_pT = d_["xw"], d_["r_xw"], d_["nT"], d_["r_nT"], d_["pT"], d_["r_pT"]
                for hf in range(2):
                    hs_ = slice(hf * 512, (hf + 1) * 512)
                    for k in range(8):
                        mm(PS[0 + 2 * hf][:, :], nT[:, k, :], wpg_sb[:, k, hs_], k == 0, k == 7, [r_nT, r_wpg], [PR[0 + 2 * hf]])
                    for k in range(2):
                        mm(PS[1 + 2 * hf][:, :], pT[:, k * 128:(k + 1) * 128], wpp_sb[:, k, hs_], k == 0, k == 1, [r_pT, r_wpp], [PR[1 + 2 * hf]])
                    th, r_th = e512_rot.next()
                    act(th[:], PS[0 + 2 * hf][:, :], AF.Tanh, [PR[0 + 2 * hf]], [r_th], scale=0.5)
                    ts("dve", th[:], th[:], 0.5, 0.5, ALU.mult, ALU.add, [r_th], [r_th])
                    tt("dve", th[:], th[:], PS[1 + 2 * hf][:, :], ALU.mult, [r_th, PR[1 + 2 * hf]], [r_th])
                    tt("dve", xw[:, hs_], xw[:, hs_], th[:], ALU.add, [r_xw, r_th], [r_xw])
                s1, r_s1 = norm_stats(xw[:], r_xw)
                yt, r_yt = yt_rot.next()
                stt(yt[:], xw[:], s1[:, 1:2], gfin[:], ALU.mult, ALU.mult, [r_xw, r_s1, r_gfin], [r_yt])
                stq(out_d[r0:r0 + 128, :], yt[:], [r_yt], [r_out], stream="sto")
            for t in range(NT + 4):
                if t < NT:
                    d_0(t)
                if 0 <= t - 2 < NT:
                    d_a(t - 2)
                if 0 <= t - 3 < NT:
                    d_b(t - 3)
                if 0 <= t - 4 < NT:
                    d_c(t - 4)
        barrier()
        kb.emit()
    nc.dbg_names = dbg_names
    nc.arena_hw = arena_hw[0]
    return nc


WKEYS = ['g_mix', 'w_in', 'b_forget', 'conv_w', 'a_log', 'dt_bias', 'g_onorm', 'w_o_fox', 'w_o_delta', 'w_out', 'g_ffn',
         'w_group', 'b_group', 'w_router', 'b_router', 'w_gate', 'w_up', 'w_down', 'g_ple', 'w_ple_gate', 'w_ple_proj']
_NC_CACHE = {}


def kernel(**inputs):
    x = np.ascontiguousarray(np.asarray(inputs['x'], dtype=np.float32))
    p = np.ascontiguousarray(np.asarray(inputs['p'], dtype=np.float32))
    B, S, _ = x.shape
    nseq = B // NCORES
    shared = {k: np.ascontiguousarray(np.asarray(inputs[k], dtype=np.float32)[0]) for k in WKEYS}
    shared['g_final'] = np.ascontiguousarray(np.asarray(inputs['g_final'], dtype=np.float32))
    shared.update(host_consts())
    key = (nseq, S)
    if key not in _NC_CACHE:
        _NC_CACHE[key] = build(nseq, S)
    nc = _NC_CACHE[key]
    in_maps = []
    for c in range(NCORES):
        m = dict(shared)
        m['x'] = x[c * nseq:(c + 1) * nseq].reshape(nseq * S, D)
        m['p'] = p[0, c * nseq:(c + 1) * nseq].reshape(nseq * S, 256)
        in_maps.append(m)
    res = run_bass_kernel_spmd(nc, in_maps, core_ids=list(range(NCORES)))
    out = np.concatenate([np.asarray(r['out']).reshape(nseq, S, D) for r in res.results], axis=0)
    return out.astype(np.float32)
```

```python
import numpy as np
from contextlib import ExitStack
import concourse.bass as bass
import concourse.mybir as mybir
from concourse.bass_utils import run_bass_kernel_spmd

F32 = mybir.dt.float32
BF16 = mybir.dt.bfloat16
AF = mybir.ActivationFunctionType
ALU = mybir.AluOpType

NCORES = 8
D = 1024
EPS = 1e-6
NEG = -60000.0
SAME_ENGINE_SYNC = True
EMBED_WAIT = True
FOX_FILL = False
FOX_K = 128
FOX_M = 128
FOX_FILL_N = 256


class Res:
    __slots__ = ("name", "w", "r", "nowaw")

    def __init__(self, name="", nowaw=False):
        self.name = name
        self.nowaw = nowaw
        self.w = {}
        self.r = {}


class KB:
    ENGS = ("pe", "act", "dve", "pool", "sp")
    NDSEM = 20

    def __init__(self, nc, stack):
        self.nc = nc
        self.stack = stack
        self.sem = {}
        self.count = {}
        self.prog = {e: [] for e in self.ENGS}
        self.waited = {e: {} for e in self.ENGS}
        for e in self.ENGS:
            self.sem[e] = stack.enter_context(nc.semaphore("s_" + e))
            self.count[e] = 0
        self.dpool = {}
        self.dnext = {}
        for q in ("sp", "pool"):
            self.dpool[q] = []
            self.dnext[q] = 0
            for i in range(self.NDSEM):
                k = f"d{q}{i}"
                self.sem[k] = stack.enter_context(nc.semaphore(k))
                self.count[k] = 0
                self.dpool[q].append(k)

    def res(self, name=""):
        return Res(name)

    def _need(self, eng, reads, writes, extra=()):
        need = {}

        def add(k, v):
            if need.get(k, 0) < v:
                need[k] = v
        for k, v in extra:
            add(k, v)
        for r in reads:
            for k, v in r.w.items():
                add(k, v)
        for w in writes:
            if not w.nowaw:
                for k, v in w.w.items():
                    add(k, v)
            for k, v in w.r.items():
                add(k, v)
        out = []
        for k, v in need.items():
            if k == eng and (not SAME_ENGINE_SYNC or eng == "pe" or v > self.count[eng]):
                continue
            if self.waited[eng].get(k, 0) >= v:
                continue
            self.waited[eng][k] = v
            out.append((k, v))
        return out

    def _commit(self, ticket, reads, writes):
        k, v = ticket
        for w in writes:
            if w.w.get(k, 0) < v:
                w.w[k] = v
            w.r = {}
        for r in reads:
            if r.r.get(k, 0) < v:
                r.r[k] = v

    def op(self, eng, fn, reads=(), writes=(), sig=True):
        for k, v in self._need(eng, reads, writes):
            self.prog[eng].append(("wait", k, v))
        if sig:
            self.count[eng] += 1
            ticket = (eng, self.count[eng])
            self.prog[eng].append(("op", fn, eng, 1))
        else:
            ticket = (eng, self.count[eng] + 1)
            self.prog[eng].append(("op", fn, None, 0))
        self._commit(ticket, reads, writes)

    def dma(self, queue, stream, fn, reads=(), writes=()):
        pool = self.dpool[queue]
        k = pool[self.dnext[queue] % len(pool)]
        self.dnext[queue] += 1
        extra = [(k, self.count[k])] if self.count[k] > 0 else []
        for kk, v in self._need(queue, reads, writes, extra):
            self.prog[queue].append(("wait", kk, v))
        self.count[k] += 16
        ticket = (k, self.count[k])
        self.prog[queue].append(("op", fn, k, 16))
        self._commit(ticket, reads, writes)

    def emit(self):
        nc = self.nc
        with nc.Block() as block:
            def run(engname, e):
                pend = []
                for item in self.prog[engname]:
                    if item[0] == "wait":
                        pend.append(item)
                        continue
                    _, fn, semk, inc = item
                    is_dma = semk is not None and semk not in self.ENGS
                    fuse = EMBED_WAIT and pend and not is_dma
                    for w in (pend[:-1] if fuse else pend):
                        e.wait_ge(self.sem[w[1]], w[2])
                    ins = fn(e)
                    if fuse:
                        ins._wait_ge(self.sem[pend[-1][1]], pend[-1][2])
                    pend = []
                    if semk is not None:
                        ins.then_inc(self.sem[semk], inc)
                for w in pend:
                    e.wait_ge(self.sem[w[1]], w[2])

            @block.tensor
            def _(e):
                run("pe", e)

            @block.scalar
            def _(e):
                run("act", e)

            @block.vector
            def _(e):
                run("dve", e)

            @block.gpsimd
            def _(e):
                run("pool", e)

            @block.sync
            def _(e):
                run("sp", e)


def host_consts():
    c = {}
    c["identF"] = np.eye(128, dtype=np.float32)
    p = np.arange(128)[:, None]
    f = np.arange(128)[None, :]
    c["mask_u"] = np.where(f >= p, 0.0, NEG).astype(np.float32)
    c["mask_l"] = np.where(f >= p, -NEG, 0.0).astype(np.float32)
    sel = np.zeros((128, 4, 128), np.float32)
    for h in range(4):
        sel[32 + h, h, :] = 1.0
    c["selA"] = sel.reshape(128, 512)
    c["tstart"] = np.tile((np.arange(160, dtype=np.float32) * 128.0)[None, :], (128, 1))
    c["pcol"] = np.arange(128, dtype=np.float32)[:, None]
    c["ustrict"] = (p < f).astype(np.float32)
    return c


def build(NSEQ, S, stages=99, debug=False):
    T = NSEQ * S
    NT = S // 128
    NG = S // 512
    nc = bass.Bass("TRN2", target_bir_lowering=False)

    def din(name, shape, dt=F32):
        return nc.dram_tensor(name, list(shape), dt, kind="ExternalInput").ap()

    def dscr(name, shape, dt=BF16):
        return nc.dram_tensor(name, list(shape), dt, kind="ExternalOutput" if debug else "Internal").ap()

    x_d = din("x", [T, D])
    p_d = din("p", [T, 256])
    g_mix = din("g_mix", [D]); w_in = din("w_in", [D, 5648]); b_forget = din("b_forget", [8])
    conv_w = din("conv_w", [4, 1536]); a_log = din("a_log", [4]); dt_bias = din("dt_bias", [4])
    g_onorm = din("g_onorm", [128]); w_o_fox = din("w_o_fox", [512, D]); w_o_delta = din("w_o_delta", [512, D])
    w_out = din("w_out", [D, D]); g_ffn = din("g_ffn", [D]); w_group = din("w_group", [D, 4])
    b_group = din("b_group", [4]); w_router = din("w_router", [D, 32]); b_router = din("b_router", [32])
    w_gate = din("w_gate", [32, D, 256]); w_up = din("w_up", [32, D, 256]); w_down = din("w_down", [32, 256, D])
    g_ple = din("g_ple", [D]); w_ple_gate = din("w_ple_gate", [D, D]); w_ple_proj = din("w_ple_proj", [256, D])
    g_final = din("g_final", [D])
    identF_d = din("identF", [128, 128]); mask_u_d = din("mask_u", [128, 128]); mask_l_d = din("mask_l", [128, 128])
    selA_d = din("selA", [128, 512])
    out_d = nc.dram_tensor("out", [T, D], F32, kind="ExternalOutput").ap()

    WX = dscr("WX", [32 * 128, 6144])
    NSLT_ = (2 * S) // 128 + 32
    Xg = [dscr(f"Xg{s}", [NSLT_ * 128, D]) for s in range(NSEQ)]
    Yg = [dscr(f"Yg{s}", [NSLT_ * 128, D]) for s in range(NSEQ)]
    X1 = [dscr(f"X1{s}", [S, D], F32) for s in range(NSEQ)]
    tstart_d = din("tstart", [128, 160]); pcol_d = din("pcol", [128, 1]); ustrict_d = din("ustrict", [128, 128])
    QT = [dscr(f"QT{s}", [512, S]) for s in range(NSEQ)]
    KT = [dscr(f"KT{s}", [512, S]) for s in range(NSEQ)]
    GQKV = [dscr(f"GQKV{s}", [1536, S]) for s in range(NSEQ)]
    DZ = [dscr(f"DZ{s}", [512, S]) for s in range(NSEQ)]
    THF = [dscr(f"THF{s}", [D, S]) for s in range(NSEQ)]
    THD = [dscr(f"THD{s}", [D, S]) for s in range(NSEQ)]
    AUGQ = [dscr(f"AUGQ{s}", [8, 6, S]) for s in range(NSEQ)]
    AUGK = [dscr(f"AUGK{s}", [8, 6, S]) for s in range(NSEQ)]
    ATs = [dscr(f"AT{s}", [512, S]) for s in range(NSEQ)]
    OGs = [dscr(f"OG{s}", [512, S]) for s in range(NSEQ)]
    GTs = [dscr(f"GT{s}", [32, S]) for s in range(NSEQ)]

    with ExitStack() as st:
        kb = KB(nc, st)

        ARENA_BYTES = 209920
        arena_t = st.enter_context(nc.sbuf_tensor("arena", [128, ARENA_BYTES // 4], F32))
        arena_off = [0]
        arena_hw = [0]

        def sb(name, shape, dt):
            esz = 2 if dt == BF16 else 4
            n = 1
            for d_ in shape[1:]:
                n *= d_
            nbytes = (n * esz + 31) // 32 * 32
            o = arena_off[0]
            assert o + nbytes <= ARENA_BYTES, (name, o, nbytes)
            arena_off[0] = o + nbytes
            arena_hw[0] = max(arena_hw[0], o + nbytes)
            v = arena_t[:, o // 4:(o + nbytes) // 4]
            if dt != F32:
                v = v.bitcast(dt)
            v = v[:, 0:n]
            if len(shape) == 3:
                v = v.rearrange("p (a b) -> p a b", a=shape[1])
            elif len(shape) == 4:
                v = v.rearrange("p (a b c) -> p a b c", a=shape[1], b=shape[2])
            return v

        def R(name=""):
            return kb.res(name)

        def mm(out, lhsT, rhs, start, stop, reads, writes, sig=None):
            if sig is None:
                sig = stop
            kb.op("pe", lambda e: e.matmul(out, lhsT=lhsT, rhs=rhs, start=start, stop=stop),
                  reads=reads, writes=writes, sig=sig)

        def tr(out, in_, ident, reads, writes):
            kb.op("pe", lambda e: e.transpose(out, in_, ident), reads=reads, writes=writes)

        def act(out, in_, func, reads, writes, bias=None, scale=None, accum=None):
            kw = {}
            if bias is not None:
                kw["bias"] = bias
            if scale is not None:
                kw["scale"] = scale
            if accum is not None:
                kw["accum_out"] = accum
            kb.op("act", lambda e: e.activation(out=out, in_=in_, func=func, **kw), reads=reads, writes=writes)

        def ts(eng, out, in0, s1, s2, op0, op1, reads, writes):
            if s2 is None:
                kb.op(eng, lambda e: e.tensor_scalar(out=out, in0=in0, scalar1=s1, scalar2=None, op0=op0),
                      reads=reads, writes=writes)
            else:
                kb.op(eng, lambda e: e.tensor_scalar(out=out, in0=in0, scalar1=s1, scalar2=s2, op0=op0, op1=op1),
                      reads=reads, writes=writes)

        def tt(eng, out, in0, in1, op, reads, writes):
            kb.op(eng, lambda e: e.tensor_tensor(out=out, in0=in0, in1=in1, op=op), reads=reads, writes=writes)

        def stt(out, in0, scalar, in1, op0, op1, reads, writes, eng="dve"):
            kb.op(eng, lambda e: e.scalar_tensor_tensor(out=out, in0=in0, scalar=scalar, in1=in1, op0=op0, op1=op1),
                  reads=reads, writes=writes)

        def cp(eng, out, in_, reads, writes):
            if eng == "act":
                kb.op("act", lambda e: e.copy(out, in_), reads=reads, writes=writes)
            else:
                kb.op(eng, lambda e: e.tensor_copy(out, in_), reads=reads, writes=writes)

        def ms(eng, ap, val, writes):
            kb.op(eng, lambda e: e.memset(ap, val), writes=writes)

        def ld(out, in_, writes, reads=(), stream="ld", q="sp", nonc=False):
            if nonc:
                kb.dma(q, stream, lambda e: e.dma_start(out=out, in_=in_, allow_slow_non_contiguous=True),
                       reads=reads, writes=writes)
            else:
                kb.dma(q, stream, lambda e: e.dma_start(out=out, in_=in_), reads=reads, writes=writes)

        def ldc(out, in_, writes, reads=(), stream="ldc", nonc=False):
            ld(out, in_, writes, reads, stream=stream, q="pool", nonc=nonc)

        def stq(out, in_, reads, writes=(), stream="st"):
            kb.dma("sp", stream, lambda e: e.dma_start(out=out, in_=in_), reads=reads, writes=writes)

        dbg_names = []

        def dbg(name, ap, reads, dt=F32):
            if not debug:
                return
            shp = list(ap.shape)
            d_ = nc.dram_tensor("dbg_" + name, shp, dt, kind="ExternalOutput").ap()
            dbg_names.append("dbg_" + name)
            idx = tuple(slice(None) for _ in shp)
            stq(d_[idx], ap, reads)

        class Rot:
            def __init__(self, name, shape, dt, n):
                self.t = [sb(f"{name}{i}", shape, dt) for i in range(n)]
                self.r = [R(f"{name}{i}") for i in range(n)]
                self.i = 0
                self.n = n

            def next(self):
                k = self.i % self.n
                self.i += 1
                return self.t[k], self.r[k]

        def barrier():
            keys = list(kb.count.keys())
            for e_ in KB.ENGS:
                for k in keys:
                    v = kb.count[k]
                    if v > 0 and kb.waited[e_].get(k, 0) < v and k != e_:
                        kb.waited[e_][k] = v
                        kb.prog[e_].append(("wait", k, v))

        PS = [st.enter_context(nc.psum_tensor(f"ps{i}", [128, 512], F32)) for i in range(7)]
        PR = [R(f"ps{i}") for i in range(7)]
        PB = st.enter_context(nc.psum_tensor("psb", [128, 1024], BF16))
        PBR = R("psb")

        identF = sb("identF", [128, 128], F32); r_identF = R()
        identB = sb("identB", [128, 128], BF16); r_identB = R()
        ident4F = sb("ident4F", [128, 512], F32); r_ident4F = R()
        masku = sb("masku", [128, 128], BF16); r_masku = R()
        maskl = sb("maskl", [128, 128], BF16); r_maskl = R()
        selA = sb("selA", [128, 512], F32); r_selA = R()
        onesF = sb("onesF", [128, 128], F32); r_onesF = R()
        onesB = sb("onesB", [128, 128], BF16); r_onesB = R()
        nhalf = sb("nhalf", [128, 512], F32); r_nhalf = R()
        onesS = sb("onesS", [128, 1024], BF16); r_onesS = R()
        ld(identF[:], identF_d[:, :], [r_identF])
        ldc(identB[:], identF_d[:, :], [r_identB])
        for h in range(4):
            ld(ident4F[:, h * 128:(h + 1) * 128], identF_d[:, :], [r_ident4F])
        ldc(masku[:], mask_u_d[:, :], [r_masku])
        ldc(maskl[:], mask_l_d[:, :], [r_maskl])
        ld(selA[:], selA_d[:, :], [r_selA])
        ms("dve", onesF[:], 1.0, [r_onesF])
        ms("dve", onesB[:], 1.0, [r_onesB])
        ms("dve", nhalf[:], -0.5, [r_nhalf])
        ms("dve", onesS[:], 1.0, [r_onesS])

        def colvec(name, src, nch):
            t = sb(name, [128, nch], F32); r = R()
            ld(t[:], src.rearrange("(c p) -> p c", p=128), [r], nonc=True)
            return t, r
        gmixT, r_gmixT = colvec("gmixT", g_mix, 8)
        gffnT, r_gffnT = colvec("gffnT", g_ffn, 8)
        gpleT, r_gpleT = colvec("gpleT", g_ple, 8)
        gonT, r_gonT = colvec("gonT", g_onorm, 1)
        gfin = sb("gfin", [128, D], F32); r_gfin = R()
        ld(gfin[:], g_final[None, :].to_broadcast([128, D]), [r_gfin])
        cw = sb("cw", [128, 12, 4], F32); r_cw = R()
        for j in range(4):
            ld(cw[:, :, j], conv_w[j, :].rearrange("(c p) -> p c", p=128), [r_cw], nonc=True)
        wrt_sb = sb("wrt", [128, 8, 36], BF16); r_wrt = R()
        ldc(wrt_sb[:, :, 0:4], w_group.rearrange("(k p) c -> p k c", p=128), [r_wrt], nonc=True)
        ldc(wrt_sb[:, :, 4:36], w_router.rearrange("(k p) c -> p k c", p=128), [r_wrt], nonc=True)
        brt = sb("brt", [128, 36], F32); r_brt = R()
        ld(brt[:, 0:4], b_group[None, :].to_broadcast([128, 4]), [r_brt])
        ld(brt[:, 4:36], b_router[None, :].to_broadcast([128, 32]), [r_brt])
        tots = sb("tots", [128, max(NT, 8)], F32); r_tots = R()
        C_FF, C_QKV, C_DA, C_DB, C_DZ, C_GF, C_GD = 1536, 1544, 3080, 3084, 3088, 3600, 4624

        def wcols(c0, n):
            return w_in[:, c0:c0 + n].rearrange("(k p) c -> p k c", p=128)

        def wres(name, src, kch, ncol):
            t = sb(name, [128, kch, ncol], BF16); r = R()
            ldc(t[:], src.rearrange("(k p) c -> p k c", p=128), [r])
            return t, r
        prmA = sb("prmA", [128, 2], F32); r_prmA = R()
        prmB = sb("prmB", [128, 2], F32); r_prmB = R()
        ms("dve", prmA[:], 0.0, [r_prmA])
        ms("dve", prmB[:], 0.0, [r_prmB])
        ld(prmA[0:8, 0:1], b_forget[:, None], [r_prmA], nonc=True)
        for o in (32, 64, 96):
            ld(prmA[o:o + 4, 0:1], dt_bias[:, None], [r_prmA], nonc=True)
            ld(prmA[o:o + 4, 1:2], a_log[:, None], [r_prmA], nonc=True)
        ld(prmB[64:68, 0:1], dt_bias[:, None], [r_prmB], nonc=True)
        ld(prmB[64:68, 1:2], a_log[:, None], [r_prmB], nonc=True)
        ts("dve", prmA[0:8, 0:1], prmA[0:8, 0:1], -1.0, None, ALU.mult, None, [r_prmA], [r_prmA])
        act(prmA[:, 1:2], prmA[:, 1:2], AF.Exp, [r_prmA], [r_prmA])
        ts("dve", prmA[:, 1:2], prmA[:, 1:2], -1.0, None, ALU.mult, None, [r_prmA], [r_prmA])
        act(prmB[:, 1:2], prmB[:, 1:2], AF.Exp, [r_prmB], [r_prmB])
        ts("dve", prmB[:, 1:2], prmB[:, 1:2], -1.0, None, ALU.mult, None, [r_prmB], [r_prmB])

        r_wexp = Res("wexp", nowaw=True)
        if stages >= 7:
            for e_ in range(32):
                rows = WX[e_ * 128:(e_ + 1) * 128, :]
                gu = rows[:, 0:4096].rearrange("p (k t f) -> p k t f", k=8, t=2)
                ldc(gu[:, :, 0, :], w_gate[e_].rearrange("(k p) f -> p k f", p=128), [r_wexp], stream="wx")
                ldc(gu[:, :, 1, :], w_up[e_].rearrange("(k p) f -> p k f", p=128), [r_wexp], stream="wx")
                ldc(rows[:, 4096:6144].rearrange("p (c f) -> p c f", c=2), w_down[e_].rearrange("(c p) f -> p c f", p=128),
                    [r_wexp], stream="wx")

        def RD():
            return Res("dram", nowaw=True)
        r_QT = [RD() for _ in range(NSEQ)]; r_KT = [RD() for _ in range(NSEQ)]; r_GQKV = [RD() for _ in range(NSEQ)]
        r_DZ = [RD() for _ in range(NSEQ)]; r_THF = [RD() for _ in range(NSEQ)]; r_THD = [RD() for _ in range(NSEQ)]
        r_aug = [RD() for _ in range(NSEQ)]; r_AT = [RD() for _ in range(NSEQ)]; r_OG = [RD() for _ in range(NSEQ)]
        r_GT = [RD() for _ in range(NSEQ)]; r_out = RD()
        r_Xg = [RD() for _ in range(NSEQ)]; r_Yg = [RD() for _ in range(NSEQ)]; r_X1 = [RD() for _ in range(NSEQ)]
        r_Xgz = [RD() for _ in range(NSEQ)]
        tstart = sb("tstart", [128, 160], F32); r_tstart = R()
        pcol = sb("pcol", [128, 1], F32); r_pcol = R()
        ustrict = sb("ustrict", [128, 128], BF16); r_ustrict = R()
        ld(tstart[:], tstart_d[:, :], [r_tstart])
        ld(pcol[:], pcol_d[:, :], [r_pcol])
        ldc(ustrict[:], ustrict_d[:, :], [r_ustrict])

        junk = sb("junk", [128, D], BF16); r_junk = R()
        st1_rot = Rot("st1", [128, 2], F32, 10)
        MARK0 = arena_off[0]

        def norm_stats(src, r_src):
            s1, r_s1 = st1_rot.next()
            ms("dve", s1[:, 0:1], 0.0, [r_s1])
            act(junk[:], src, AF.Square, [r_src, r_s1], [r_junk, r_s1], accum=s1[:, 0:1])
            ts("dve", s1[:, 0:1], s1[:, 0:1], 1.0 / D, EPS, ALU.mult, ALU.add, [r_s1], [r_s1])
            tt("pool", s1[:, 1:2], s1[:, 0:1], nhalf[:, 0:1], ALU.pow, [r_s1, r_nhalf], [r_s1])
            return s1, r_s1

        def norm_transpose(src, r_src, gT, r_gT, dst_fn, r_dst, xn_rot):
            s1, r_s1 = norm_stats(src, r_src)
            xn, r_xn = xn_rot.next()
            ts("dve", xn[:], src, s1[:, 1:2], None, ALU.mult, None, [r_src, r_s1], [r_xn])
            for c in range(8):
                tr(PB[:, c * 128:(c + 1) * 128], xn[:, c * 128:(c + 1) * 128], identB[:], [r_xn, r_identB], [PBR])
            for c in range(8):
                if c % 2 == 0:
                    act(dst_fn(c), PB[:, c * 128:(c + 1) * 128], AF.Copy, [PBR, r_gT], [r_dst], scale=gT[:, c:c + 1])
                else:
                    ts("dve", dst_fn(c), PB[:, c * 128:(c + 1) * 128], gT[:, c:c + 1], None, ALU.mult, None,
                       [PBR, r_gT], [r_dst])

        scale_q = 64 ** -0.5
        scale_gq = 128 ** -0.5

        for s in range(NSEQ):
            tok0 = s * S
            barrier()
            arena_off[0] = MARK0
            ZA = sb("ZA", [128, S], F32); r_ZA = R("ZA")
            ZB = sb("ZB", [128, S], F32); r_ZB = R("ZB")
            MARK1 = arena_off[0]
            Vall = sb("Vall", [128, NT, 8, 65], BF16); r_V = [R(f"V{i}") for i in range(NT)]
            MARK2 = arena_off[0]
            ms("dve", Vall[:], 1.0, r_V)
            hT = sb("hT", [128, 8, S], BF16)
            r_hT = [R(f"hT{i}") for i in range(NT)]
            wv_sb, r_wv = wres("wv", w_in[:, 1024:1536], 8, 512)
            wgA = sb("wgA", [128, 8, 128], BF16); r_wgA = R()
            wgB = sb("wgB", [128, 8, 128], BF16); r_wgB = R()
            ms("dve", wgA[:], 0.0, [r_wgA])
            ms("dve", wgB[:], 0.0, [r_wgB])
            ldc(wgA[:, :, 0:8], wcols(C_FF, 8), [r_wgA], nonc=True)
            for o in (32, 64, 96):
                ldc(wgA[:, :, o:o + 4], wcols(C_DA, 4), [r_wgA], nonc=True)
            ldc(wgB[:, :, 0:4], wcols(C_DB, 4), [r_wgB], nonc=True)
            ldc(wgB[:, :, 32:36], wcols(C_DB, 4), [r_wgB], nonc=True)
            ldc(wgB[:, :, 64:68], wcols(C_DA, 4), [r_wgB], nonc=True)
            xt_rot = Rot("xt", [128, D], F32, 2)
            xn_rot = Rot("xn", [128, D], BF16, 4)
            stg_rot = Rot("stg", [128, 512], BF16, 6)
            f32_rot = Rot("f32w", [128, 520], F32, 5)
            win_rot = Rot("win", [128, 8, 128], BF16, 3)
            p1st = {}

            def p1_a(i):
                xt, r_xt = xt_rot.next()
                ld(xt[:], x_d[tok0 + i * 128: tok0 + (i + 1) * 128, :], [r_xt])
                s1, r_s1 = norm_stats(xt[:], r_xt)
                xn, r_xn = xn_rot.next()
                ts("dve", xn[:], xt[:], s1[:, 1:2], None, ALU.mult, None, [r_xt, r_s1], [r_xn])
                p1st[i] = (xn, r_xn)

            def p1_b(i):
                xn, r_xn = p1st.pop(i)
                for c in range(8):
                    tr(PB[:, c * 128:(c + 1) * 128], xn[:, c * 128:(c + 1) * 128], identB[:], [r_xn, r_identB], [PBR])
                for c in range(8):
                    dstc = hT[:, c, i * 128:(i + 1) * 128]
                    if c % 2 == 0:
                        act(dstc, PB[:, c * 128:(c + 1) * 128], AF.Copy, [PBR, r_gmixT], [r_hT[i]], scale=gmixT[:, c:c + 1])
                    else:
                        ts("dve", dstc, PB[:, c * 128:(c + 1) * 128], gmixT[:, c:c + 1], None, ALU.mult, None,
                           [PBR, r_gmixT], [r_hT[i]])
            for t in range(NT + 2):
                if t < NT:
                    p1_a(t)
                if 0 <= t - 2 < NT:
                    p1_b(t - 2)
            if stages < 1.1:
                continue
            for i in range(NT):
                b = i % 2
                for k in range(8):
                    mm(PS[b][:, :], hT[:, k, i * 128:(i + 1) * 128], wv_sb[:, k, :], k == 0, k == 7,
                       [r_hT[i], r_wv], [PR[b]])
                cp("act" if i % 2 else "dve", Vall[:, i, :, 0:64],
                   PS[b][:, :].rearrange("p (h d) -> p h d", h=8), [PR[b]], [r_V[i]])
            if stages < 1.2:
                continue
            for g in range(NG):
                gt = [r_hT[4 * g + j] for j in range(4)]
                for (wg_, r_wg_, Z, r_Z, b) in ((wgA, r_wgA, ZA, r_ZA, 2), (wgB, r_wgB, ZB, r_ZB, 3)):
                    for k in range(8):
                        mm(PS[b][:, :], wg_[:, k, :], hT[:, k, g * 512:(g + 1) * 512], k == 0, k == 7,
                           gt + [r_wg_], [PR[b]])
                    cp("act", Z[:, g * 512:(g + 1) * 512], PS[b][:, :], [PR[b]], [r_Z])
            if stages < 1.3:
                continue
            chunks = []
            for c in range(4):
                chunks.append((c * 128, "q", c))
            for c in range(4):
                chunks.append((512 + c * 128, "k", c))
            for c in range(12):
                chunks.append((C_QKV + c * 128, "gdn", c))
            for c in range(4):
                chunks.append((C_DZ + c * 128, "dz", c))
            for c in range(8):
                chunks.append((C_GF + c * 128, "gf", c))
            for c in range(8):
                chunks.append((C_GD + c * 128, "gd", c))
            bsel = 0
            p2cnt = [0]
            p2pend = [None]
            if stages < 1.4:
                chunks = chunks[0:8]
            elif stages < 1.5:
                chunks = chunks[0:20]
            for (c0, kind, ci) in chunks:
                wt, r_wt = win_rot.next()
                ldc(wt[:], wcols(c0, 128), [r_wt], stream="ldw")
                halo, r_halo = None, None
                for g in range(NG):
                    gt = [r_hT[4 * g + j] for j in range(4)]
                    b = 4 + (bsel % 2)
                    bsel += 1
                    for k in range(8):
                        mm(PS[b][:, :], wt[:, k, :], hT[:, k, g * 512:(g + 1) * 512], k == 0, k == 7,
                           gt + [r_wt], [PR[b]])
                    cols = slice(g * 512, (g + 1) * 512)
                    if not (kind == "gdn" and ci < 8) and p2pend[0] is not None:
                        p2pend[0]()
                        p2pend[0] = None
                    stg, r_stg = stg_rot.next()
                    if kind == "q":
                        act(stg[:], PS[b][:, :], AF.Copy, [PR[b]], [r_stg], scale=scale_q)
                        stq(QT[s][ci * 128:(ci + 1) * 128, cols], stg[:], [r_stg], [r_QT[s]])
                    elif kind == "k":
                        cp("dve", stg[:], PS[b][:, :], [PR[b]], [r_stg])
                        stq(KT[s][ci * 128:(ci + 1) * 128, cols], stg[:], [r_stg], [r_KT[s]])
                    elif kind == "dz":
                        act(stg[:], PS[b][:, :], AF.Silu, [PR[b]], [r_stg])
                        stq(DZ[s][ci * 128:(ci + 1) * 128, cols], stg[:], [r_stg], [r_DZ[s]])
                    elif kind in ("gf", "gd"):
                        act(stg[:], PS[b][:, :], AF.Tanh, [PR[b]], [r_stg], scale=0.5)
                        dst, r_d = (THF[s], r_THF[s]) if kind == "gf" else (THD[s], r_THD[s])
                        stq(dst[ci * 128:(ci + 1) * 128, cols], stg[:], [r_stg], [r_d])
                    else:
                        zb, r_zb = f32_rot.next()
                        if g == 0:
                            ms("dve", zb[:, 0:3], 0.0, [r_zb])
                        else:
                            cp("dve", zb[:, 0:3], halo[:, 512:515], [r_halo], [r_zb])
                        cp("act", zb[:, 3:515], PS[b][:, :], [PR[b]], [r_zb])
                        halo, r_halo = zb, r_zb
                        cv, r_cv = f32_rot.next()
                        ts("dve", cv[:, 0:512], zb[:, 3:515], cw[:, ci, 3:4], None, ALU.mult, None, [r_zb, r_cw], [r_cv])
                        for j in range(3):
                            stt(cv[:, 0:512], zb[:, j:j + 512], cw[:, ci, j:j + 1], cv[:, 0:512], ALU.mult, ALU.add,
                                [r_zb, r_cw, r_cv], [r_cv])
                        if ci >= 8:
                            act(stg[:], cv[:, 0:512], AF.Silu, [r_cv], [r_stg])
                            stq(GQKV[s][ci * 128:(ci + 1) * 128, cols], stg[:], [r_stg], [r_GQKV[s]])
                        else:
                            act(cv[:, 0:512], cv[:, 0:512], AF.Silu, [r_cv], [r_cv])
                            sq, r_sq = stg_rot.next()
                            act(sq[:], cv[:, 0:512], AF.Square, [r_cv], [r_sq])
                            pss = p2cnt[0] % 2
                            p2cnt[0] += 1
                            for j in range(4):
                                mm(PS[pss][:, j:j + 1], sq[:, j * 128:(j + 1) * 128], onesB[:, 0:1], True, True,
                                   [r_onesB, r_sq], [PR[pss]], sig=(j == 3))

                            def g2(cv=cv, r_cv=r_cv, stg=stg, r_stg=r_stg, pss=pss, ci=ci, cols=cols):
                                rw, r_rw = f32_rot.next()
                                ts("dve", rw[:, 0:4], PS[pss][:, 0:4], EPS, None, ALU.add, None, [PR[pss]], [r_rw])
                                tt("pool", rw[:, 4:8], rw[:, 0:4], nhalf[:, 0:4], ALU.pow, [r_rw, r_nhalf], [r_rw])
                                for j in range(4):
                                    ts("dve", rw[:, 8 + j * 128:8 + (j + 1) * 128], identF[:], rw[:, 4 + j:5 + j], None, ALU.mult, None,
                                       [r_identF, r_rw], [r_rw])
                                for j in range(4):
                                    mm(PS[6][:, j * 128:(j + 1) * 128], onesF[:], rw[:, 8 + j * 128:8 + (j + 1) * 128], True, True,
                                       [r_onesF, r_rw], [PR[6]], sig=(j == 3))
                                stt(stg[:], cv[:, 0:512], scale_gq if ci < 4 else 1.0, PS[6][:, :], ALU.mult, ALU.mult,
                                    [r_cv, PR[6]], [r_stg])
                                stq(GQKV[s][ci * 128:(ci + 1) * 128, cols], stg[:], [r_stg], [r_GQKV[s]])
                            if p2pend[0] is not None:
                                p2pend[0]()
                            p2pend[0] = g2
            if p2pend[0] is not None:
                p2pend[0]()
                p2pend[0] = None
            if stages < 3:
                continue
            barrier()
            arena_off[0] = MARK2
            augb_rot = Rot("augb", [128, 6, 1024], BF16, 1)
            augf_rot = Rot("augf", [128, 2, 1024], F32, 1)
            def softplus_rows(Z, r_Z, prm, r_prm, lo, n, escale):
                rows = slice(lo, lo + n)
                act(Z[rows, :], Z[rows, :], AF.Exp, [r_Z, r_prm], [r_Z], bias=prm[rows, 0:1], scale=escale)
                act(Z[rows, :], Z[rows, :], AF.Ln, [r_Z], [r_Z], bias=1.0)
                ts("dve", Z[rows, :], Z[rows, :], prm[rows, 1:2], None, ALU.mult, None, [r_Z, r_prm], [r_Z])
            softplus_rows(ZA, r_ZA, prmA, r_prmA, 0, 8, -1.0)
            for o in (32, 64, 96):
                softplus_rows(ZA, r_ZA, prmA, r_prmA, o, 4, 1.0)
            softplus_rows(ZB, r_ZB, prmB, r_prmB, 0, 4, -1.0)
            softplus_rows(ZB, r_ZB, prmB, r_prmB, 32, 4, -1.0)
            softplus_rows(ZB, r_ZB, prmB, r_prmB, 64, 4, 1.0)

            def scan(Z, r_Z, rows, c0, n, init):
                kb.op("dve", lambda e: e.tensor_tensor_scan(out=Z[rows, c0:c0 + n], data0=onesS[rows, 0:n],
                                                            data1=Z[rows, c0:c0 + n], initial=init,
                                                            op0=ALU.mult, op1=ALU.add),
                      reads=[r_Z, r_onesS], writes=[r_Z])
            AB = min(1024, S)
            for blk in range(S // AB):
                scan(ZA, r_ZA, slice(0, 8), blk * AB, AB, 0.0 if blk == 0 else ZA[0:8, blk * AB - 1:blk * AB])
            for blk in range(S // AB):
                cs = slice(blk * AB, (blk + 1) * AB)
                ab, r_ab = augb_rot.next()
                af, r_af = augf_rot.next()
                cp("dve", ab[0:8, 0, 0:AB], ZA[0:8, cs], [r_ZA], [r_ab])
                tt("dve", af[0:8, 0, 0:AB], ZA[0:8, cs], ab[0:8, 0, 0:AB], ALU.subtract, [r_ZA, r_ab], [r_af])
                cp("dve", ab[0:8, 1, 0:AB], af[0:8, 0, 0:AB], [r_af], [r_ab])
                tt("dve", af[0:8, 1, 0:AB], af[0:8, 0, 0:AB], ab[0:8, 1, 0:AB], ALU.subtract, [r_af, r_ab], [r_af])
                cp("dve", ab[0:8, 2, 0:AB], af[0:8, 1, 0:AB], [r_af], [r_ab])
                for j in range(3):
                    ts("dve", ab[0:8, 3 + j, 0:AB], ab[0:8, j, 0:AB], -1.0, None, ALU.mult, None, [r_ab], [r_ab])
                for j in range(3):
                    stq(AUGQ[s][:, j, cs], ab[0:8, j, 0:AB], [r_ab], [r_aug[s]])
                    stq(AUGQ[s][:, 3 + j, cs], onesS[0:8, 0:AB], [r_onesS], [r_aug[s]])
                    stq(AUGK[s][:, j, cs], onesS[0:8, 0:AB], [r_onesS], [r_aug[s]])
                    stq(AUGK[s][:, 3 + j, cs], ab[0:8, 3 + j, 0:AB], [r_ab], [r_aug[s]])
            for n in range(NT):
                c0 = n * 128
                for o in (32, 64, 96):
                    scan(ZA, r_ZA, slice(o, o + 4), c0, 128, 0.0)
                scan(ZB, r_ZB, slice(64, 68), c0, 128, 0.0)
                for o in (64, 96):
                    cp("dve", tots[o:o + 4, n:n + 1], ZA[o:o + 4, c0 + 127:c0 + 128], [r_ZA], [r_tots])
                ts("dve", ZA[64:68, c0:c0 + 128], ZA[64:68, c0:c0 + 128], -1.0, tots[64:68, n:n + 1], ALU.mult, ALU.add,
                   [r_ZA, r_tots], [r_ZA])
                ts("dve", ZA[96:100, c0:c0 + 128], ZA[96:100, c0:c0 + 128], 0.0, tots[96:100, n:n + 1], ALU.mult, ALU.add,
                   [r_ZA, r_tots], [r_ZA])
            act(ZA[64:68, :], ZA[64:68, :], AF.Exp, [r_ZA], [r_ZA])
            act(ZA[96:100, :], ZA[96:100, :], AF.Exp, [r_ZA], [r_ZA])
            act(ZB[0:4, :], ZB[0:4, :], AF.Exp, [r_ZB], [r_ZB])
            tt("dve", ZB[32:36, :], ZB[32:36, :], ZA[32:36, :], ALU.add, [r_ZB, r_ZA], [r_ZB])
            act(ZB[32:36, :], ZB[32:36, :], AF.Exp, [r_ZB], [r_ZB])
            ts("dve", ZB[64:68, :], ZB[64:68, :], -1.0, None, ALU.mult, None, [r_ZB], [r_ZB])
            if stages < 4:
                continue
            barrier()
            arena_off[0] = MARK2
            qa_rot = Rot("qa", [128, S], BF16, 2)
            ka_rot = Rot("ka", [128, S], BF16, 2)
            vh_rot = Rot("vh", [128, NT, 128], BF16, 2)
            for i_ in range(2):
                ms("pool", qa_rot.t[i_][64:128, :], 0.0, [qa_rot.r[i_]])
                ms("pool", ka_rot.t[i_][64:128, :], 0.0, [ka_rot.r[i_]])
                ms("pool", vh_rot.t[i_][:], 0.0, [vh_rot.r[i_]])
            pt_rot = Rot("pt", [128, 512], BF16, 3)
            f32_rot = Rot("f32w", [128, 520], F32, 4)
            stg_rot = Rot("stg", [128, 512], BF16, 4)
            fox_pend = []
            fox_cnt = [0]
            for h in range(8):
                QA, r_QA = qa_rot.next()
                KA, r_KA = ka_rot.next()
                ld(QA[0:64, 0:S], QT[s][h * 64:(h + 1) * 64, :], [r_QA], [r_QT[s]])
                ld(QA[64:70, 0:S], AUGQ[s][h], [r_QA], [r_aug[s]])
                ld(KA[0:64, 0:S], KT[s][h * 64:(h + 1) * 64, :], [r_KA], [r_KT[s]])
                ld(KA[64:70, 0:S], AUGK[s][h], [r_KA], [r_aug[s]])
                Vh, r_Vh = vh_rot.next()
                cp("pool", Vh[:, :, 0:65], Vall[:, :, h, :], r_V, [r_Vh])
                for g in range(NG):
                    last = 4 * g + 3
                    po = 2 + 2 * (fox_cnt[0] % 2)
                    fox_cnt[0] += 1
                    pbc = po + 1

                    def scores(j):
                        m = j - 4 * g
                        c0 = max(m, 0) * 128
                        N = 512 - c0
                        b = j % 2
                        mm(PS[b][:, 0:N], KA[0:FOX_K, j * 128:(j + 1) * 128], QA[0:FOX_K, g * 512 + c0:(g + 1) * 512],
                           True, m < 0, [r_KA, r_QA], [PR[b]], sig=(m < 0))
                        if m >= 0:
                            mm(PS[b][:, 0:128], identB[:], masku[:], False, True, [r_identB, r_masku], [PR[b]])
                    scores(0)
                    for j in range(last + 1):
                        m = j - 4 * g
                        c0 = max(m, 0) * 128
                        N = 512 - c0
                        b = j % 2
                        if j + 1 <= last:
                            scores(j + 1)
                        if j == min(1, last) and len(fox_pend) > 0:
                            fox_pend.pop(0)()
                        pt, r_pt = pt_rot.next()
                        act(pt[:, 0:N], PS[b][:, 0:N], AF.Exp, [PR[b]], [r_pt])
                        mm(PS[po][0:FOX_M, c0:512], Vh[:, j, 0:FOX_M], pt[:, 0:N], j == 0, j == last,
                           [r_Vh, r_pt], [PR[po]])
                        if FOX_FILL:
                            kb.op("pe", lambda e: e.matmul(PS[6][:, 0:FOX_FILL_N], lhsT=identB[:], rhs=onesS[:, 0:FOX_FILL_N], start=True, stop=True),
                                  sig=False)
                    def epilogue(po=po, pbc=pbc, g=g, h=h):
                        ob, r_ob = f32_rot.next()
                        cp("act", ob[0:65, 0:512], PS[po][0:65, :], [PR[po]], [r_ob])
                        mm(PS[pbc][0:64, :], onesF[64:65, 0:64], ob[64:65, 0:512], True, True, [r_onesF, r_ob], [PR[pbc]])
                        rb, r_rb = f32_rot.next()
                        kb.op("dve", lambda e, rb=rb, pbc=pbc: e.reciprocal(rb[0:64, 0:512], PS[pbc][0:64, :]), reads=[PR[pbc]], writes=[r_rb])
                        stg, r_stg = stg_rot.next()
                        tt("dve", stg[0:64, :], ob[0:64, 0:512], rb[0:64, 0:512], ALU.mult, [r_ob, r_rb], [r_stg])
                        stq(ATs[s][h * 64:(h + 1) * 64, g * 512:(g + 1) * 512], stg[0:64, :], [r_stg], [r_AT[s]])
                    fox_pend.append(epilogue)
            while fox_pend:
                fox_pend.pop(0)()
            if stages < 5:
                continue
            barrier()
            arena_off[0] = MARK1
            qkv_rot = Rot("qkv", [128, 12, 128], BF16, 2)
            dz_rot = Rot("dzr", [128, 4, 128], BF16, 3)
            tok_rot = Rot("tok", [128, 128], F32, 4)
            e512_rot = Rot("e512", [128, 512], F32, 16)
            wl_rot = Rot("wl", [128, 512], BF16, 18)
            ws_rot = Rot("ws", [128, 512], F32, 8)
            f32_rot = Rot("f32w", [128, 520], F32, 2)
            stg_rot = Rot("stg", [128, 512], BF16, 3)
            Sst = sb("Sst", [128, 512], F32); r_Sst = R()
            Sbf = sb("Sbf", [128, 512], BF16); r_Sbf = R()
            ms("dve", Sst[:], 0.0, [r_Sst])
            ms("dve", Sbf[:], 0.0, [r_Sbf])
            gst_ = {}
            gst2_ = {}

            def g_pre(n):
                c0 = n * 128
                ccols = slice(c0, c0 + 128)
                qkvT, r_qkv = qkv_rot.next()
                ld(qkvT[:], GQKV[s][:, ccols].rearrange("(c p) t -> p c t", p=128), [r_qkv], [r_GQKV[s]])
                dzT, r_dz = dz_rot.next()
                ld(dzT[:], DZ[s][:, ccols].rearrange("(c p) t -> p c t", p=128), [r_dz], [r_DZ[s]])
                tokA, r_tokA = tok_rot.next()
                tokB, r_tokB = tok_rot.next()
                tr(PS[3][:, 0:128], ZA[:, ccols], identF[:], [r_ZA, r_identF], [PR[3]])
                cp("act", tokA[:], PS[3][:, 0:128], [PR[3]], [r_tokA])
                tr(PS[4][:, 0:128], ZB[:, ccols], identF[:], [r_ZB, r_identF], [PR[4]])
                cp("dve", tokB[:], PS[4][:, 0:128], [PR[4]], [r_tokB])
                HS = [slice(h * 128, (h + 1) * 128) for h in range(4)]
                for h in range(4):
                    kTh = qkvT[:, 4 + h, :]
                    mm(PS[0][:, HS[h]], kTh, kTh, True, True, [r_qkv], [PR[0]], sig=(h == 3))
                for h in range(4):
                    mm(PS[1][:, HS[h]], qkvT[:, 4 + h, :], qkvT[:, h, :], True, True, [r_qkv], [PR[1]], sig=(h == 3))
                for h in range(4):
                    mm(PS[2][:, HS[h]], selA[:, HS[h]], ZA[:, ccols], True, False, [r_selA, r_ZA], [PR[2]], sig=False)
                    mm(PS[2][:, HS[h]], identB[:], masku[:], False, True, [r_identB, r_masku], [PR[2]], sig=(h == 3))
                for h in range(4):
                    mm(PS[3][:, HS[h]], selA[:, HS[h]], ZA[:, ccols], True, False, [r_selA, r_ZA], [PR[3]], sig=False)
                    mm(PS[3][:, HS[h]], identB[:], maskl[:], False, True, [r_identB, r_maskl], [PR[3]], sig=(h == 3))
                for h in range(4):
                    mm(PS[4][:, HS[h]], selA[:, HS[h]], ZA[:, ccols], True, True, [r_selA, r_ZA], [PR[4]], sig=(h == 3))
                Eu, r_Eu = e512_rot.next()
                El, r_El = e512_rot.next()
                Ep, r_Ep = e512_rot.next()
                for h in range(4):
                    act(Eu[:, HS[h]], PS[2][:, HS[h]], AF.Exp, [PR[2], r_tokB], [r_Eu], bias=tokB[:, 64 + h:65 + h])
                    act(El[:, HS[h]], PS[3][:, HS[h]], AF.Exp, [PR[3], r_tokA], [r_El], bias=tokA[:, 32 + h:33 + h], scale=-1.0)
                act(Ep[:], PS[4][:, :], AF.Exp, [PR[4]], [r_Ep])
                ATt, r_ATt = wl_rot.next()
                tt("dve", ATt[:], PS[1][:, :], Eu[:], ALU.mult, [PR[1], r_Eu], [r_ATt])
                Lt, r_Lt = ws_rot.next()
                for h in range(4):
                    stt(Lt[:, HS[h]], PS[0][:, HS[h]], tokB[:, h:h + 1], El[:, HS[h]], ALU.mult, ALU.mult,
                        [PR[0], r_tokB, r_El], [r_Lt])
                qdec, r_qdec = wl_rot.next()
                tt("dve", qdec[:], qkvT[:, 0:4, :].rearrange("p c t -> p (c t)"), Ep[:], ALU.mult, [r_qkv, r_Ep], [r_qdec])
                for h in range(4):
                    tr(PB[:, HS[h]], qkvT[:, 4 + h, :], identB[:], [r_qkv, r_identB], [PBR])
                    tr(PB[:, 512 + h * 128:512 + (h + 1) * 128], qkvT[:, 8 + h, :], identB[:], [r_qkv, r_identB], [PBR])
                kbg, r_kbg = wl_rot.next()
                kdec, r_kdec = wl_rot.next()
                vb, r_vb = wl_rot.next()
                for h in range(4):
                    ts("dve", kbg[:, HS[h]], PB[:, HS[h]], tokB[:, 32 + h:33 + h], None, ALU.mult, None, [PBR, r_tokB], [r_kbg])
                    act(kdec[:, HS[h]], PB[:, HS[h]], AF.Copy, [PBR, r_tokA], [r_kdec], scale=tokA[:, 64 + h:65 + h])
                    ts("dve", vb[:, HS[h]], PB[:, 512 + h * 128:512 + (h + 1) * 128], tokB[:, h:h + 1], None, ALU.mult, None,
                       [PBR, r_tokB], [r_vb])
                for h in range(4):
                    tr(PS[5][:, HS[h]], Lt[:, HS[h]], identF[:], [r_Lt, r_identF], [PR[5]])
                Mt, r_Mt = ws_rot.next()
                cp("act", Mt[:], PS[5][:, :], [PR[5]], [r_Mt])
                Rt, r_Rt = ws_rot.next()
                tt("dve", Rt[:], ident4F[:], PS[5][:, :], ALU.subtract, [r_ident4F, PR[5]], [r_Rt])
                Pc, r_Pc, Qc, r_Qc = Lt, r_Lt, Mt, r_Mt
                for lvl in range(6):
                    for h in range(4):
                        mm(PS[0][:, HS[h]], Qc[:, HS[h]], Pc[:, HS[h]], True, True, [r_Qc, r_Pc], [PR[0]], sig=(h == 3))
                    if lvl < 5:
                        for h in range(4):
                            mm(PS[1][:, HS[h]], Pc[:, HS[h]], Qc[:, HS[h]], True, True, [r_Qc, r_Pc], [PR[1]], sig=(h == 3))
                    Pn, r_Pn = ws_rot.next()
                    cp("act", Pn[:], PS[0][:, :], [PR[0]], [r_Pn])
                    if lvl < 5:
                        Qn, r_Qn = ws_rot.next()
                        cp("dve", Qn[:], PS[1][:, :], [PR[1]], [r_Qn])
                    for h in range(4):
                        mm(PS[2][:, HS[h]], Pn[:, HS[h]], Rt[:, HS[h]], True, True, [r_Pn, r_Rt], [PR[2]], sig=(h == 3))
                    Rn, r_Rn = ws_rot.next()
                    tt("dve", Rn[:], Rt[:], PS[2][:, :], ALU.add, [r_Rt, PR[2]], [r_Rn])
                    Rt, r_Rt = Rn, r_Rn
                    Pc, r_Pc = Pn, r_Pn
                    if lvl < 5:
                        Qc, r_Qc = Qn, r_Qn
                Rf, r_Rf = Rt, r_Rt
                Rt, r_Rt = wl_rot.next()
                cp("act", Rt[:], Rf[:], [r_Rf], [r_Rt])
                for h in range(4):
                    mm(PS[3][:, HS[h]], kbg[:, HS[h]], Rt[:, HS[h]], True, True, [r_kbg, r_Rt], [PR[3]], sig=(h == 3))
                for h in range(4):
                    mm(PS[4][:, HS[h]], Rt[:, HS[h]], vb[:, HS[h]], True, True, [r_vb, r_Rt], [PR[4]], sig=(h == 3))
                wT, r_wT = wl_rot.next()
                cp("act", wT[:], PS[3][:, :], [PR[3]], [r_wT])
                uu, r_uu = e512_rot.next()
                cp("dve", uu[:], PS[4][:, :], [PR[4]], [r_uu])
                gst_[n] = dict(ccols=ccols, HS=HS, tokA=tokA, r_tokA=r_tokA, dzT=dzT, r_dz=r_dz, ATt=ATt, r_ATt=r_ATt,
                               qdec=qdec, r_qdec=r_qdec, kdec=kdec, r_kdec=r_kdec, wT=wT, r_wT=r_wT, uu=uu, r_uu=r_uu)

            def g_scan(n):
                d_ = gst_.pop(n)
                ccols, HS, tokA, r_tokA, dzT, r_dz = d_['ccols'], d_['HS'], d_['tokA'], d_['r_tokA'], d_['dzT'], d_['r_dz']
                ATt, r_ATt, qdec, r_qdec, kdec, r_kdec = d_['ATt'], d_['r_ATt'], d_['qdec'], d_['r_qdec'], d_['kdec'], d_['r_kdec']
                wT, r_wT, uu, r_uu = d_['wT'], d_['r_wT'], d_['uu'], d_['r_uu']
                for h in range(4):
                    mm(PS[5][:, HS[h]], wT[:, HS[h]], Sbf[:, HS[h]], True, True, [r_wT, r_Sbf], [PR[5]], sig=(h == 3))
                vnew, r_vnew = wl_rot.next()
                tt("dve", vnew[:], uu[:], PS[5][:, :], ALU.subtract, [r_uu, PR[5]], [r_vnew])
                for h in range(4):
                    mm(PS[6][:, HS[h]], Sbf[:, HS[h]], qdec[:, HS[h]], True, False, [r_Sbf, r_qdec], [PR[6]], sig=False)
                    mm(PS[6][:, HS[h]], vnew[:, HS[h]], ATt[:, HS[h]], False, True, [r_vnew, r_ATt], [PR[6]], sig=(h == 3))
                for h in range(4):
                    mm(PS[0][:, HS[h]], kdec[:, HS[h]], vnew[:, HS[h]], True, True, [r_kdec, r_vnew], [PR[0]], sig=(h == 3))
                for h in range(4):
                    stt(Sst[:, HS[h]], Sst[:, HS[h]], tokA[:, 96 + h:97 + h], PS[0][:, HS[h]], ALU.mult, ALU.add,
                        [r_Sst, r_tokA, PR[0]], [r_Sst])
                cp("act", Sbf[:], Sst[:], [r_Sst], [r_Sbf])
                osb, r_osb = e512_rot.next()
                cp("act", osb[:], PS[6][:, :], [PR[6]], [r_osb])
                sq, r_sq = wl_rot.next()
                act(sq[:], osb[:], AF.Square, [r_osb], [r_sq])
                for h in range(4):
                    mm(PS[1][:, h:h + 1], sq[:, HS[h]], onesB[:, 0:1], True, True, [r_onesB, r_sq], [PR[1]], sig=(h == 3))
                rw, r_rw = f32_rot.next()
                ts("dve", rw[:, 0:4], PS[1][:, 0:4], 1.0 / 128, EPS, ALU.mult, ALU.add, [PR[1]], [r_rw])
                tt("pool", rw[:, 4:8], rw[:, 0:4], nhalf[:, 0:4], ALU.pow, [r_rw, r_nhalf], [r_rw])
                gst2_[n] = dict(ccols=ccols, HS=HS, dzT=dzT, r_dz=r_dz, osb=osb, r_osb=r_osb, rw=rw, r_rw=r_rw)

            def g_scanb(n):
                d_ = gst2_.pop(n)
                ccols, HS, dzT, r_dz, osb, r_osb, rw, r_rw = (d_['ccols'], d_['HS'], d_['dzT'], d_['r_dz'], d_['osb'], d_['r_osb'],
                                                              d_['rw'], d_['r_rw'])
                Dg, r_Dg = e512_rot.next()
                for h in range(4):
                    ts("dve", Dg[:, HS[h]], identF[:], rw[:, 4 + h:5 + h], None, ALU.mult, None, [r_identF, r_rw], [r_Dg])
                for h in range(4):
                    mm(PS[2][:, HS[h]], onesF[:], Dg[:, HS[h]], True, True, [r_onesF, r_Dg], [PR[2]], sig=(h == 3))
                o1, r_o1 = e512_rot.next()
                stt(o1[:], osb[:], gonT[:, 0:1], PS[2][:, :], ALU.mult, ALU.mult, [r_osb, r_gonT, PR[2]], [r_o1])
                stg, r_stg = stg_rot.next()
                tt("dve", stg[:], o1[:], dzT[:].rearrange("p c t -> p (c t)"), ALU.mult, [r_o1, r_dz], [r_stg])
                stq(OGs[s][:, ccols].rearrange("(h p) c -> p h c", p=128), stg[:].rearrange("p (h c) -> p h c", h=4),
                    [r_stg], [r_OG[s]])
            for t in range(NT + 2):
                if t < NT:
                    g_pre(t)
                if 0 <= t - 1 < NT:
                    g_scan(t - 1)
                if 0 <= t - 2 < NT:
                    g_scanb(t - 2)
            if stages < 6:
                continue
            barrier()
            arena_off[0] = MARK0
            NSLT = (2 * S) // 128 + 32
            Tt = sb("Tt", [128, NT, D], BF16); r_Tt = [R() for _ in range(NT)]
            A12 = sb("A12", [128, NT, 64], F32); r_A12 = [R() for _ in range(NT)]
            RTW = sb("RTW", [128, NT, 2], F32); r_RTW = [R() for _ in range(NT)]
            RK = sb("RK", [128, NT, 32], F32); r_RK = [R() for _ in range(NT)]
            SLf = sb("SLf", [128, NT, 2], F32); r_SLf = R()
            SLi = sb("SLi", [128, NT, 2], mybir.dt.int32); r_SLi = R()
            WIf = sb("WIf", [128, NSLT], F32); r_WIf = R()
            WIi = sb("WIi", [128, NSLT], mybir.dt.int32); r_WIi = R()
            cnt = sb("cnt", [128, 5, 32], F32); r_cnt = R()
            gffn_b = sb("gffn_b", [128, D], F32); r_gffn_b = R()
            ld(gffn_b[:], g_ffn[None, :].to_broadcast([128, D]), [r_gffn_b])
            MARKT = arena_off[0]
            wout_sb, r_wout = wres("wout", w_out, 8, D)
            wofox_sb, r_wofox = wres("wofox", w_o_fox, 4, D)
            wodel_sb, r_wodel = wres("wodel", w_o_delta, 4, D)
            xg = sb("xg", [128, 4, D], F32); r_xg = [R() for _ in range(4)]
            mrg = sb("mrg", [128, 8, 512], BF16); r_mrg = R()
            at_rot = Rot("atr", [128, 4, 512], BF16, 2)
            th_rot = Rot("thr", [128, 8, 512], BF16, 2)
            e512_rot = Rot("e512", [128, 512], F32, 4)
            rt_rot = Rot("rtr", [128, 128], F32, 2)
            tTt_rot = Rot("tTt", [128, 8, 128], BF16, 2)
            zt = sb("zt", [128, D], BF16); r_zt = R()
            ms("dve", zt[:], 0.0, [r_zt])
            for i in range(NSLT):
                stq(Xg[s][i * 128:(i + 1) * 128, :], zt[:], [r_zt], [r_Xgz[s]])
            for g in range(NG):
                cols = slice(g * 512, (g + 1) * 512)
                atT, r_atT = at_rot.next()
                ogT, r_ogT = at_rot.next()
                ld(atT[:], ATs[s][:, cols].rearrange("(k p) t -> p k t", p=128), [r_atT], [r_AT[s]])
                ld(ogT[:], OGs[s][:, cols].rearrange("(k p) t -> p k t", p=128), [r_ogT], [r_OG[s]])
                thf, r_thf = th_rot.next()
                thd, r_thd = th_rot.next()
                ld(thf[:], THF[s][:, cols].rearrange("(k p) t -> p k t", p=128), [r_thf], [r_THF[s]])
                ld(thd[:], THD[s][:, cols].rearrange("(k p) t -> p k t", p=128), [r_thd], [r_THD[s]])
                for j in range(4):
                    r0 = tok0 + g * 512 + j * 128
                    ld(xg[:, j, :], x_d[r0:r0 + 128, :], [r_xg[j]])
                for m in range(8):
                    ms_ = slice(m * 128, (m + 1) * 128)
                    for k in range(4):
                        mm(PS[0][:, :], wofox_sb[:, k, ms_], atT[:, k, :], k == 0, k == 3, [r_wofox, r_atT], [PR[0]])
                    for k in range(4):
                        mm(PS[1][:, :], wodel_sb[:, k, ms_], ogT[:, k, :], k == 0, k == 3, [r_wodel, r_ogT], [PR[1]])
                    m1, r_m1 = e512_rot.next()
                    m2, r_m2 = e512_rot.next()
                    stt(m1[:], thf[:, m, :], 1.0, PS[0][:, :], ALU.add, ALU.mult, [r_thf, PR[0]], [r_m1])
                    stt(m2[:], thd[:, m, :], 1.0, PS[1][:, :], ALU.add, ALU.mult, [r_thd, PR[1]], [r_m2])
                    tt("pool", mrg[:, m, :], m1[:], m2[:], ALU.add, [r_m1, r_m2], [r_mrg])
                for j in range(4):
                    for hf in range(2):
                        b = 2 + hf
                        for k in range(8):
                            mm(PS[b][:, :], mrg[:, k, j * 128:(j + 1) * 128], wout_sb[:, k, hf * 512:(hf + 1) * 512],
                               k == 0, k == 7, [r_mrg, r_wout], [PR[b]])
                        stt(xg[:, j, hf * 512:(hf + 1) * 512], PS[b][:, :], 0.5, xg[:, j, hf * 512:(hf + 1) * 512],
                            ALU.mult, ALU.add, [PR[b], r_xg[j]], [r_xg[j]])
                for j in range(4):
                    n = 4 * g + j
                    stq(X1[s][n * 128:(n + 1) * 128, :], xg[:, j, :], [r_xg[j]], [r_X1[s]])
                    s1, r_s1 = norm_stats(xg[:, j, :], r_xg[j])
                    stt(Tt[:, n, :], xg[:, j, :], s1[:, 1:2], gffn_b[:], ALU.mult, ALU.mult, [r_xg[j], r_s1, r_gffn_b], [r_Tt[n]])
                    for c in range(8):
                        tr(PB[:, c * 128:(c + 1) * 128], Tt[:, n, c * 128:(c + 1) * 128], identB[:], [r_Tt[n], r_identB], [PBR])
                    tTt, r_tTt = tTt_rot.next()
                    cp("act", tTt[:].rearrange("p c t -> p (c t)"), PB[:, :], [PBR], [r_tTt])
                    for k in range(8):
                        mm(PS[4][:, 0:36], tTt[:, k, :], wrt_sb[:, k, :], k == 0, k == 7, [r_tTt, r_wrt], [PR[4]])
                    rt, r_rt = rt_rot.next()
                    lg = rt[:, 0:36]
                    tt("dve", lg, PS[4][:, 0:36], brt[:], ALU.add, [PR[4], r_brt], [r_rt])
                    gmax = rt[:, 40:41]; ngm = rt[:, 41:42]; sg = rt[:, 42:43]; psel = rt[:, 43:44]
                    oh = rt[:, 44:48]; el = rt[:, 48:56]; m8 = rt[:, 56:64]; msk = rt[:, 64:72]; a1 = rt[:, 72:80]
                    nm1 = rt[:, 80:81]; e21 = rt[:, 81:82]; cc = rt[:, 82:83]; eg = rt[:, 84:88]; a2 = rt[:, 88:96]
                    rr_ = [r_rt]
                    kb.op("dve", lambda e, lg=lg, gmax=gmax: e.tensor_reduce(out=gmax, in_=lg[:, 0:4], axis=mybir.AxisListType.X, op=ALU.max),
                          reads=rr_, writes=rr_)
                    ts("dve", oh, lg[:, 0:4], gmax, None, ALU.is_ge, None, rr_, rr_)
                    ts("dve", ngm, gmax, -1.0, None, ALU.mult, None, rr_, rr_)
                    ms("dve", sg, 0.0, rr_)
                    act(eg, lg[:, 0:4], AF.Exp, rr_, rr_, bias=ngm, accum=sg)
                    kb.op("dve", lambda e, psel=psel, sg=sg: e.reciprocal(psel, sg), reads=rr_, writes=rr_)
                    ts("dve", el, lg[:, 4:12], oh[:, 0:1], None, ALU.mult, None, rr_, rr_)
                    for gg in range(1, 4):
                        stt(el, lg[:, 4 + 8 * gg:12 + 8 * gg], oh[:, gg:gg + 1], el, ALU.mult, ALU.add, rr_, rr_)
                    kb.op("dve", lambda e, m8=m8, el=el: e.max(out=m8, in_=el), reads=rr_, writes=rr_)
                    ts("dve", msk, el, m8[:, 1:2], None, ALU.is_ge, None, rr_, rr_)
                    ts("dve", a1, el, m8[:, 0:1], None, ALU.is_ge, None, rr_, rr_)
                    tt("dve", a2, msk, a1, ALU.subtract, rr_, rr_)
                    ts("dve", nm1, m8[:, 0:1], -1.0, None, ALU.mult, None, rr_, rr_)
                    act(e21, m8[:, 1:2], AF.Exp, rr_, rr_, bias=nm1)
                    ts("dve", cc, e21, 1.0, None, ALU.add, None, rr_, rr_)
                    kb.op("dve", lambda e, cc=cc: e.reciprocal(cc, cc), reads=rr_, writes=rr_)
                    tt("dve", RTW[:, n, 0:1], cc, psel, ALU.mult, rr_, [r_RTW[n]])
                    tt("dve", RTW[:, n, 1:2], RTW[:, n, 0:1], e21, ALU.mult, rr_ + [r_RTW[n]], [r_RTW[n]])
                    for gg in range(4):
                        ts("dve", A12[:, n, 8 * gg:8 * gg + 8], a1, oh[:, gg:gg + 1], None, ALU.mult, None, rr_, [r_A12[n]])
                        ts("dve", A12[:, n, 32 + 8 * gg:40 + 8 * gg], a2, oh[:, gg:gg + 1], None, ALU.mult, None, rr_, [r_A12[n]])
            barrier()
            arena_off[0] = MARKT
            asum_rot = Rot("asum", [128, 32], BF16, 3)
            tmp_rot = Rot("tmpr", [128, 64], F32, 3)
            ms("dve", cnt[:, 0, :], 0.0, [r_cnt])
            for n in range(NT):
                asum, r_asum = asum_rot.next()
                tt("dve", asum[:], A12[:, n, 0:32], A12[:, n, 32:64], ALU.add, [r_A12[n]], [r_asum])
                mm(PS[0][:, 0:32], ustrict[:], asum[:], True, True, [r_ustrict, r_asum], [PR[0]])
                mm(PS[1][:, 0:32], onesB[:], asum[:], True, True, [r_onesB, r_asum], [PR[1]])
                tt("dve", RK[:, n, :], PS[0][:, 0:32], cnt[:, 0, :], ALU.add, [PR[0], r_cnt], [r_RK[n]])
                tt("dve", cnt[:, 0, :], cnt[:, 0, :], PS[1][:, 0:32], ALU.add, [PR[1], r_cnt], [r_cnt])
            for e_ in range(32):
                tmp, r_tmp = tmp_rot.next()
                ts("dve", tmp[:, 0:64], tstart[:, 0:64], cnt[:, 0, e_:e_ + 1], None, ALU.is_lt, None, [r_tstart, r_cnt], [r_tmp])
                kb.op("dve", lambda e, tmp=tmp, e_=e_, cnt=cnt: e.tensor_reduce(out=cnt[:, 2, e_:e_ + 1], in_=tmp[:, 0:64],
                                                                        axis=mybir.AxisListType.X, op=ALU.add),
                      reads=[r_tmp], writes=[r_cnt])
            ts("dve", cnt[:, 2, :], cnt[:, 2, :], 128.0, None, ALU.mult, None, [r_cnt], [r_cnt])
            kb.op("dve", lambda e, o_=cnt[:, 3, :], d_=cnt[:, 2, :]: e.tensor_tensor_scan(out=o_, data0=onesS[:, 0:32], data1=d_, initial=0.0,
                                                                             op0=ALU.mult, op1=ALU.add), reads=[r_cnt, r_onesS], writes=[r_cnt])
            tt("dve", cnt[:, 4, :], cnt[:, 3, :], cnt[:, 2, :], ALU.subtract, [r_cnt], [r_cnt])
            ms("dve", WIf[:], 0.0, [r_WIf])
            for e_ in range(32):
                stt(WIf[:], tstart[:, 0:NSLT], cnt[:, 3, e_:e_ + 1], WIf[:], ALU.is_ge, ALU.add, [r_tstart, r_cnt, r_WIf], [r_WIf])
            ts("dve", WIf[:], WIf[:], 31.0, None, ALU.min, None, [r_WIf], [r_WIf])
            ts("dve", WIf[:], WIf[:], 128.0, pcol[:, 0:1], ALU.mult, ALU.add, [r_WIf, r_pcol], [r_WIf])
            cp("dve", WIi[:], WIf[:], [r_WIf], [r_WIi])
            for n in range(NT):
                tmp, r_tmp = tmp_rot.next()
                tt("dve", tmp[:, 0:32], RK[:, n, :], cnt[:, 4, :], ALU.add, [r_RK[n], r_cnt], [r_tmp])
                for k in range(2):
                    tt("dve", tmp[:, 32:64], tmp[:, 0:32], A12[:, n, 32 * k:32 * k + 32], ALU.mult, [r_tmp, r_A12[n]], [r_tmp])
                    kb.op("dve", lambda e, tmp=tmp, n=n, k=k, SLf=SLf: e.tensor_reduce(out=SLf[:, n, k:k + 1], in_=tmp[:, 32:64],
                                                                              axis=mybir.AxisListType.X, op=ALU.add),
                          reads=[r_tmp], writes=[r_SLf])
            cp("dve", SLi[:], SLf[:], [r_SLf], [r_SLi])
            for n in range(NT):
                for k in range(2):
                    kb.dma("pool", "sc", lambda e, o_=Xg[s][:, :], i_=SLi[:, n, k:k + 1], t_=Tt[:, n, :]: e.indirect_dma_start(
                        out=o_, out_offset=bass.IndirectOffsetOnAxis(ap=i_, axis=0),
                        in_=t_, in_offset=None), reads=[r_Tt[n], r_SLi, r_Xgz[s]], writes=[r_Xg[s]])
            wx_rot = Rot("wx", [128, 6144], BF16, 4)
            xs_rot = Rot("xs", [128, D], BF16, 3)
            xsT_rot = Rot("xsT", [128, 8, 128], BF16, 3)
            hs_rot = Rot("hs", [128, 256], F32, 2)
            hb_rot = Rot("hb", [128, 256], BF16, 3)
            hT_rot = Rot("hTr", [128, 2, 128], BF16, 2)
            ys_rot = Rot("ys", [128, D], BF16, 2)
            PB2 = PS[6][:, :].bitcast(BF16)
            cst = {}

            def c_load(i):
                wx, r_wx = wx_rot.next()
                kb.dma("pool", "wxg", lambda e, o_=wx[:], i_=WIi[:, i:i + 1], w_=WX[:, :]: e.indirect_dma_start(
                    out=o_, out_offset=None, in_=w_,
                    in_offset=bass.IndirectOffsetOnAxis(ap=i_, axis=0)), reads=[r_WIi, r_wexp], writes=[r_wx])
                xs, r_xs = xs_rot.next()
                ld(xs[:], Xg[s][i * 128:(i + 1) * 128, :], [r_xs], [r_Xg[s]])
                cst[i] = dict(wx=wx, r_wx=r_wx, xs=xs, r_xs=r_xs)

            def c_tr(i):
                d_ = cst[i]
                for c in range(8):
                    tr(PB[:, c * 128:(c + 1) * 128], d_["xs"][:, c * 128:(c + 1) * 128], identB[:], [d_["r_xs"], r_identB], [PBR])
                xsT, r_xsT = xsT_rot.next()
                cp("dve", xsT[:].rearrange("p c t -> p (c t)"), PB[:, :], [PBR], [r_xsT])
                d_["xsT"], d_["r_xsT"] = xsT, r_xsT

            def c_gu(i):
                d_ = cst[i]
                b0 = 3 * (i % 2)
                for k in range(8):
                    mm(PS[b0][:, :], d_["xsT"][:, k, :], d_["wx"][:, k * 512:(k + 1) * 512], k == 0, k == 7,
                       [d_["r_xsT"], d_["r_wx"]], [PR[b0]])
                hsg, r_hsg = hs_rot.next()
                act(hsg[:], PS[b0][:, 0:256], AF.Silu, [PR[b0]], [r_hsg])
                hb, r_hb = hb_rot.next()
                tt("dve", hb[:], hsg[:], PS[b0][:, 256:512], ALU.mult, [r_hsg, PR[b0]], [r_hb])
                d_["hb"], d_["r_hb"] = hb, r_hb

            def c_down(i):
                d_ = cst.pop(i)
                b0 = 3 * (i % 2)
                for c in range(2):
                    tr(PB2[:, c * 128:(c + 1) * 128], d_["hb"][:, c * 128:(c + 1) * 128], identB[:], [d_["r_hb"], r_identB], [PR[6]])
                hTt, r_hTt = hT_rot.next()
                cp("act", hTt[:].rearrange("p c t -> p (c t)"), PB2[:, 0:256], [PR[6]], [r_hTt])
                ys, r_ys = ys_rot.next()
                wx = d_["wx"]
                for hf in range(2):
                    b = b0 + 1 + hf
                    for c in range(2):
                        mm(PS[b][:, :], hTt[:, c, :], wx[:, 4096 + c * 1024 + hf * 512:4096 + c * 1024 + (hf + 1) * 512],
                           c == 0, c == 1, [r_hTt, d_["r_wx"]], [PR[b]])
                    cp("act" if hf else "dve", ys[:, hf * 512:(hf + 1) * 512], PS[b][:, :], [PR[b]], [r_ys])
                stq(Yg[s][i * 128:(i + 1) * 128, :], ys[:], [r_ys], [r_Yg[s]])
            for t in range(NSLT + 3):
                if t < NSLT:
                    c_load(t)
                if 0 <= t - 1 < NSLT:
                    c_tr(t - 1)
                if 0 <= t - 2 < NSLT:
                    c_gu(t - 2)
                if 0 <= t - 3 < NSLT:
                    c_down(t - 3)
            barrier()
            arena_off[0] = MARKT
            wpg_sb, r_wpg = wres("wpg", w_ple_gate, 8, D)
            wpp_sb, r_wpp = wres("wpp", w_ple_proj, 2, D)
            xw_rot = Rot("xw", [128, D], F32, 6)
            yg_rot = Rot("yg", [128, D], BF16, 8)
            e512_rot = Rot("e512", [128, 512], F32, 4)
            nT_rot = Rot("nTr", [128, 8, 128], BF16, 3)
            xn_rot = Rot("xn", [128, D], BF16, 2)
            stg_rot = Rot("stg", [128, 512], BF16, 6)
            f32_rot = Rot("f32w", [128, 520], F32, 4)
            yt_rot = Rot("yt", [128, D], F32, 2)
            dst = {}

            def d_0(n):
                r0 = tok0 + n * 128
                xw, r_xw = xw_rot.next()
                ld(xw[:], X1[s][n * 128:(n + 1) * 128, :], [r_xw], [r_X1[s]])
                pt_, r_pt_ = f32_rot.next()
                ld(pt_[:, 0:256], p_d[r0:r0 + 128, :], [r_pt_])
                ygs = []
                for k in range(2):
                    yg, r_yg = yg_rot.next()
                    kb.dma("pool", "yg", lambda e, o_=yg[:], y_=Yg[s][:, :], i_=SLi[:, n, k:k + 1]: e.indirect_dma_start(
                        out=o_, out_offset=None, in_=y_,
                        in_offset=bass.IndirectOffsetOnAxis(ap=i_, axis=0)), reads=[r_SLi, r_Yg[s]], writes=[r_yg])
                    ygs.append((yg, r_yg))
                dst[n] = dict(xw=xw, r_xw=r_xw, pt_=pt_, r_pt_=r_pt_, ygs=ygs)

            def d_a(n):
                d_ = dst[n]
                xw, r_xw, pt_, r_pt_ = d_["xw"], d_["r_xw"], d_["pt_"], d_["r_pt_"]
                for k in range(2):
                    yg, r_yg = d_["ygs"][k]
                    stt(xw[:], yg[:], RTW[:, n, k:k + 1], xw[:], ALU.mult, ALU.add, [r_yg, r_RTW[n], r_xw], [r_xw])
                pbf, r_pbf = stg_rot.next()
                cp("dve", pbf[:, 0:256], pt_[:, 0:256], [r_pt_], [r_pbf])
                s1, r_s1 = norm_stats(xw[:], r_xw)
                d_.update(pbf=pbf, r_pbf=r_pbf, s1=s1, r_s1=r_s1)

            def d_b(n):
                d_ = dst[n]
                xw, r_xw, s1, r_s1 = d_["xw"], d_["r_xw"], d_["s1"], d_["r_s1"]
                xn, r_xn = xn_rot.next()
                ts("dve", xn[:], xw[:], s1[:, 1:2], None, ALU.mult, None, [r_xw, r_s1], [r_xn])
                for c in range(8):
                    tr(PB[:, c * 128:(c + 1) * 128], xn[:, c * 128:(c + 1) * 128], identB[:], [r_xn, r_identB], [PBR])
                nT, r_nT = nT_rot.next()
                for c in range(8):
                    if c % 2 == 0:
                        act(nT[:, c, :], PB[:, c * 128:(c + 1) * 128], AF.Copy, [PBR, r_gpleT], [r_nT], scale=gpleT[:, c:c + 1])
                    else:
                        ts("dve", nT[:, c, :], PB[:, c * 128:(c + 1) * 128], gpleT[:, c:c + 1], None, ALU.mult, None,
                           [PBR, r_gpleT], [r_nT])
                PB2 = PS[6][:, :].bitcast(BF16)
                for k in range(2):
                    tr(PB2[:, k * 128:(k + 1) * 128], d_["pbf"][:, k * 128:(k + 1) * 128], identB[:], [d_["r_pbf"], r_identB], [PR[6]])
                pT, r_pT = stg_rot.next()
                cp("act", pT[:, 0:256], PB2[:, 0:256], [PR[6]], [r_pT])
                d_.update(nT=nT, r_nT=r_nT, pT=pT, r_pT=r_pT)

            def d_c(n):
                d_ = dst.pop(n)
                r0 = tok0 + n * 128
                xw, r_xw, nT, r_nT, pT, r_pT = d_["xw"], d_["r_xw"], d_["nT"], d_["r_nT"], d_["pT"], d_["r_pT"]
                for hf in range(2):
                    hs_ = slice(hf * 512, (hf + 1) * 512)
                    for k in range(8):
                        mm(PS[0 + 2 * hf][:, :], nT[:, k, :], wpg_sb[:, k, hs_], k == 0, k == 7, [r_nT, r_wpg], [PR[0 + 2 * hf]])
                    for k in range(2):
                        mm(PS[1 + 2 * hf][:, :], pT[:, k * 128:(k + 1) * 128], wpp_sb[:, k, hs_], k == 0, k == 1, [r_pT, r_wpp], [PR[1 + 2 * hf]])
                    th, r_th = e512_rot.next()
                    act(th[:], PS[0 + 2 * hf][:, :], AF.Tanh, [PR[0 + 2 * hf]], [r_th], scale=0.5)
                    ts("dve", th[:], th[:], 0.5, 0.5, ALU.mult, ALU.add, [r_th], [r_th])
                    tt("dve", th[:], th[:], PS[1 + 2 * hf][:, :], ALU.mult, [r_th, PR[1 + 2 * hf]], [r_th])
                    tt("dve", xw[:, hs_], xw[:, hs_], th[:], ALU.add, [r_xw, r_th], [r_xw])
                s1, r_s1 = norm_stats(xw[:], r_xw)
                yt, r_yt = yt_rot.next()
                stt(yt[:], xw[:], s1[:, 1:2], gfin[:], ALU.mult, ALU.mult, [r_xw, r_s1, r_gfin], [r_yt])
                stq(out_d[r0:r0 + 128, :], yt[:], [r_yt], [r_out], stream="sto")
            for t in range(NT + 4):
                if t < NT:
                    d_0(t)
                if 0 <= t - 2 < NT:
                    d_a(t - 2)
                if 0 <= t - 3 < NT:
                    d_b(t - 3)
                if 0 <= t - 4 < NT:
                    d_c(t - 4)
        barrier()
        kb.emit()
    nc.dbg_names = dbg_names
    nc.arena_hw = arena_hw[0]
    return nc


WKEYS = ['g_mix', 'w_in', 'b_forget', 'conv_w', 'a_log', 'dt_bias', 'g_onorm', 'w_o_fox', 'w_o_delta', 'w_out', 'g_ffn',
         'w_group', 'b_group', 'w_router', 'b_router', 'w_gate', 'w_up', 'w_down', 'g_ple', 'w_ple_gate', 'w_ple_proj']
_NC_CACHE = {}


def kernel(**inputs):
    x = np.ascontiguousarray(np.asarray(inputs['x'], dtype=np.float32))
    p = np.ascontiguousarray(np.asarray(inputs['p'], dtype=np.float32))
    B, S, _ = x.shape
    nseq = B // NCORES
    shared = {k: np.ascontiguousarray(np.asarray(inputs[k], dtype=np.float32)[0]) for k in WKEYS}
    shared['g_final'] = np.ascontiguousarray(np.asarray(inputs['g_final'], dtype=np.float32))
    shared.update(host_consts())
    key = (nseq, S)
    if key not in _NC_CACHE:
        _NC_CACHE[key] = build(nseq, S)
    nc = _NC_CACHE[key]
    in_maps = []
    for c in range(NCORES):
        m = dict(shared)
        m['x'] = x[c * nseq:(c + 1) * nseq].reshape(nseq * S, D)
        m['p'] = p[0, c * nseq:(c + 1) * nseq].reshape(nseq * S, 256)
        in_maps.append(m)
    res = run_bass_kernel_spmd(nc, in_maps, core_ids=list(range(NCORES)))
    out = np.concatenate([np.asarray(r['out']).reshape(nseq, S, D) for r in res.results], axis=0)
    return out.astype(np.float32)
```
